# Optimizing a Trainium2 kernel written in Bass

```python
import jax, jax.numpy as jnp
from jax import lax
import numpy as np

D_MODEL = 2048
BATCH = 1
SEQ = 16384
DEPTH = 2

GRID_W = 64
CTX_LEN = 256
N_HEADS = 16
N_KV_HEADS = 4
HEAD_DIM = 128
GQA_GROUP = N_HEADS // N_KV_HEADS
Q_BLOCK = 128
ROPE_THETA = 10000.0
ROPE_FREQS = HEAD_DIM // 4
ATTN_SCALE = HEAD_DIM ** -0.5
FOURIER_DIM = D_MODEL // 2
N_FOURIER_GROUPS = 4
FOURIER_GROUP = FOURIER_DIM // N_FOURIER_GROUPS
D_FF = 5632
N_EXPERTS = 8
TOP_K = 2
D_FF_EXPERT = D_FF // TOP_K
N_DENSE = (DEPTH + 1) // 2
N_MOE = DEPTH // 2
EPS = 1e-6
Q_W = N_HEADS * HEAD_DIM
KV_W = N_KV_HEADS * HEAD_DIM
IN_COLS = Q_W + 2 * KV_W + FOURIER_DIM + 2 * D_MODEL
SPLITS = (Q_W, Q_W + KV_W, Q_W + 2 * KV_W, Q_W + 2 * KV_W + FOURIER_DIM)

kernel_name = "hybrid_gqa_fnet_moe_diffusion_block"


def rms_norm(t, g):
    tf = t.astype(jnp.float32)
    y = tf * lax.rsqrt(jnp.mean(tf * tf, axis=-1, keepdims=True) + EPS)
    return (y * g.astype(jnp.float32)).astype(t.dtype)


def modulate(h, shift, scale):
    return h * (1.0 + scale) + shift


def ada_modulation(cond, w, b):
    m = jax.nn.silu(cond) @ w + b
    return jnp.split(m[:, None, :], 6, axis=-1)


def axial_rope_tables(rows):
    row = jnp.repeat(jnp.arange(rows, dtype=jnp.float32), GRID_W)
    col = jnp.tile(jnp.arange(GRID_W, dtype=jnp.float32), rows)
    inv = ROPE_THETA ** (-jnp.arange(ROPE_FREQS, dtype=jnp.float32) / ROPE_FREQS)
    ar = row[:, None] * inv
    ac = col[:, None] * inv
    ang = jnp.concatenate([ar, ar, ac, ac], axis=-1)
    return jnp.cos(ang), jnp.sin(ang)


def apply_rope(t, cos, sin):
    tf = t.astype(jnp.float32)
    seg = tf.reshape(*t.shape[:-1], 2, 2, ROPE_FREQS)
    rot = jnp.stack([-seg[..., 1, :], seg[..., 0, :]], axis=-2).reshape(t.shape)
    return (tf * cos[:, None, :] + rot * sin[:, None, :]).astype(t.dtype)


def _heads(t, n):
    return t.reshape(*t.shape[:-1], n, HEAD_DIM)


def project(h, w_in, b_gate, q_g, k_g):
    p = h @ w_in
    q, k, v, f, g = jnp.split(p, SPLITS, axis=-1)
    q = rms_norm(_heads(q, N_HEADS), q_g)
    k = rms_norm(_heads(k, N_KV_HEADS), k_g)
    v = _heads(v, N_KV_HEADS)
    return q, k, v, f, g + b_gate


def gqa(q, k, v):
    B, Q = q.shape[:2]
    qg = q.reshape(B, Q, N_KV_HEADS, GQA_GROUP, HEAD_DIM)
    s = jnp.einsum('bqhgd,bkhd->bhgqk', qg, k, preferred_element_type=jnp.float32) * ATTN_SCALE
    p = jax.nn.softmax(s, axis=-1).astype(v.dtype)
    o = jnp.einsum('bhgqk,bkhd->bqhgd', p, v)
    return o.reshape(B, Q, N_HEADS * HEAD_DIM)


def latent_attention(q, k_all, v_all):
    B, S = q.shape[:2]
    nb = S // Q_BLOCK
    qb = jnp.moveaxis(q.reshape(B, nb, Q_BLOCK, N_HEADS, HEAD_DIM), 1, 0)
    o = lax.map(lambda qi: gqa(qi, k_all, v_all), qb)
    return jnp.moveaxis(o, 0, 1).reshape(B, S, N_HEADS * HEAD_DIM)


def fourier_mix(u):
    B, N = u.shape[:2]
    ug = u.astype(jnp.float32).reshape(B, N, N_FOURIER_GROUPS, FOURIER_GROUP)
    f = jnp.fft.fftn(ug, axes=(1, 3), norm='ortho').real
    return f.reshape(B, N, FOURIER_DIM).astype(u.dtype)


def merge(attn, four, gate_logits, w_attn_proj, w_four_proj, w_out):
    gates = jax.nn.sigmoid(gate_logits.astype(jnp.float32)).astype(attn.dtype)
    g_attn, g_four = jnp.split(gates, 2, axis=-1)
    y = g_attn * (attn @ w_attn_proj) + g_four * (four @ w_four_proj)
    return y @ w_out


def swiglu(h, wg, wu, wd):
    return (jax.nn.silu(h @ wg) * (h @ wu)) @ wd


def moe_swiglu(h, router_w, wg, wu, wd):
    logits = jnp.einsum('bsd,de->bse', h, router_w, preferred_element_type=jnp.float32)
    top_v, top_i = lax.top_k(logits, TOP_K)
    w = jax.nn.softmax(top_v, axis=-1)
    combine = jnp.sum(jax.nn.one_hot(top_i, N_EXPERTS, dtype=jnp.float32) * w[..., None], axis=-2)
    out = jnp.zeros_like(h)
    for e in range(N_EXPERTS):
        out = out + combine[..., e:e + 1].astype(h.dtype) * swiglu(h, wg[e], wu[e], wd[e])
    return out


def channel_mixer(l, h, ffn_w_gate, ffn_w_up, ffn_w_down, router_w, moe_w_gate, moe_w_up, moe_w_down):
    i = l // 2
    if l % 2 == 0:
        return swiglu(h, ffn_w_gate[i], ffn_w_up[i], ffn_w_down[i])
    return moe_swiglu(h, router_w[i], moe_w_gate[i], moe_w_up[i], moe_w_down[i])


def setup_inputs(seed: int = 0) -> dict:
    key = jax.random.key(seed)
    ks = jax.random.split(key, 24)
    f32 = jnp.float32

    def nrm(k, shape, fan_in):
        return jax.random.normal(k, shape, f32) * (fan_in ** -0.5)

    def gain(k, shape):
        return 1.0 + 0.02 * jax.random.normal(k, shape, f32)

    def small(k, shape):
        return 0.02 * jax.random.normal(k, shape, f32)

    D = D_MODEL
    return {
        'x': jax.random.normal(ks[0], (BATCH, SEQ, D), f32),
        'c': jax.random.normal(ks[1], (BATCH, D), f32),
        'ctx': jax.random.normal(ks[2], (BATCH, CTX_LEN, D), f32),
        'c_ctx': jax.random.normal(ks[3], (D,), f32),
        'ada_w': nrm(ks[4], (DEPTH, D, 6 * D), D),
        'ada_b': small(ks[5], (DEPTH, 6 * D)),
        'norm_attn_g': gain(ks[6], (DEPTH, D)),
        'norm_ffn_g': gain(ks[7], (DEPTH, D)),
        'w_in': nrm(ks[8], (DEPTH, D, IN_COLS), D),
        'b_gate': small(ks[9], (DEPTH, 2 * D)),
        'q_norm_g': gain(ks[10], (DEPTH, HEAD_DIM)),
        'k_norm_g': gain(ks[11], (DEPTH, HEAD_DIM)),
        'w_attn_proj': nrm(ks[12], (DEPTH, Q_W, D), Q_W),
        'w_four_proj': nrm(ks[13], (DEPTH, FOURIER_DIM, D), FOURIER_DIM),
        'w_out': nrm(ks[14], (DEPTH, D, D), D),
        'ffn_w_gate': nrm(ks[15], (N_DENSE, D, D_FF), D),
        'ffn_w_up': nrm(ks[16], (N_DENSE, D, D_FF), D),
        'ffn_w_down': nrm(ks[17], (N_DENSE, D_FF, D), D_FF),
        'router_w': nrm(ks[18], (N_MOE, D, N_EXPERTS), D),
        'moe_w_gate': nrm(ks[19], (N_MOE, N_EXPERTS, D, D_FF_EXPERT), D),
        'moe_w_up': nrm(ks[20], (N_MOE, N_EXPERTS, D, D_FF_EXPERT), D),
        'moe_w_down': nrm(ks[21], (N_MOE, N_EXPERTS, D_FF_EXPERT, D), D_FF_EXPERT),
        'final_norm_g': gain(ks[22], (D,)),
    }


def reference(x, c, ctx, c_ctx, ada_w, ada_b, norm_attn_g, norm_ffn_g, w_in, b_gate, q_norm_g, k_norm_g,
              w_attn_proj, w_four_proj, w_out, ffn_w_gate, ffn_w_up, ffn_w_down, router_w, moe_w_gate,
              moe_w_up, moe_w_down, final_norm_g):
    S = x.shape[1]
    ROWS = S // GRID_W
    cos, sin = axial_rope_tables(ROWS)
    xc = ctx
    for l in range(DEPTH):
        last = l == DEPTH - 1
        sh1, sc1, ga1, sh2, sc2, ga2 = ada_modulation(c, ada_w[l], ada_b[l])
        mod_c = ada_modulation(c_ctx[None, :], ada_w[l], ada_b[l])

        hc = modulate(rms_norm(xc, norm_attn_g[l]), mod_c[0], mod_c[1])
        if last:
            kc, vc = jnp.split(hc @ w_in[l][:, Q_W:Q_W + 2 * KV_W], 2, axis=-1)
            kc = rms_norm(_heads(kc, N_KV_HEADS), k_norm_g[l])
            vc = _heads(vc, N_KV_HEADS)
        else:
            qc, kc, vc, fc, gc = project(hc, w_in[l], b_gate[l], q_norm_g[l], k_norm_g[l])
            yc = merge(gqa(qc, kc, vc), fourier_mix(fc), gc, w_attn_proj[l], w_four_proj[l], w_out[l])
            xc_next = xc + mod_c[2] * yc
            hc2 = modulate(rms_norm(xc_next, norm_ffn_g[l]), mod_c[3], mod_c[4])
            xc_next = xc_next + mod_c[5] * channel_mixer(l, hc2, ffn_w_gate, ffn_w_up, ffn_w_down,
                                                         router_w, moe_w_gate, moe_w_up, moe_w_down)

        h = modulate(rms_norm(x, norm_attn_g[l]), sh1, sc1)
        q, k, v, f, g = project(h, w_in[l], b_gate[l], q_norm_g[l], k_norm_g[l])
        q = apply_rope(q, cos, sin)
        k = apply_rope(k, cos, sin)
        k_all = jnp.concatenate([kc, k], axis=1)
        v_all = jnp.concatenate([vc, v], axis=1)
        y = merge(latent_attention(q, k_all, v_all), fourier_mix(f), g, w_attn_proj[l], w_four_proj[l], w_out[l])
        x = x + ga1 * y
        h2 = modulate(rms_norm(x, norm_ffn_g[l]), sh2, sc2)
        x = x + ga2 * channel_mixer(l, h2, ffn_w_gate, ffn_w_up, ffn_w_down,
                                    router_w, moe_w_gate, moe_w_up, moe_w_down)
        if not last:
            xc = xc_next
    return rms_norm(x, final_norm_g)
```

```python
import math
import numpy as np
import ml_dtypes
import concourse.bass as bass
import concourse.mybir as mybir
from concourse.bass_utils import run_bass_kernel_spmd

F32 = mybir.dt.float32
BF16 = mybir.dt.bfloat16
ALU = mybir.AluOpType
AF = mybir.ActivationFunctionType
AX = mybir.AxisListType
NPBF = ml_dtypes.bfloat16

NCORES = 8
D = 2048
KC = 16
SEQ = 16384
CTX = 256
TPC = SEQ // NCORES
CPC = CTX // NCORES
TT = TPC + CPC
NH, NKV, HD = 16, 4, 128
QW, KVW, FD = 2048, 512, 1024
INC = 8192
DFF = 5632
NE = 8
DFE = 2816
EPS = 1e-6
GRID_W = 64
ENGS = ("pe", "act", "dve", "pool", "sp")


class Buf:
    __slots__ = ("t", "last_w", "readers", "name", "ws")

    def __init__(self, t, name=""):
        self.t = t
        self.last_w = None
        self.ws = []
        self.readers = {}
        self.name = name

    def __getitem__(self, idx):
        return self.t[idx]


class Rot:
    def __init__(self, bufs):
        self.bufs = bufs
        self.i = 0

    def next(self):
        b = self.bufs[self.i % len(self.bufs)]
        self.i += 1
        return b


class Prog:
    def __init__(self, nc, same_engine_sync=True, n_dma_sems=8):
        self.nc = nc
        self.streams = {e: [] for e in ENGS}
        self.cnt = {e: 0 for e in ENGS}
        self.waited = {e: {} for e in ENGS}
        self.same_engine_sync = same_engine_sync
        self.sems = {}
        self.ctx = []
        for e in ENGS:
            self.sems[("eng", e)] = self._sem("s_" + e)
        self.dma_rot = {}
        self.dma_val = {}
        for q in ("sp", "act", "pool"):
            lst = []
            for i in range(n_dma_sems):
                k = ("dma", q + str(i))
                self.sems[k] = self._sem("d_%s%d" % (q, i))
                self.dma_val[k] = 0
                lst.append(k)
            self.dma_rot[q] = [lst, 0]
        self.out_tokens = []

    def _sem(self, name):
        cm = self.nc.semaphore(name)
        s = cm.__enter__()
        self.ctx.append(cm)
        return s

    def sbuf(self, name, shape, dtype):
        cm = self.nc.sbuf_tensor(name, shape, dtype)
        t = cm.__enter__()
        self.ctx.append(cm)
        return Buf(t, name)

    def psum(self, name, shape, dtype=F32):
        cm = self.nc.psum_tensor(name, shape, dtype)
        t = cm.__enter__()
        self.ctx.append(cm)
        return Buf(t, name)

    def rot(self, name, n, shape, dtype, psum=False):
        return Rot([(self.psum if psum else self.sbuf)("%s%d" % (name, i), shape, dtype) for i in range(n)])

    def _collect(self, e, reads, writes, acc=False):
        waits = {}

        def need(tok):
            if tok is None:
                return
            k, v = tok
            if k == ("eng", e):
                if e == "pe" or not self.same_engine_sync:
                    return
            if waits.get(k, 0) < v:
                waits[k] = v

        for b in reads:
            need(b.last_w)
            for t_ in b.ws:
                need(t_)
        for b in writes:
            need(b.last_w)
            for t_ in b.ws:
                if acc and t_[0][0] == "dma":
                    continue
                need(t_)
            for k, v in b.readers.items():
                if k == ("eng", e):
                    continue
                need((k, v))
        wl = []
        for k, v in waits.items():
            if self.waited[e].get(k, 0) >= v:
                continue
            self.waited[e][k] = v
            wl.append((k, v))
        return wl

    def _mark(self, tok, reads, writes, acc=False):
        k, v = tok
        for b in writes:
            if acc:
                b.ws.append(tok)
                continue
            b.last_w = tok
            b.ws = []
            b.readers = {}
        for b in reads:
            if b.readers.get(k, 0) < v:
                b.readers[k] = v

    def emit(self, e, fn, reads=(), writes=(), inc=True):
        wl = self._collect(e, reads, writes)
        if inc:
            self.cnt[e] += 1
            tok = (("eng", e), self.cnt[e])
        else:
            tok = (("eng", e), self.cnt[e] + 1)
        self.streams[e].append((wl, fn, ("eng", e) if inc else None, 1))
        self._mark(tok, reads, writes)
        return tok

    def dma(self, q, out, in_, reads=(), writes=(), is_output=False, acc=False):
        lst, i = self.dma_rot[q]
        k = lst[i % len(lst)]
        self.dma_rot[q][1] = i + 1
        wl = self._collect(q, reads, writes, acc)
        prev = self.dma_val[k]
        if prev > 0 and self.waited[q].get(k, 0) < prev:
            self.waited[q][k] = prev
            wl.append((k, prev))
        self.dma_val[k] = prev + 16
        tok = (k, prev + 16)
        self.streams[q].append((wl, lambda e: e.dma_start(out=out, in_=in_), k, 16))
        self._mark(tok, reads, writes, acc)
        if is_output:
            self.out_tokens.append(tok)
        return tok

    def finish(self):
        fin = {}
        for k, v in self.out_tokens:
            fin[k] = max(fin.get(k, 0), v)
        nc = self.nc
        sems = self.sems
        streams = self.streams
        emap = {"pe": "tensor", "act": "scalar", "dve": "vector", "pool": "gpsimd", "sp": "sync"}
        with nc.Block() as block:
            for e in ENGS:
                def body(eng, e=e):
                    for wl, fn, inc_k, inc_v in streams[e]:
                        for k, v in wl:
                            eng.wait_ge(sems[k], v)
                        ins = fn(eng)
                        if inc_k is not None:
                            ins.then_inc(sems[inc_k], inc_v)
                    if e == "sp":
                        for k, v in fin.items():
                            eng.wait_ge(sems[k], v)
                getattr(block, emap[e])(body)
        for cm in reversed(self.ctx):
            cm.__exit__(None, None, None)
        self.ctx = []


def new_nc():
    return bass.Bass("TRN2", target_bir_lowering=False)


def din(nc, name, shape, dt=F32):
    return nc.dram_tensor(name, list(shape), dt, kind="ExternalInput").ap()


def dout(nc, name, shape, dt=F32):
    return nc.dram_tensor(name, list(shape), dt, kind="ExternalOutput").ap()


def run(nc, in_maps):
    res = run_bass_kernel_spmd(nc, in_maps, core_ids=list(range(NCORES)))
    return res.results


def fm(v):
    v = np.asarray(v)
    return np.ascontiguousarray(v.reshape(-1, 128).T)


NMC = 12


def build_p0():
    nc = new_nc()
    cond = din(nc, "cond", [128, KC, 2])
    adaw = din(nc, "adaw", [2, D, NMC * 128])
    adab = din(nc, "adab", [128, 2, NMC])
    o = dout(nc, "mod", [128, 2, NMC, 2])
    P = Prog(nc)
    cs = P.sbuf("cs", [128, KC, 2], F32)
    cb = P.sbuf("cb", [128, KC, 2], BF16)
    bs = P.sbuf("bs", [128, 2, NMC], F32)
    ob = P.sbuf("ob", [128, 2, NMC, 2], F32)
    wb = [P.sbuf("w%d" % l, [128, KC, NMC * 128], BF16) for l in range(2)]
    ps = P.psum("ps", [128, 2, NMC, 2], F32)
    P.dma("sp", cs[:], cond, writes=[cs])
    P.dma("sp", bs[:], adab, writes=[bs])
    for l in range(2):
        for h in range(2):
            P.dma("pool", wb[l][:, h * 8:(h + 1) * 8, :],
                  adaw[l, h * 1024:(h + 1) * 1024, :].rearrange("(kc p) n -> p kc n", p=128), writes=[wb[l]], acc=(h > 0))
    P.emit("act", lambda e: e.activation(out=cb[:], in_=cs[:], func=AF.Silu), reads=[cs], writes=[cb])
    for l in range(2):
        for j in range(NMC):
            for kc in range(KC):
                P.emit("pe", lambda e, l=l, j=j, kc=kc: e.matmul(
                    ps[:, l, j, :], lhsT=wb[l][:, kc, j * 128:(j + 1) * 128], rhs=cb[:, kc, :],
                    start=(kc == 0), stop=(kc == KC - 1)), reads=[wb[l], cb], writes=[ps],
                    inc=(kc == KC - 1 and j == NMC - 1))
        P.emit("dve", lambda e, l=l: e.tensor_tensor(
            out=ob[:, l], in0=ps[:, l], in1=bs[:, l].unsqueeze(2).to_broadcast([128, NMC, 2]), op=ALU.add),
            reads=[ps, bs], writes=[ob])
    P.dma("sp", o, ob[:], reads=[ob], is_output=True)
    P.finish()
    return nc


def run_p0(inp):
    nc = build_p0()
    cond = np.stack([fm(inp["c"][0]), fm(inp["c_ctx"])], axis=-1).astype(np.float32)
    maps = []
    for i in range(NCORES):
        sl = slice(i * NMC * 128, (i + 1) * NMC * 128)
        adab = np.stack([fm(inp["ada_b"][l, sl]) for l in range(2)], axis=1)
        maps.append({"cond": cond, "adaw": np.ascontiguousarray(inp["ada_w"][:, :, sl]),
                     "adab": np.ascontiguousarray(adab)})
    res = run(nc, maps)
    full = np.concatenate([r["mod"] for r in res], axis=2)
    return full


CHUNKS = [(0, 512), (512, 512), (1024, 512), (1536, 512), (2048, CPC)]
SUBCH = [(i * 128, 128) for i in range(16)] + [(2048, CPC)]


def build_p1():
    nc = new_nc()
    xT = din(nc, "xT", [D, TT])
    w = din(nc, "w", [D, INC])
    modv = din(nc, "modv", [128, 2, 2, KC])
    gain = din(nc, "gain", [128, KC])
    bgate = din(nc, "bgate", [128, 32])
    qkg = din(nc, "qkg", [128, 2])
    cosd = din(nc, "cosT", [128, TT])
    sind = din(nc, "sinT", [128, TT])
    rmd = din(nc, "rm", [128, 128], BF16)
    qT = dout(nc, "qT", [QW, TT], BF16)
    kT = dout(nc, "kT", [KVW, TT], BF16)
    vo = dout(nc, "v", [TT, KVW], BF16)
    fT = dout(nc, "fT", [FD, TT], BF16)
    gT = dout(nc, "gT", [2 * D, TT], BF16)
    P = Prog(nc)
    hT = P.sbuf("hT", [128, KC, TT], BF16)
    cosb = P.sbuf("cosb", [128, TT], F32)
    sinb = P.sbuf("sinb", [128, TT], F32)
    modb = P.sbuf("modb", [128, 2, 2, KC], F32)
    gb = P.sbuf("gb", [128, KC], F32)
    ab = P.sbuf("ab", [128, 2, KC], F32)
    bgb = P.sbuf("bgb", [128, 32], F32)
    qkb = P.sbuf("qkb", [128, 2], F32)
    qks = P.sbuf("qks", [128, 2], F32)
    rmb = P.sbuf("rmb", [128, 128], BF16)
    ones = P.sbuf("ones", [128, 128], BF16)
    xs_rot = P.rot("xs", 2, [128, KC, 128], F32)
    sq_rot = P.rot("sqc", 2, [128, KC, 128], BF16)
    rs_rot = P.rot("rs", 3, [128, 3, 512], F32)
    wrot = P.rot("wp", 3, [128, KC, 512], BF16)
    psm = P.rot("psm", 3, [128, 512], F32, psum=True)
    pss = P.rot("pss", 2, [128, 512], F32, psum=True)
    psr = P.rot("psr", 2, [128, 512], F32, psum=True)
    sqh = P.rot("sqh", 2, [128, 512], BF16)
    qn_rot = P.rot("qn", 3, [128, 512], BF16)
    t1_rot = P.rot("t1", 2, [128, 512], F32)
    t2_rot = P.rot("t2", 2, [128, 512], F32)
    ob_rot = P.rot("ob", 4, [128, 512], BF16)

    for (dst, src) in ((cosb, cosd), (sinb, sind), (modb, modv), (gb, gain), (bgb, bgate), (qkb, qkg), (rmb, rmd)):
        P.dma("sp", dst[:], src, writes=[dst])
    P.emit("pool", lambda e: e.memset(ones[:], 1.0), writes=[ones])
    for r in rs_rot.bufs:
        P.emit("pool", lambda e, r=r: e.memset(r[:, 2, :], -0.5), writes=[r])
    for wi in range(2):
        P.emit("dve", lambda e, wi=wi: e.tensor_scalar(out=ab[:, wi, :], in0=modb[:, wi, 1, :], scalar1=1.0,
                                                       scalar2=float(math.sqrt(D)), op0=ALU.add, op1=ALU.mult),
               reads=[modb], writes=[ab])
        P.emit("dve", lambda e, wi=wi: e.tensor_tensor(out=ab[:, wi, :], in0=ab[:, wi, :], in1=gb[:], op=ALU.mult),
               reads=[ab, gb], writes=[ab])
    P.emit("dve", lambda e: e.tensor_scalar(out=qks[:], in0=qkb[:], scalar1=float(math.sqrt(HD)), scalar2=None,
                                            op0=ALU.mult), reads=[qkb], writes=[qks])

    class V:
        pass
    a_lat = ab.t[:, 0, :]
    a_ctx = ab.t[:, 1, :]
    b_lat = modb.t[:, 0, 0, :]
    b_ctx = modb.t[:, 1, 0, :]

    for (t0, tn) in SUBCH:
        isctx = t0 >= TPC
        a, b = (a_ctx, b_ctx) if isctx else (a_lat, b_lat)
        xs = xs_rot.next()
        P.dma("sp", xs[:, :, 0:tn], xT[:, t0:t0 + tn].rearrange("(kc p) t -> p kc t", p=128), writes=[xs])
        sq = sq_rot.next()
        P.emit("act", lambda e, sq=sq, xs=xs, tn=tn: e.activation(out=sq[:, :, 0:tn], in_=xs[:, :, 0:tn], func=AF.Square),
               reads=[xs], writes=[sq])
        ps_ = pss.next()
        for kc in range(KC):
            P.emit("pe", lambda e, ps_=ps_, sq=sq, kc=kc, tn=tn: e.matmul(
                ps_[:, 0:tn], lhsT=ones[:], rhs=sq[:, kc, 0:tn], start=(kc == 0), stop=(kc == KC - 1)),
                reads=[sq, ones], writes=[ps_], inc=(kc == KC - 1))
        rs = rs_rot.next()
        P.emit("dve", lambda e, rs=rs, ps_=ps_, tn=tn: e.tensor_scalar(
            out=rs[:, 0, 0:tn], in0=ps_[:, 0:tn], scalar1=float(D * EPS), scalar2=None, op0=ALU.add),
            reads=[ps_], writes=[rs])
        P.emit("pool", lambda e, rs=rs, tn=tn: e.tensor_tensor(
            out=rs[:, 1, 0:tn], in0=rs[:, 0, 0:tn], in1=rs[:, 2, 0:tn], op=ALU.pow), reads=[rs], writes=[rs])
        for kc in range(KC):
            eng = "dve" if kc % 2 == 0 else "pool"
            P.emit(eng, lambda e, kc=kc, rs=rs, xs=xs, tn=tn: e.tensor_tensor(
                out=xs[:, kc, 0:tn], in0=xs[:, kc, 0:tn], in1=rs[:, 1, 0:tn], op=ALU.mult), reads=[xs, rs], writes=[xs])
            P.emit("act", lambda e, xs=xs, kc=kc, a=a, b=b, t0=t0, tn=tn: e.activation(
                out=hT[:, kc, t0:t0 + tn], in_=xs[:, kc, 0:tn], func=AF.Identity,
                bias=b[:, kc:kc + 1], scale=a[:, kc:kc + 1]), reads=[xs, ab, modb], writes=[hT])

    pending = []

    def flush(n_keep):
        while len(pending) > n_keep:
            st = pending.pop(0)
            st()

    def head_epilogue(ps_, j, t0, tn, is_q):
        gcol = 0 if is_q else 1
        st = {}

        def stageA():
            sq = sqh.next()
            st["sq"] = sq
            P.emit("act", lambda e: e.activation(out=sq[:, 0:tn], in_=ps_[:, 0:tn], func=AF.Square),
                   reads=[ps_], writes=[sq])

        def stageB():
            sq = st["sq"]
            p2 = pss.next()
            P.emit("pe", lambda e: e.matmul(p2[:, 0:tn], lhsT=ones[:], rhs=sq[:, 0:tn], start=True, stop=True),
                   reads=[sq, ones], writes=[p2])
            rs = rs_rot.next()
            P.emit("dve", lambda e: e.tensor_scalar(out=rs[:, 0, 0:tn], in0=p2[:, 0:tn], scalar1=float(HD * EPS),
                                                    scalar2=None, op0=ALU.add), reads=[p2], writes=[rs])
            P.emit("pool", lambda e: e.tensor_tensor(out=rs[:, 1, 0:tn], in0=rs[:, 0, 0:tn], in1=rs[:, 2, 0:tn],
                                                     op=ALU.pow), reads=[rs], writes=[rs])
            qn = qn_rot.next()
            st["qn"] = qn
            P.emit("dve", lambda e: e.scalar_tensor_tensor(
                out=qn[:, 0:tn], in0=ps_[:, 0:tn], scalar=qks[:, gcol:gcol + 1], in1=rs[:, 1, 0:tn],
                op0=ALU.mult, op1=ALU.mult), reads=[ps_, qks, rs], writes=[qn])

        def stageC():
            qn = st["qn"]
            p3 = psr.next()
            P.emit("pe", lambda e: e.matmul(p3[:, 0:tn], lhsT=rmb[:], rhs=qn[:, 0:tn], start=True, stop=True),
                   reads=[qn, rmb], writes=[p3])
            t1 = t1_rot.next()
            P.emit("pool", lambda e: e.tensor_tensor(out=t1[:, 0:tn], in0=qn[:, 0:tn], in1=cosb[:, t0:t0 + tn],
                                                     op=ALU.mult), reads=[qn, cosb], writes=[t1])
            t2 = t2_rot.next()
            P.emit("dve", lambda e: e.tensor_tensor(out=t2[:, 0:tn], in0=p3[:, 0:tn], in1=sinb[:, t0:t0 + tn],
                                                    op=ALU.mult), reads=[p3, sinb], writes=[t2])
            ob = ob_rot.next()
            P.emit("pool", lambda e: e.tensor_tensor(out=ob[:, 0:tn], in0=t1[:, 0:tn], in1=t2[:, 0:tn], op=ALU.add),
                   reads=[t1, t2], writes=[ob])
            dst = qT[j * 128:(j + 1) * 128, t0:t0 + tn] if is_q else kT[(j - 16) * 128:(j - 15) * 128, t0:t0 + tn]
            P.dma("sp", dst, ob[:, 0:tn], reads=[ob], is_output=True)

        stageA()
        pending.append(stageB)
        pending.append(stageC)

    for pn in range(16):
        wbuf = wrot.next()
        for h in range(2):
            P.dma("pool", wbuf[:, h * 8:(h + 1) * 8, :],
                  w[h * 1024:(h + 1) * 1024, pn * 512:(pn + 1) * 512].rearrange("(kc p) n -> p kc n", p=128),
                  writes=[wbuf], acc=(h > 0))
        if pn == 5:
            tiles = [(i * 128, 128) for i in range(16)] + [(2048, CPC)]
            for (t0, tn) in tiles:
                ps_ = psm.next()
                for kc in range(KC):
                    P.emit("pe", lambda e, ps_=ps_, kc=kc, t0=t0, tn=tn, wbuf=wbuf: e.matmul(
                        ps_[0:tn, :], lhsT=hT[:, kc, t0:t0 + tn], rhs=wbuf[:, kc, :], start=(kc == 0), stop=(kc == KC - 1)),
                        reads=[hT, wbuf], writes=[ps_], inc=(kc == KC - 1))
                ob = ob_rot.next()
                P.emit("act", lambda e, ob=ob, ps_=ps_, tn=tn: e.activation(out=ob[0:tn, :], in_=ps_[0:tn, :], func=AF.Copy),
                       reads=[ps_], writes=[ob])
                P.dma("sp", vo[t0:t0 + tn, :], ob[0:tn, :], reads=[ob], is_output=True)
                flush(1)
            continue
        for jj in range(4):
            j = pn * 4 + jj
            for (t0, tn) in CHUNKS:
                ps_ = psm.next()
                for kc in range(KC):
                    P.emit("pe", lambda e, ps_=ps_, kc=kc, t0=t0, tn=tn, wbuf=wbuf, jj=jj: e.matmul(
                        ps_[:, 0:tn], lhsT=wbuf[:, kc, jj * 128:(jj + 1) * 128], rhs=hT[:, kc, t0:t0 + tn],
                        start=(kc == 0), stop=(kc == KC - 1)),
                        reads=[hT, wbuf], writes=[ps_], inc=(kc == KC - 1))
                flush(1)
                if j < 20:
                    head_epilogue(ps_, j, t0, tn, j < 16)
                elif j < 32:
                    ob = ob_rot.next()
                    P.emit("act", lambda e, ob=ob, ps_=ps_, tn=tn: e.activation(out=ob[:, 0:tn], in_=ps_[:, 0:tn], func=AF.Copy),
                           reads=[ps_], writes=[ob])
                    P.dma("sp", fT[(j - 24) * 128:(j - 23) * 128, t0:t0 + tn], ob[:, 0:tn], reads=[ob], is_output=True)
                else:
                    g = j - 32
                    ob = ob_rot.next()
                    P.emit("act", lambda e, ob=ob, ps_=ps_, tn=tn, g=g: e.activation(
                        out=ob[:, 0:tn], in_=ps_[:, 0:tn], func=AF.Sigmoid, bias=bgb[:, g:g + 1]),
                        reads=[ps_, bgb], writes=[ob])
                    P.dma("sp", gT[g * 128:(g + 1) * 128, t0:t0 + tn], ob[:, 0:tn], reads=[ob], is_output=True)
    flush(0)
    P.finish()
    return nc


def rope_tables():
    t = np.arange(SEQ)
    row = (t // GRID_W).astype(np.float32)
    col = (t % GRID_W).astype(np.float32)
    inv = (10000.0 ** (-np.arange(32, dtype=np.float32) / 32)).astype(np.float32)
    ar = row[:, None] * inv
    ac = col[:, None] * inv
    ang = np.concatenate([ar, ar, ac, ac], axis=-1)
    return np.cos(ang).T.astype(np.float32), np.sin(ang).T.astype(np.float32)


def rot_matrix():
    R = np.zeros((128, 128), np.float32)
    for base in (0, 64):
        for i in range(32):
            R[base + 32 + i, base + i] = -1.0
            R[base + i, base + 32 + i] = 1.0
    return R.astype(NPBF)


def run_p1(nc, l, inp, xT_cores, mod):
    cosT, sinT = rope_tables()
    rm = rot_matrix()
    maps = []
    modv = np.stack([np.stack([mod[:, l, 0:16, wi], mod[:, l, 16:32, wi]], axis=1) for wi in range(2)], axis=1)
    modv = np.ascontiguousarray(modv.astype(np.float32))
    for i in range(NCORES):
        cs = np.concatenate([cosT[:, i * TPC:(i + 1) * TPC], np.ones((128, CPC), np.float32)], axis=1)
        sn = np.concatenate([sinT[:, i * TPC:(i + 1) * TPC], np.zeros((128, CPC), np.float32)], axis=1)
        maps.append({
            "xT": xT_cores[i], "w": inp["w_in"][l], "modv": modv, "gain": fm(inp["norm_attn_g"][l]),
            "bgate": fm(inp["b_gate"][l]),
            "qkg": np.ascontiguousarray(np.stack([inp["q_norm_g"][l], inp["k_norm_g"][l]], axis=1)),
            "cosT": np.ascontiguousarray(cs), "sinT": np.ascontiguousarray(sn), "rm": rm})
    return run(nc, maps)


NKEY = CTX + SEQ
NKC = NKEY // 128
ATTN_SCALE = HD ** -0.5


def build_p2(with_ctx):
    nc = new_nc()
    nq = SEQ + (CTX if with_ctx else 0)
    qT = din(nc, "qT", [2, 128, nq], BF16)
    kT = din(nc, "kT", [128, NKEY], BF16)
    v = din(nc, "v", [NKEY, 128], BF16)
    oT = dout(nc, "oT", [2, 128, nq], BF16)
    P = Prog(nc)
    kb = P.sbuf("kb", [128, NKEY], BF16)
    vb = P.sbuf("vb", [128, NKC, 128], BF16)
    ones = P.sbuf("ones", [128, 128], BF16)
    qrot = P.rot("qc", 3, [128, 512], BF16)
    prot = P.rot("pb", 4, [128, 512], BF16)
    rrot = P.rot("ri", 2, [128, 512], F32)
    orot = P.rot("ob", 2, [128, 512], BF16)
    psS = P.rot("psS", 4, [128, 512], F32, psum=True)
    psO = P.rot("psO", 2, [128, 512], F32, psum=True)
    psL = P.rot("psL", 2, [128, 512], F32, psum=True)
    P.emit("pool", lambda e: e.memset(ones[:], 1.0), writes=[ones])
    for h in range(4):
        c0, c1 = h * 4160, (h + 1) * 4160
        P.dma("sp", kb[:, c0:c1], kT[:, c0:c1], writes=[kb], acc=(h > 0))
    for h in range(5):
        c0, c1 = h * 26, (h + 1) * 26
        P.dma("sp", vb[:, c0:c1, :], v[c0 * 128:c1 * 128, :].rearrange("(c p) d -> p c d", p=128), writes=[vb], acc=(h > 0))
    qchunks = [(i * 512, 512, NKC) for i in range(SEQ // 512)]
    if with_ctx:
        qchunks.append((SEQ, CTX, CTX // 128))
    def do_chunk(hh, t0, tn, nk):
        qc = qrot.next()
        P.dma("sp", qc[:, 0:tn], qT[hh, :, t0:t0 + tn], writes=[qc])
        po = psO.next()
        pl = psL.next()
        sbufs = {}

        def S(kc):
            ps = psS.next()
            sbufs[kc] = ps
            P.emit("pe", lambda e: e.matmul(ps[:, 0:tn], lhsT=kb[:, kc * 128:(kc + 1) * 128],
                                            rhs=qc[:, 0:tn], start=True, stop=True),
                   reads=[kb, qc], writes=[ps])

        def step(kc):
            ps = sbufs.pop(kc)
            pb = prot.next()
            P.emit("act", lambda e: e.activation(out=pb[:, 0:tn], in_=ps[:, 0:tn], func=AF.Exp,
                                                 scale=float(ATTN_SCALE)), reads=[ps], writes=[pb])
            P.emit("pe", lambda e: e.matmul(po[:, 0:tn], lhsT=vb[:, kc, :], rhs=pb[:, 0:tn],
                                            start=(kc == 0), stop=(kc == nk - 1)),
                   reads=[vb, pb], writes=[po], inc=False)
            P.emit("pe", lambda e: e.matmul(pl[:, 0:tn], lhsT=ones[:], rhs=pb[:, 0:tn],
                                            start=(kc == 0), stop=(kc == nk - 1)),
                   reads=[ones, pb], writes=[pl], inc=(kc == nk - 1))
        S(0)
        for kc in range(nk):
            if kc + 1 < nk:
                S(kc + 1)
            step(kc)
        ri = rrot.next()
        P.emit("dve", lambda e: e.reciprocal(out=ri[:, 0:tn], in_=pl[:, 0:tn]), reads=[pl], writes=[ri])
        ob = orot.next()
        P.emit("dve", lambda e: e.tensor_tensor(out=ob[:, 0:tn], in0=po[:, 0:tn], in1=ri[:, 0:tn],
                                                op=ALU.mult), reads=[po, ri], writes=[ob])
        P.dma("sp", oT[hh, :, t0:t0 + tn], ob[:, 0:tn], reads=[ob], is_output=True)

    for hh in range(2):
        for (t0, tn, nk) in qchunks:
            do_chunk(hh, t0, tn, nk)
    P.finish()
    return nc


def run_p2(nc, with_ctx, qT_full, kT_full, v_full):
    maps = []
    for i in range(NCORES):
        kv = i // 2
        maps.append({"qT": np.ascontiguousarray(qT_full[i * 256:(i + 1) * 256].reshape(2, 128, -1)),
                     "kT": np.ascontiguousarray(kT_full[kv * 128:(kv + 1) * 128]),
                     "v": np.ascontiguousarray(v_full[:, kv * 128:(kv + 1) * 128])})
    res = run(nc, maps)
    return np.concatenate([r["oT"].reshape(256, -1) for r in res], axis=0)


def build_p3a():
    nc = new_nc()
    fT = din(nc, "fT", [256, SEQ], BF16)
    ccs_d = din(nc, "ccs", [128, 2, 256], BF16)
    YT = dout(nc, "YT", [2, 128, SEQ], BF16)
    P = Prog(nc)
    fb = P.sbuf("fb", [128, 2, SEQ], BF16)
    ccs = P.sbuf("ccs_s", [128, 2, 256], BF16)
    yb = P.sbuf("yb", [128, 2, SEQ], BF16)
    psr = P.rot("ps", 4, [128, 512], F32, psum=True)
    P.dma("sp", ccs[:], ccs_d, writes=[ccs])
    for cc in range(2):
        for h in range(4):
            P.dma("sp", fb[:, cc, h * 4096:(h + 1) * 4096], fT[cc * 128:(cc + 1) * 128, h * 4096:(h + 1) * 4096],
                  writes=[fb], acc=(cc + h > 0))
    n = 0
    for tc in range(SEQ // 512):
        for ri in range(2):
            ps = psr.next()
            for cc in range(2):
                P.emit("pe", lambda e, ps=ps, tc=tc, ri=ri, cc=cc: e.matmul(
                    ps[:], lhsT=ccs[:, cc, ri * 128:(ri + 1) * 128], rhs=fb[:, cc, tc * 512:(tc + 1) * 512],
                    start=(cc == 0), stop=(cc == 1)), reads=[ccs, fb], writes=[ps], inc=(cc == 1))
            if n % 2 == 0:
                P.emit("act", lambda e, ps=ps, tc=tc, ri=ri: e.activation(out=yb[:, ri, tc * 512:(tc + 1) * 512], in_=ps[:], func=AF.Copy),
                       reads=[ps], writes=[yb])
            else:
                P.emit("dve", lambda e, ps=ps, tc=tc, ri=ri: e.tensor_copy(out=yb[:, ri, tc * 512:(tc + 1) * 512], in_=ps[:]),
                       reads=[ps], writes=[yb])
            n += 1
    for ri in range(2):
        for h in range(4):
            P.dma("sp", YT[ri, :, h * 4096:(h + 1) * 4096], yb[:, ri, h * 4096:(h + 1) * 4096], reads=[yb], is_output=True)
    P.finish()
    return nc


def build_p3b(with_ctx):
    nc = new_nc()
    Yd = din(nc, "Y", [2, 128, SEQ], BF16)
    fc_d = din(nc, "fc", [256, CTX], BF16)
    ccs_d = din(nc, "ccs", [128, 2, 256], BF16)
    w1_d = din(nc, "w1", [128, 2, 256], BF16)
    tw_d = din(nc, "tw", [128, 2, 512])
    w2_d = din(nc, "w2", [128, 2, 128], BF16)
    w256_d = din(nc, "w256", [128, 2, 2, 256], BF16)
    Fo = dout(nc, "Fo", [128, SEQ], BF16)
    Fc = dout(nc, "Fc", [128, CTX], BF16)
    P = Prog(nc)
    fb = P.sbuf("fb", [128, 2, SEQ], BF16)
    yr = P.sbuf("yr", [128, SEQ], BF16)
    yi = P.sbuf("yi", [128, SEQ], BF16)
    ccs = P.sbuf("ccs_s", [128, 2, 256], BF16)
    w1 = P.sbuf("w1_s", [128, 2, 256], BF16)
    tw = P.sbuf("tw_s", [128, 2, 512], F32)
    w2 = P.sbuf("w2_s", [128, 2, 128], BF16)
    w256 = P.sbuf("w256_s", [128, 2, 2, 256], BF16)
    fcb = P.sbuf("fcb", [128, 2, CTX], BF16)
    ycb = P.sbuf("ycb", [128, 2, 256], BF16)
    ocb = P.sbuf("ocb", [128, CTX], BF16)
    arot = P.rot("A", 2, [128, 512], F32)
    brot = P.rot("B", 2, [128, 512], F32)
    psr = P.rot("ps", 4, [128, 512], F32, psum=True)
    for (dst, src) in ((ccs, ccs_d), (w1, w1_d), (tw, tw_d), (w2, w2_d), (w256, w256_d)):
        P.dma("sp", dst[:], src, writes=[dst])
    for h in range(4):
        P.dma("sp", yr[:, h * 4096:(h + 1) * 4096], Yd[0, :, h * 4096:(h + 1) * 4096], writes=[yr], acc=(h > 0))
    for h in range(4):
        P.dma("sp", yi[:, h * 4096:(h + 1) * 4096], Yd[1, :, h * 4096:(h + 1) * 4096], writes=[yi], acc=(h > 0))
    yrv = yr.t[:, :].rearrange("p (j n) -> p j n", n=128)
    yiv = yi.t[:, :].rearrange("p (j n) -> p j n", n=128)
    for jp in range(64):
        ps = psr.next()
        psv = ps.t[:, :].rearrange("p (a c) -> p a c", c=256)
        for q in range(2):
            j = jp * 2 + q
            P.emit("pe", lambda e, psv=psv, q=q, j=j: e.matmul(psv[:, q, :], lhsT=yrv[:, j, :], rhs=w1[:, 0, :],
                                                               start=True, stop=False), reads=[yr, w1], writes=[ps], inc=False)
            P.emit("pe", lambda e, psv=psv, q=q, j=j: e.matmul(psv[:, q, :], lhsT=yiv[:, j, :], rhs=w1[:, 1, :],
                                                               start=False, stop=True), reads=[yi, w1], writes=[ps], inc=(q == 1))
        A = arot.next()
        B = brot.next()
        P.emit("dve", lambda e, A=A, ps=ps: e.tensor_tensor(out=A[:], in0=ps[:], in1=tw[:, 0, :], op=ALU.mult),
               reads=[ps, tw], writes=[A])
        P.emit("dve", lambda e, B=B, ps=ps: e.tensor_tensor(out=B[:], in0=ps[:], in1=tw[:, 1, :], op=ALU.mult),
               reads=[ps, tw], writes=[B])
        for q in range(2):
            o0 = jp * 256 + q * 128
            P.emit("pool", lambda e, A=A, B=B, q=q, o0=o0: e.tensor_tensor(
                out=fb[:, 0, o0:o0 + 128], in0=A[:, q * 256:q * 256 + 128], in1=B[:, q * 256 + 128:q * 256 + 256],
                op=ALU.subtract), reads=[A, B], writes=[fb])
            P.emit("pool", lambda e, A=A, B=B, q=q, o0=o0: e.tensor_tensor(
                out=fb[:, 1, o0:o0 + 128], in0=B[:, q * 256:q * 256 + 128], in1=A[:, q * 256 + 128:q * 256 + 256],
                op=ALU.add), reads=[A, B], writes=[fb])
    for cq in range(32):
        ps = psr.next()
        P.emit("pe", lambda e, ps=ps, cq=cq: e.matmul(ps[:], lhsT=w2[:, 0, :], rhs=fb[:, 0, cq * 512:(cq + 1) * 512],
                                                      start=True, stop=False), reads=[fb, w2], writes=[ps], inc=False)
        P.emit("pe", lambda e, ps=ps, cq=cq: e.matmul(ps[:], lhsT=w2[:, 1, :], rhs=fb[:, 1, cq * 512:(cq + 1) * 512],
                                                      start=False, stop=True), reads=[fb, w2], writes=[ps])
        if cq % 2 == 0:
            P.emit("act", lambda e, ps=ps, cq=cq: e.activation(out=yr[:, cq * 512:(cq + 1) * 512], in_=ps[:], func=AF.Copy,
                                                               scale=float(1.0 / 2048.0)), reads=[ps], writes=[yr])
        else:
            P.emit("dve", lambda e, ps=ps, cq=cq: e.tensor_scalar(out=yr[:, cq * 512:(cq + 1) * 512], in0=ps[:],
                                                                  scalar1=float(1.0 / 2048.0), scalar2=None, op0=ALU.mult),
                   reads=[ps], writes=[yr])
    for h in range(4):
        P.dma("sp", Fo[:, h * 4096:(h + 1) * 4096], yr[:, h * 4096:(h + 1) * 4096], reads=[yr], is_output=True)
    if with_ctx:
        for cc in range(2):
            P.dma("sp", fcb[:, cc, :], fc_d[cc * 128:(cc + 1) * 128, :], writes=[fcb], acc=(cc > 0))
        for tt in range(2):
            ps = psr.next()
            for cc in range(2):
                P.emit("pe", lambda e, ps=ps, tt=tt, cc=cc: e.matmul(
                    ps[:, 0:256], lhsT=fcb[:, cc, tt * 128:(tt + 1) * 128], rhs=ccs[:, cc, :], start=(cc == 0), stop=(cc == 1)),
                    reads=[fcb, ccs], writes=[ps], inc=(cc == 1))
            P.emit("act", lambda e, ps=ps, tt=tt: e.activation(out=ycb[:, tt, :], in_=ps[:, 0:256], func=AF.Copy),
                   reads=[ps], writes=[ycb])
        ps = psr.next()
        n = 0
        for tt in range(2):
            for ri in range(2):
                P.emit("pe", lambda e, ps=ps, tt=tt, ri=ri, n=n: e.matmul(
                    ps[:, 0:256], lhsT=ycb[:, tt, ri * 128:(ri + 1) * 128], rhs=w256[:, tt, ri, :], start=(n == 0), stop=(n == 3)),
                    reads=[ycb, w256], writes=[ps], inc=(n == 3))
                n += 1
        P.emit("act", lambda e, ps=ps: e.activation(out=ocb[:], in_=ps[:, 0:256], func=AF.Copy, scale=float(1.0 / 256.0)),
               reads=[ps], writes=[ocb])
    else:
        P.emit("pool", lambda e: e.memset(ocb[:], 0.0), writes=[ocb])
    P.dma("sp", Fc, ocb[:], reads=[ocb], is_output=True)
    P.finish()
    return nc


def p3_consts(half):
    p = np.arange(128)
    out = {}
    ccs = np.zeros((128, 2, 256), np.float64)
    j = 128 * half + np.arange(128)
    for cc in range(2):
        c = cc * 128 + p
        ang = 2 * np.pi * np.outer(c, j) / 256.0
        ccs[:, cc, 0:128] = np.cos(ang)
        ccs[:, cc, 128:256] = np.sin(ang)
    out["ccs"] = ccs.astype(NPBF)
    a128 = 2 * np.pi * np.outer(p, p) / 128.0
    C, S = np.cos(a128), np.sin(a128)
    w1 = np.zeros((128, 2, 256))
    w1[:, 0, 0:128] = C
    w1[:, 0, 128:256] = S
    w1[:, 1, 0:128] = -S
    w1[:, 1, 128:256] = C
    out["w1"] = w1.astype(NPBF)
    psi = 2 * np.pi * np.outer(p, p) / float(SEQ)
    tw = np.zeros((128, 2, 512))
    tw[:, 0, :] = np.tile(np.cos(psi), (1, 4))
    tw[:, 1, :] = np.tile(np.sin(psi), (1, 4))
    out["tw"] = tw.astype(np.float32)
    w2 = np.zeros((128, 2, 128))
    w2[:, 0] = C
    w2[:, 1] = -S
    out["w2"] = w2.astype(NPBF)
    w256 = np.zeros((128, 2, 2, 256))
    for tt in range(2):
        n = tt * 128 + p
        ang = 2 * np.pi * np.outer(n, np.arange(256)) / 256.0
        w256[:, tt, 0] = np.cos(ang)
        w256[:, tt, 1] = -np.sin(ang)
    out["w256"] = w256.astype(NPBF)
    out["ident"] = np.eye(128).astype(NPBF)
    return out


def run_p3(nca, ncb, fT_full):
    consts = [p3_consts(h) for h in range(2)]
    maps = []
    for i in range(NCORES):
        g, half = i // 2, i % 2
        maps.append({"fT": np.ascontiguousarray(fT_full[g * 256:(g + 1) * 256, :SEQ]), "ccs": consts[half]["ccs"]})
    ra = run(nca, maps)
    maps = []
    for i in range(NCORES):
        g, half = i // 2, i % 2
        c = consts[half]
        YT = ra[i]["YT"]
        Y = np.ascontiguousarray(YT.reshape(2, 128, 128, 128).transpose(0, 2, 1, 3)).reshape(2, 128, SEQ)
        maps.append({"Y": Y, "fc": np.ascontiguousarray(fT_full[g * 256:(g + 1) * 256, SEQ:]), "ccs": c["ccs"], "w1": c["w1"],
                     "tw": c["tw"], "w2": c["w2"], "w256": c["w256"]})
    rb = run(ncb, maps)
    outs = []
    for i in range(NCORES):
        Fo = rb[i]["Fo"].reshape(128, 128, 128)
        lat = np.ascontiguousarray(Fo.transpose(1, 0, 2)).reshape(128, SEQ)
        outs.append(np.concatenate([lat, rb[i]["Fc"]], axis=1))
    return np.concatenate(outs, axis=0)


DBG = {}


def build_p4(moe, with_ctx, last):
    nc = new_nc()
    ntok = TT if with_ctx else TPC
    xT = din(nc, "xT", [D, ntok])
    at_d = din(nc, "attnT", [QW, ntok], BF16)
    ft_d = din(nc, "FT", [FD, ntok], BF16)
    gt_d = din(nc, "gT", [2 * D, ntok], BF16)
    wp = din(nc, "wp", [QW, D])
    wf = din(nc, "wf", [FD, D])
    wo = din(nc, "wo", [D, D])
    if moe:
        rw_d = din(nc, "rw", [128, KC, 8])
        mg = din(nc, "mg", [NE, D, DFE])
        mu = din(nc, "mu", [NE, D, DFE])
        md = din(nc, "md", [NE, DFE, D])
        id_d = din(nc, "ident", [128, 128], BF16)
    else:
        wg = din(nc, "wg", [D, DFF])
        wu = din(nc, "wu", [D, DFF])
        wd = din(nc, "wd", [DFF, D])
    modv = din(nc, "modv", [128, 2, 4, KC])
    gain2 = din(nc, "gain2", [128, KC])
    fgain = din(nc, "fgain", [128, KC])
    xo = dout(nc, "xo", [D, ntok])
    P = Prog(nc)
    xs = P.sbuf("xs", [128, KC, 512], F32)
    atb = P.sbuf("atb", [128, KC, 512], BF16)
    fg = P.sbuf("fg", [128, (40 if moe else 44) * 512], BF16)
    yT = P.sbuf("yT", [128, KC, 512], BF16)
    ones = P.sbuf("ones", [128, 128], BF16)
    modb = P.sbuf("modb", [128, 2, 4, KC], F32)
    g2b = P.sbuf("g2b", [128, KC], F32)
    fgb = P.sbuf("fgb", [128, KC], F32)
    a2b = P.sbuf("a2b", [128, 2, KC], F32)
    wrot = P.rot("wb", 3, [128, 8192], BF16)
    t1r = P.rot("t1", 2, [128, 512], F32)
    t2r = P.rot("t2", 2, [128, 512], F32)
    sr = P.rot("sl", 2, [128, 512], F32)
    rsr = P.rot("rs", 2, [128, 3, 512], F32)
    psm = P.rot("psm", 6, [128, 512], F32, psum=True)
    pss = P.rot("pss", 2, [128, 512], F32, psum=True)
    ftv = fg.t[:, 0:8 * 512].rearrange("p (k t) -> p k t", t=512)
    gtv = fg.t[:, 8 * 512:40 * 512].rearrange("p (k t) -> p k t", t=512)
    aTv = fg.t[:, :].rearrange("p (k t) -> p k t", t=512)
    for (dst, src) in ((modb, modv), (g2b, gain2), (fgb, fgain)):
        P.dma("sp", dst[:], src, writes=[dst])
    P.emit("pool", lambda e: e.memset(ones[:], 1.0), writes=[ones])
    for r in rsr.bufs:
        P.emit("pool", lambda e, r=r: e.memset(r[:, 2, :], -0.5), writes=[r])
    for wi in range(2):
        P.emit("dve", lambda e, wi=wi: e.tensor_scalar(out=a2b[:, wi, :], in0=modb[:, wi, 2, :], scalar1=1.0,
                                                       scalar2=float(math.sqrt(D)), op0=ALU.add, op1=ALU.mult),
               reads=[modb], writes=[a2b])
        P.emit("dve", lambda e, wi=wi: e.tensor_tensor(out=a2b[:, wi, :], in0=a2b[:, wi, :], in1=g2b[:], op=ALU.mult),
               reads=[a2b, g2b], writes=[a2b])
    P.emit("dve", lambda e: e.tensor_scalar(out=fgb[:], in0=fgb[:], scalar1=float(math.sqrt(D)), scalar2=None, op0=ALU.mult),
           reads=[fgb], writes=[fgb])
    if moe:
        rwf = P.sbuf("rwf", [128, KC, 8], F32)
        rwb = P.sbuf("rwb", [128, KC, 16], BF16)
        ident = P.sbuf("ident_s", [128, 128], BF16)
        bcs = P.sbuf("bcs", [128, NE, 512], F32)
        l16 = P.sbuf("l16", [128, 16], F32)
        sm = P.sbuf("sm", [128, 8, 8], F32)
        sc1 = P.sbuf("sc1", [128, 8], F32)
        rep = P.rot("rep", 2, [128, 2, NE, 128], BF16)
        u2r = P.rot("u2", 2, [128, 512], F32)
        P.dma("sp", rwf[:], rw_d, writes=[rwf])
        P.dma("sp", ident[:], id_d, writes=[ident])
        P.emit("act", lambda e: e.activation(out=rwb[:, :, 0:8], in_=rwf[:], func=AF.Copy), reads=[rwf], writes=[rwb])
        P.emit("dve", lambda e: e.tensor_tensor(out=rwb[:, :, 8:16], in0=rwf[:], in1=rwb[:, :, 0:8], op=ALU.subtract),
               reads=[rwf, rwb], writes=[rwb])

    def load_w(src2d, k0, nkc, n0, ncols):
        wb = wrot.next()
        view = wb.t[:, 0:nkc * ncols].rearrange("p (k n) -> p k n", n=ncols)
        first = True
        for h0 in range(0, nkc, 8):
            h1 = min(nkc, h0 + 8)
            P.dma("pool", view[:, h0:h1, :],
                  src2d[k0 + h0 * 128:k0 + h1 * 128, n0:n0 + ncols].rearrange("(kc p) n -> p kc n", p=128),
                  writes=[wb], acc=(not first))
            first = False
        return wb, view

    def mm_group(ps, tn, parts, inc_last=True):
        n = len(parts)
        for i, (wbuf, lhsT, rbuf, rhs) in enumerate(parts):
            P.emit("pe", lambda e, lhsT=lhsT, rhs=rhs, i=i: e.matmul(ps[:, 0:tn], lhsT=lhsT, rhs=rhs, start=(i == 0), stop=(i == n - 1)),
                   reads=[wbuf, rbuf], writes=[ps], inc=(inc_last and i == n - 1))

    def resid(ps, j, tn, gcol, wi):
        P.emit("dve", lambda e: e.scalar_tensor_tensor(out=xs[:, j, 0:tn], in0=ps[:, 0:tn], scalar=modb[:, wi, gcol, j:j + 1],
                                                       in1=xs[:, j, 0:tn], op0=ALU.mult, op1=ALU.add),
               reads=[ps, modb, xs], writes=[xs])

    def rstd_of_xs(tn):
        P.emit("act", lambda e: e.activation(out=yT[:, :, 0:tn], in_=xs[:, :, 0:tn], func=AF.Square), reads=[xs], writes=[yT])
        ps_ = pss.next()
        mm_group(ps_, tn, [(ones, ones[:], yT, yT[:, kc, 0:tn]) for kc in range(KC)])
        rs = rsr.next()
        P.emit("dve", lambda e: e.tensor_scalar(out=rs[:, 0, 0:tn], in0=ps_[:, 0:tn], scalar1=float(D * EPS), scalar2=None,
                                                op0=ALU.add), reads=[ps_], writes=[rs])
        P.emit("pool", lambda e: e.tensor_tensor(out=rs[:, 1, 0:tn], in0=rs[:, 0, 0:tn], in1=rs[:, 2, 0:tn], op=ALU.pow),
               reads=[rs], writes=[rs])
        return rs

    def ffn_gu(c, tn, wgb, wgv, wub, wuv, jj, bc_e=None):
        psG = psm.next()
        mm_group(psG, tn, [(wgb, wgv[:, kc, jj * 128:(jj + 1) * 128], atb, atb[:, kc, 0:tn]) for kc in range(KC)])
        psU = psm.next()
        mm_group(psU, tn, [(wub, wuv[:, kc, jj * 128:(jj + 1) * 128], atb, atb[:, kc, 0:tn]) for kc in range(KC)])
        s_ = sr.next()
        P.emit("act", lambda e: e.activation(out=s_[:, 0:tn], in_=psG[:, 0:tn], func=AF.Silu), reads=[psG], writes=[s_])
        if bc_e is None:
            P.emit("dve", lambda e: e.tensor_tensor(out=aTv[:, c, 0:tn], in0=s_[:, 0:tn], in1=psU[:, 0:tn], op=ALU.mult),
                   reads=[s_, psU], writes=[fg])
        else:
            u2 = u2r.next()
            P.emit("dve", lambda e: e.tensor_tensor(out=u2[:, 0:tn], in0=psU[:, 0:tn], in1=bcs[:, bc_e, 0:tn], op=ALU.mult),
                   reads=[psU, bcs], writes=[u2])
            P.emit("pool", lambda e: e.tensor_tensor(out=aTv[:, c, 0:tn], in0=s_[:, 0:tn], in1=u2[:, 0:tn], op=ALU.mult),
                   reads=[s_, u2], writes=[fg])

    def router_tile(tt, rstage=9):
        psR = pss.next()
        parts = [(atb, atb[:, kc, tt * 128:(tt + 1) * 128], rwb, rwb[:, kc, 0:16]) for kc in range(KC)]
        n = len(parts)
        for i, (wbuf, lhsT, rbuf, rhs) in enumerate(parts):
            P.emit("pe", lambda e, lhsT=lhsT, rhs=rhs, i=i: e.matmul(psR[:, 0:16], lhsT=lhsT, rhs=rhs, start=(i == 0), stop=False),
                   reads=[wbuf, rbuf], writes=[psR], inc=False)
        for kc in range(KC):
            P.emit("pe", lambda e, kc=kc: e.matmul(psR[:, 0:8], lhsT=yT[:, kc, tt * 128:(tt + 1) * 128], rhs=rwb[:, kc, 0:8],
                                                   start=False, stop=(kc == KC - 1)),
                   reads=[yT, rwb], writes=[psR], inc=(kc == KC - 1))
        P.emit("dve", lambda e: e.tensor_copy(out=l16[:], in_=psR[:, 0:16]), reads=[psR], writes=[l16])
        if rstage < 1:
            if tt == 0:
                P.emit("pool", lambda e: e.memset(bcs[:], 0.5), writes=[bcs])
            return
        lg, m1, mk1, l2, m2, mk2, dd = (sm[:, 0, :], sm[:, 1, 0:1], sm[:, 2, :], sm[:, 3, :], sm[:, 1, 1:2], sm[:, 4, :], sm[:, 1, 2:3])
        ee, den, w1, w2 = sm[:, 1, 3:4], sm[:, 1, 4:5], sm[:, 1, 5:6], sm[:, 1, 6:7]
        comb = sm[:, 5, :]
        D_ = lambda fn: P.emit("dve", fn, reads=[sm, l16], writes=[sm])
        D_(lambda e: e.tensor_tensor(out=lg, in0=l16[:, 0:8], in1=l16[:, 8:16], op=ALU.add))
        D_(lambda e: e.reduce_max(out=m1, in_=lg, axis=AX.X))
        D_(lambda e: e.tensor_scalar(out=mk1, in0=lg, scalar1=m1, scalar2=None, op0=ALU.is_equal))
        D_(lambda e: e.scalar_tensor_tensor(out=l2, in0=mk1, scalar=-1e30, in1=lg, op0=ALU.mult, op1=ALU.add))
        D_(lambda e: e.reduce_max(out=m2, in_=l2, axis=AX.X))
        D_(lambda e: e.tensor_scalar(out=mk2, in0=l2, scalar1=m2, scalar2=None, op0=ALU.is_equal))
        D_(lambda e: e.tensor_tensor(out=dd, in0=m2, in1=m1, op=ALU.subtract))
        P.emit("act", lambda e: e.activation(out=ee, in_=dd, func=AF.Exp), reads=[sm], writes=[sm])
        D_(lambda e: e.tensor_scalar(out=den, in0=ee, scalar1=1.0, scalar2=None, op0=ALU.add))
        D_(lambda e: e.reciprocal(out=w1, in_=den))
        D_(lambda e: e.tensor_tensor(out=w2, in0=ee, in1=w1, op=ALU.mult))
        D_(lambda e: e.tensor_scalar(out=comb, in0=mk1, scalar1=w1, scalar2=None, op0=ALU.mult))
        D_(lambda e: e.scalar_tensor_tensor(out=comb, in0=mk2, scalar=w2, in1=comb, op0=ALU.mult, op1=ALU.add))
        if rstage < 2:
            if tt == 0:
                P.emit("pool", lambda e: e.memset(bcs[:], 0.5), writes=[bcs])
            return
        rp = rep.next()
        cb = comb.unsqueeze(2).to_broadcast([128, NE, 128])
        P.emit("dve", lambda e: e.tensor_copy(out=rp[:, 0], in_=cb), reads=[sm], writes=[rp])
        P.emit("dve", lambda e: e.tensor_tensor(out=rp[:, 1], in0=cb, in1=rp[:, 0], op=ALU.subtract), reads=[sm, rp], writes=[rp])
        if rstage < 3:
            if tt == 0:
                P.emit("pool", lambda e: e.memset(bcs[:], 0.5), writes=[bcs])
            return
        for e_ in range(NE):
            def bc_one(e_=e_):
                pb_ = psm.next()
                P.emit("pe", lambda e: e.matmul(pb_[:, 0:128], lhsT=rp[:, 0, e_, :], rhs=ident[:], start=True, stop=False),
                       reads=[rp, ident], writes=[pb_], inc=False)
                P.emit("pe", lambda e: e.matmul(pb_[:, 0:128], lhsT=rp[:, 1, e_, :], rhs=ident[:], start=False, stop=True),
                       reads=[rp, ident], writes=[pb_])
                if e_ % 2 == 0:
                    P.emit("act", lambda e: e.activation(out=bcs[:, e_, tt * 128:(tt + 1) * 128], in_=pb_[:, 0:128], func=AF.Copy),
                           reads=[pb_], writes=[bcs])
                else:
                    P.emit("dve", lambda e: e.tensor_copy(out=bcs[:, e_, tt * 128:(tt + 1) * 128], in_=pb_[:, 0:128]),
                           reads=[pb_], writes=[bcs])
            bc_one()

    def do_chunk(t0, tn, wi):
        P.dma("sp", xs[:, :, 0:tn], xT[:, t0:t0 + tn].rearrange("(kc p) t -> p kc t", p=128), writes=[xs])
        for h in range(2):
            P.dma("sp", atb[:, h * 8:(h + 1) * 8, 0:tn], at_d[h * 1024:(h + 1) * 1024, t0:t0 + tn].rearrange("(kc p) t -> p kc t", p=128),
                  writes=[atb], acc=(h > 0))
        P.dma("sp", ftv[:, :, 0:tn], ft_d[:, t0:t0 + tn].rearrange("(kc p) t -> p kc t", p=128), writes=[fg])
        for h in range(4):
            P.dma("sp", gtv[:, h * 8:(h + 1) * 8, 0:tn], gt_d[h * 1024:(h + 1) * 1024, t0:t0 + tn].rearrange("(kc p) t -> p kc t", p=128),
                  writes=[fg], acc=True)
        for pn in range(4):
            wpb, wpv = load_w(wp, 0, KC, pn * 512, 512)
            wfb, wfv = load_w(wf, 0, 8, pn * 512, 512)
            for jj in range(4):
                j = pn * 4 + jj

                def ya(j=j, jj=jj, wpb=wpb, wpv=wpv, wfb=wfb, wfv=wfv):
                    psA = psm.next()
                    mm_group(psA, tn, [(wpb, wpv[:, kc, jj * 128:(jj + 1) * 128], atb, atb[:, kc, 0:tn]) for kc in range(KC)])
                    psB = psm.next()
                    mm_group(psB, tn, [(wfb, wfv[:, kc, jj * 128:(jj + 1) * 128], fg, ftv[:, kc, 0:tn]) for kc in range(8)])
                    t1 = t1r.next()
                    t2 = t2r.next()
                    P.emit("dve", lambda e: e.tensor_tensor(out=t1[:, 0:tn], in0=psA[:, 0:tn], in1=gtv[:, j, 0:tn], op=ALU.mult),
                           reads=[psA, fg], writes=[t1])
                    P.emit("dve", lambda e: e.tensor_tensor(out=t2[:, 0:tn], in0=psB[:, 0:tn], in1=gtv[:, 16 + j, 0:tn], op=ALU.mult),
                           reads=[psB, fg], writes=[t2])
                    P.emit("pool", lambda e: e.tensor_tensor(out=yT[:, j, 0:tn], in0=t1[:, 0:tn], in1=t2[:, 0:tn], op=ALU.add),
                           reads=[t1, t2], writes=[yT])
                ya()
        for pn in range(4):
            wob, wov = load_w(wo, 0, KC, pn * 512, 512)
            for jj in range(4):
                j = pn * 4 + jj
                psZ = psm.next()
                mm_group(psZ, tn, [(wob, wov[:, kc, jj * 128:(jj + 1) * 128], yT, yT[:, kc, 0:tn]) for kc in range(KC)])
                resid(psZ, j, tn, 0, wi)
        rs = rstd_of_xs(tn)
        for kc in range(KC):
            def hk(kc=kc):
                t1 = t1r.next()
                P.emit("dve" if kc % 2 == 0 else "pool", lambda e: e.tensor_tensor(out=t1[:, 0:tn], in0=xs[:, kc, 0:tn], in1=rs[:, 1, 0:tn],
                                                                                   op=ALU.mult), reads=[xs, rs], writes=[t1])
                if not moe:
                    P.emit("act", lambda e: e.activation(out=atb[:, kc, 0:tn], in_=t1[:, 0:tn], func=AF.Identity,
                                                         bias=modb[:, wi, 1, kc:kc + 1], scale=a2b[:, wi, kc:kc + 1]),
                           reads=[t1, modb, a2b], writes=[atb])
                else:
                    t2 = t2r.next()
                    P.emit("act", lambda e: e.activation(out=t2[:, 0:tn], in_=t1[:, 0:tn], func=AF.Identity,
                                                         bias=modb[:, wi, 1, kc:kc + 1], scale=a2b[:, wi, kc:kc + 1]),
                           reads=[t1, modb, a2b], writes=[t2])
                    P.emit("dve", lambda e: e.tensor_copy(out=atb[:, kc, 0:tn], in_=t2[:, 0:tn]), reads=[t2], writes=[atb])
                    P.emit("pool", lambda e: e.tensor_tensor(out=yT[:, kc, 0:tn], in0=t2[:, 0:tn], in1=atb[:, kc, 0:tn], op=ALU.subtract),
                           reads=[t2, atb], writes=[yT])
            hk()
        if not moe:
            for pn in range(DFF // 512):
                wgb, wgv = load_w(wg, 0, KC, pn * 512, 512)
                wub, wuv = load_w(wu, 0, KC, pn * 512, 512)
                for jj in range(4):
                    ffn_gu(pn * 4 + jj, tn, wgb, wgv, wub, wuv, jj)
            for np_ in range(8):
                wab, wav = load_w(wd, 0, 22, np_ * 256, 256)
                wbb, wbv = load_w(wd, 22 * 128, 22, np_ * 256, 256)
                for jj in range(2):
                    j = np_ * 2 + jj
                    psD = psm.next()
                    parts = [(wab, wav[:, c, jj * 128:(jj + 1) * 128], fg, aTv[:, c, 0:tn]) for c in range(22)]
                    parts += [(wbb, wbv[:, c, jj * 128:(jj + 1) * 128], fg, aTv[:, 22 + c, 0:tn]) for c in range(22)]
                    mm_group(psD, tn, parts)
                    resid(psD, j, tn, 3, wi)
        else:
            if DBG.get("norouter"):
                P.emit("pool", lambda e: e.memset(bcs[:], 0.5), writes=[bcs])
            else:
                for tt in range(tn // 128):
                    router_tile(tt, DBG.get("rstage", 9))
            for e_ in range(DBG.get("nexp", NE)):
                for pn in range(6):
                    ncols = 512 if pn < 5 else 256
                    wgb, wgv = load_w(mg[e_], 0, KC, pn * 512, ncols)
                    wub, wuv = load_w(mu[e_], 0, KC, pn * 512, ncols)
                    for jj in range(ncols // 128):
                        ffn_gu(pn * 4 + jj, tn, wgb, wgv, wub, wuv, jj, bc_e=e_)
                for np_ in range(8):
                    wab, wav = load_w(md[e_], 0, 22, np_ * 256, 256)
                    for jj in range(2):
                        j = np_ * 2 + jj
                        psD = psm.next()
                        mm_group(psD, tn, [(wab, wav[:, c, jj * 128:(jj + 1) * 128], fg, aTv[:, c, 0:tn]) for c in range(22)])
                        resid(psD, j, tn, 3, wi)
        if last:
            rs2 = rstd_of_xs(tn)
            for kc in range(KC):
                P.emit("dve", lambda e, kc=kc: e.scalar_tensor_tensor(out=xs[:, kc, 0:tn], in0=xs[:, kc, 0:tn], scalar=fgb[:, kc:kc + 1],
                                                                      in1=rs2[:, 1, 0:tn], op0=ALU.mult, op1=ALU.mult),
                       reads=[xs, fgb, rs2], writes=[xs])
        P.dma("sp", xo[:, t0:t0 + tn].rearrange("(kc p) t -> p kc t", p=128), xs[:, :, 0:tn], reads=[xs], is_output=True)

    for (t0, tn) in CHUNKS[:DBG.get("nchunk", 9)]:
        if t0 >= TPC and not with_ctx:
            continue
        do_chunk(t0, tn, 1 if t0 >= TPC else 0)
    P.finish()
    return nc


def run_p4(nc, l, moe, with_ctx, inp, xT_cores, attnT_cores, FT_cores, gT_cores, mod):
    modv = np.stack([np.stack([mod[:, l, 32:48, wi], mod[:, l, 48:64, wi], mod[:, l, 64:80, wi], mod[:, l, 80:96, wi]], axis=1)
                     for wi in range(2)], axis=1)
    modv = np.ascontiguousarray(modv.astype(np.float32))
    maps = []
    for i in range(NCORES):
        m = {"xT": xT_cores[i], "attnT": attnT_cores[i], "FT": FT_cores[i], "gT": gT_cores[i],
             "wp": inp["w_attn_proj"][l], "wf": inp["w_four_proj"][l], "wo": inp["w_out"][l],
             "modv": modv, "gain2": fm(inp["norm_ffn_g"][l]), "fgain": fm(inp["final_norm_g"])}
        if moe:
            li = l // 2
            m["rw"] = np.ascontiguousarray(inp["router_w"][li].reshape(KC, 128, NE).transpose(1, 0, 2))
            m["mg"] = inp["moe_w_gate"][li]
            m["mu"] = inp["moe_w_up"][li]
            m["md"] = inp["moe_w_down"][li]
            m["ident"] = np.eye(128).astype(NPBF)
        else:
            li = l // 2
            m["wg"] = inp["ffn_w_gate"][li]
            m["wu"] = inp["ffn_w_up"][li]
            m["wd"] = inp["ffn_w_down"][li]
        maps.append(m)
    res = run(nc, maps)
    return [r["xo"] for r in res]


def kernel(**inp):
    inp = {k: np.asarray(v) for k, v in inp.items()}
    mod = run_p0(inp)
    x = inp["x"][0]
    ctx = inp["ctx"][0]
    xT_cores = [np.ascontiguousarray(np.concatenate([x[i * TPC:(i + 1) * TPC].T, ctx[i * CPC:(i + 1) * CPC].T], axis=1))
                for i in range(NCORES)]
    out = None
    for l in range(2):
        with_ctx = (l == 0)
        moe = (l % 2 == 1)
        last = (l == 1)
        r1 = run_p1(build_p1(), l, inp, xT_cores, mod)

        def gather(name, axis_tok):
            lat = np.concatenate([np.take(r[name], range(0, TPC), axis=axis_tok) for r in r1], axis=axis_tok)
            cx = np.concatenate([np.take(r[name], range(TPC, TT), axis=axis_tok) for r in r1], axis=axis_tok)
            return lat, cx
        q_lat, q_ctx = gather("qT", 1)
        k_lat, k_ctx = gather("kT", 1)
        v_lat, v_ctx = gather("v", 0)
        f_lat, f_ctx = gather("fT", 1)
        q_full = np.concatenate([q_lat, q_ctx], axis=1) if with_ctx else q_lat
        kT_full = np.concatenate([k_ctx, k_lat], axis=1)
        v_full = np.concatenate([v_ctx, v_lat], axis=0)
        oT = run_p2(build_p2(with_ctx), with_ctx, q_full, kT_full, v_full)
        fT_full = np.concatenate([f_lat, f_ctx], axis=1)
        FT = run_p3(build_p3a(), build_p3b(with_ctx), fT_full)

        def percore(a):
            res = []
            for i in range(NCORES):
                if with_ctx:
                    res.append(np.ascontiguousarray(np.concatenate(
                        [a[:, i * TPC:(i + 1) * TPC], a[:, SEQ + i * CPC:SEQ + (i + 1) * CPC]], axis=1)))
                else:
                    res.append(np.ascontiguousarray(a[:, i * TPC:(i + 1) * TPC]))
            return res
        attn_c = percore(oT)
        FT_c = percore(FT)
        ntok = TT if with_ctx else TPC
        g_c = [np.ascontiguousarray(r["gT"][:, :ntok]) for r in r1]
        x_c = [np.ascontiguousarray(xc[:, :ntok]) for xc in xT_cores]
        xo = run_p4(build_p4(moe, with_ctx, last), l, moe, with_ctx, inp, x_c, attn_c, FT_c, g_c, mod)
        if last:
            out = np.concatenate([xo[i][:, :TPC].T for i in range(NCORES)], axis=0)
        else:
            xT_cores = xo
    return np.ascontiguousarray(out.reshape(1, SEQ, D).astype(np.float32))
```

```python
import math
import numpy as np
import ml_dtypes
import concourse.bass as bass
import concourse.mybir as mybir
from concourse.bass_utils import run_bass_kernel_spmd

F32 = mybir.dt.float32
BF16 = mybir.dt.bfloat16
ALU = mybir.AluOpType
AF = mybir.ActivationFunctionType
AX = mybir.AxisListType
NPBF = ml_dtypes.bfloat16

NCORES = 8
D = 2048
KC = 16
SEQ = 16384
CTX = 256
TPC = SEQ // NCORES
CPC = CTX // NCORES
TT = TPC + CPC
NH, NKV, HD = 16, 4, 128
QW, KVW, FD = 2048, 512, 1024
INC = 8192
DFF = 5632
NE = 8
DFE = 2816
EPS = 1e-6
GRID_W = 64
ENGS = ("pe", "act", "dve", "pool", "sp")


class Buf:
    __slots__ = ("t", "last_w", "readers", "name", "ws")

    def __init__(self, t, name=""):
        self.t = t
        self.last_w = None
        self.ws = []
        self.readers = {}
        self.name = name

    def __getitem__(self, idx):
        return self.t[idx]


class Rot:
    def __init__(self, bufs):
        self.bufs = bufs
        self.i = 0

    def next(self):
        b = self.bufs[self.i % len(self.bufs)]
        self.i += 1
        return b


class Prog:
    def __init__(self, nc, same_engine_sync=True, n_dma_sems=8):
        self.nc = nc
        self.streams = {e: [] for e in ENGS}
        self.cnt = {e: 0 for e in ENGS}
        self.waited = {e: {} for e in ENGS}
        self.same_engine_sync = same_engine_sync
        self.sems = {}
        self.ctx = []
        for e in ENGS:
            self.sems[("eng", e)] = self._sem("s_" + e)
        self.dma_rot = {}
        self.dma_val = {}
        for q in ("sp", "act", "pool"):
            lst = []
            for i in range(n_dma_sems):
                k = ("dma", q + str(i))
                self.sems[k] = self._sem("d_%s%d" % (q, i))
                self.dma_val[k] = 0
                lst.append(k)
            self.dma_rot[q] = [lst, 0]
        self.out_tokens = []

    def _sem(self, name):
        cm = self.nc.semaphore(name)
        s = cm.__enter__()
        self.ctx.append(cm)
        return s

    def sbuf(self, name, shape, dtype):
        cm = self.nc.sbuf_tensor(name, shape, dtype)
        t = cm.__enter__()
        self.ctx.append(cm)
        return Buf(t, name)

    def psum(self, name, shape, dtype=F32):
        cm = self.nc.psum_tensor(name, shape, dtype)
        t = cm.__enter__()
        self.ctx.append(cm)
        return Buf(t, name)

    def rot(self, name, n, shape, dtype, psum=False):
        return Rot([(self.psum if psum else self.sbuf)("%s%d" % (name, i), shape, dtype) for i in range(n)])

    def _collect(self, e, reads, writes, acc=False):
        waits = {}

        def need(tok):
            if tok is None:
                return
            k, v = tok
            if k == ("eng", e):
                if e == "pe" or not self.same_engine_sync:
                    return
            if waits.get(k, 0) < v:
                waits[k] = v

        for b in reads:
            need(b.last_w)
            for t_ in b.ws:
                need(t_)
        for b in writes:
            need(b.last_w)
            for t_ in b.ws:
                if acc and t_[0][0] == "dma":
                    continue
                need(t_)
            for k, v in b.readers.items():
                if k == ("eng", e):
                    continue
                need((k, v))
        wl = []
        for k, v in waits.items():
            if self.waited[e].get(k, 0) >= v:
                continue
            self.waited[e][k] = v
            wl.append((k, v))
        return wl

    def _mark(self, tok, reads, writes, acc=False):
        k, v = tok
        for b in writes:
            if acc:
                b.ws.append(tok)
                continue
            b.last_w = tok
            b.ws = []
            b.readers = {}
        for b in reads:
            if b.readers.get(k, 0) < v:
                b.readers[k] = v

    def emit(self, e, fn, reads=(), writes=(), inc=True):
        wl = self._collect(e, reads, writes)
        if inc:
            self.cnt[e] += 1
            tok = (("eng", e), self.cnt[e])
        else:
            tok = (("eng", e), self.cnt[e] + 1)
        self.streams[e].append((wl, fn, ("eng", e) if inc else None, 1))
        self._mark(tok, reads, writes)
        return tok

    def dma(self, q, out, in_, reads=(), writes=(), is_output=False, acc=False):
        lst, i = self.dma_rot[q]
        k = lst[i % len(lst)]
        self.dma_rot[q][1] = i + 1
        wl = self._collect(q, reads, writes, acc)
        prev = self.dma_val[k]
        if prev > 0 and self.waited[q].get(k, 0) < prev:
            self.waited[q][k] = prev
            wl.append((k, prev))
        self.dma_val[k] = prev + 16
        tok = (k, prev + 16)
        self.streams[q].append((wl, lambda e: e.dma_start(out=out, in_=in_), k, 16))
        self._mark(tok, reads, writes, acc)
        if is_output:
            self.out_tokens.append(tok)
        return tok

    def finish(self):
        fin = {}
        for k, v in self.out_tokens:
            fin[k] = max(fin.get(k, 0), v)
        nc = self.nc
        sems = self.sems
        streams = self.streams
        emap = {"pe": "tensor", "act": "scalar", "dve": "vector", "pool": "gpsimd", "sp": "sync"}
        with nc.Block() as block:
            for e in ENGS:
                def body(eng, e=e):
                    for wl, fn, inc_k, inc_v in streams[e]:
                        for k, v in wl:
                            eng.wait_ge(sems[k], v)
                        ins = fn(eng)
                        if inc_k is not None:
                            ins.then_inc(sems[inc_k], inc_v)
                    if e == "sp":
                        for k, v in fin.items():
                            eng.wait_ge(sems[k], v)
                getattr(block, emap[e])(body)
        for cm in reversed(self.ctx):
            cm.__exit__(None, None, None)
        self.ctx = []


def new_nc():
    return bass.Bass("TRN2", target_bir_lowering=False)


def din(nc, name, shape, dt=F32):
    return nc.dram_tensor(name, list(shape), dt, kind="ExternalInput").ap()


def dout(nc, name, shape, dt=F32):
    return nc.dram_tensor(name, list(shape), dt, kind="ExternalOutput").ap()


def run(nc, in_maps):
    res = run_bass_kernel_spmd(nc, in_maps, core_ids=list(range(NCORES)))
    return res.results


def fm(v):
    v = np.asarray(v)
    return np.ascontiguousarray(v.reshape(-1, 128).T)


NMC = 12


def build_p0():
    nc = new_nc()
    cond = din(nc, "cond", [128, KC, 2])
    adaw = din(nc, "adaw", [2, D, NMC * 128])
    adab = din(nc, "adab", [128, 2, NMC])
    o = dout(nc, "mod", [128, 2, NMC, 2])
    P = Prog(nc)
    cs = P.sbuf("cs", [128, KC, 2], F32)
    cb = P.sbuf("cb", [128, KC, 2], BF16)
    bs = P.sbuf("bs", [128, 2, NMC], F32)
    ob = P.sbuf("ob", [128, 2, NMC, 2], F32)
    wb = [P.sbuf("w%d" % l, [128, KC, NMC * 128], BF16) for l in range(2)]
    ps = P.psum("ps", [128, 2, NMC, 2], F32)
    P.dma("sp", cs[:], cond, writes=[cs])
    P.dma("sp", bs[:], adab, writes=[bs])
    for l in range(2):
        for h in range(2):
            P.dma("pool", wb[l][:, h * 8:(h + 1) * 8, :],
                  adaw[l, h * 1024:(h + 1) * 1024, :].rearrange("(kc p) n -> p kc n", p=128), writes=[wb[l]], acc=(h > 0))
    P.emit("act", lambda e: e.activation(out=cb[:], in_=cs[:], func=AF.Silu), reads=[cs], writes=[cb])
    for l in range(2):
        for j in range(NMC):
            for kc in range(KC):
                P.emit("pe", lambda e, l=l, j=j, kc=kc: e.matmul(
                    ps[:, l, j, :], lhsT=wb[l][:, kc, j * 128:(j + 1) * 128], rhs=cb[:, kc, :],
                    start=(kc == 0), stop=(kc == KC - 1)), reads=[wb[l], cb], writes=[ps],
                    inc=(kc == KC - 1 and j == NMC - 1))
        P.emit("dve", lambda e, l=l: e.tensor_tensor(
            out=ob[:, l], in0=ps[:, l], in1=bs[:, l].unsqueeze(2).to_broadcast([128, NMC, 2]), op=ALU.add),
            reads=[ps, bs], writes=[ob])
    P.dma("sp", o, ob[:], reads=[ob], is_output=True)
    P.finish()
    return nc


def run_p0(inp):
    nc = build_p0()
    cond = np.stack([fm(inp["c"][0]), fm(inp["c_ctx"])], axis=-1).astype(np.float32)
    maps = []
    for i in range(NCORES):
        sl = slice(i * NMC * 128, (i + 1) * NMC * 128)
        adab = np.stack([fm(inp["ada_b"][l, sl]) for l in range(2)], axis=1)
        maps.append({"cond": cond, "adaw": np.ascontiguousarray(inp["ada_w"][:, :, sl]),
                     "adab": np.ascontiguousarray(adab)})
    res = run(nc, maps)
    full = np.concatenate([r["mod"] for r in res], axis=2)
    return full


CHUNKS = [(0, 512), (512, 512), (1024, 512), (1536, 512), (2048, CPC)]
SUBCH = [(i * 128, 128) for i in range(16)] + [(2048, CPC)]


def build_p1():
    nc = new_nc()
    xT = din(nc, "xT", [D, TT])
    w = din(nc, "w", [D, INC])
    modv = din(nc, "modv", [128, 2, 2, KC])
    gain = din(nc, "gain", [128, KC])
    bgate = din(nc, "bgate", [128, 32])
    qkg = din(nc, "qkg", [128, 2])
    cosd = din(nc, "cosT", [128, TT])
    sind = din(nc, "sinT", [128, TT])
    rmd = din(nc, "rm", [128, 128], BF16)
    qT = dout(nc, "qT", [QW, TT], BF16)
    kT = dout(nc, "kT", [KVW, TT], BF16)
    vo = dout(nc, "v", [TT, KVW], BF16)
    fT = dout(nc, "fT", [FD, TT], BF16)
    gT = dout(nc, "gT", [2 * D, TT], BF16)
    P = Prog(nc)
    hT = P.sbuf("hT", [128, KC, TT], BF16)
    cosb = P.sbuf("cosb", [128, TT], F32)
    sinb = P.sbuf("sinb", [128, TT], F32)
    modb = P.sbuf("modb", [128, 2, 2, KC], F32)
    gb = P.sbuf("gb", [128, KC], F32)
    ab = P.sbuf("ab", [128, 2, KC], F32)
    bgb = P.sbuf("bgb", [128, 32], F32)
    qkb = P.sbuf("qkb", [128, 2], F32)
    qks = P.sbuf("qks", [128, 2], F32)
    rmb = P.sbuf("rmb", [128, 128], BF16)
    ones = P.sbuf("ones", [128, 128], BF16)
    xs_rot = P.rot("xs", 2, [128, KC, 128], F32)
    sq_rot = P.rot("sqc", 2, [128, KC, 128], BF16)
    rs_rot = P.rot("rs", 3, [128, 3, 512], F32)
    wrot = P.rot("wp", 3, [128, KC, 512], BF16)
    psm = P.rot("psm", 3, [128, 512], F32, psum=True)
    pss = P.rot("pss", 2, [128, 512], F32, psum=True)
    psr = P.rot("psr", 2, [128, 512], F32, psum=True)
    sqh = P.rot("sqh", 2, [128, 512], BF16)
    qn_rot = P.rot("qn", 3, [128, 512], BF16)
    t1_rot = P.rot("t1", 2, [128, 512], F32)
    t2_rot = P.rot("t2", 2, [128, 512], F32)
    ob_rot = P.rot("ob", 4, [128, 512], BF16)

    for (dst, src) in ((cosb, cosd), (sinb, sind), (modb, modv), (gb, gain), (bgb, bgate), (qkb, qkg), (rmb, rmd)):
        P.dma("sp", dst[:], src, writes=[dst])
    P.emit("pool", lambda e: e.memset(ones[:], 1.0), writes=[ones])
    epsb = P.sbuf("epsb", [128, 1], F32)
    P.emit("pool", lambda e: e.memset(epsb[:], float(HD * EPS)), writes=[epsb])
    for r in rs_rot.bufs:
        P.emit("pool", lambda e, r=r: e.memset(r[:, 2, :], -0.5), writes=[r])
    for wi in range(2):
        P.emit("dve", lambda e, wi=wi: e.tensor_scalar(out=ab[:, wi, :], in0=modb[:, wi, 1, :], scalar1=1.0,
                                                       scalar2=float(math.sqrt(D)), op0=ALU.add, op1=ALU.mult),
               reads=[modb], writes=[ab])
        P.emit("dve", lambda e, wi=wi: e.tensor_tensor(out=ab[:, wi, :], in0=ab[:, wi, :], in1=gb[:], op=ALU.mult),
               reads=[ab, gb], writes=[ab])
    P.emit("dve", lambda e: e.tensor_scalar(out=qks[:], in0=qkb[:], scalar1=float(math.sqrt(HD)), scalar2=None,
                                            op0=ALU.mult), reads=[qkb], writes=[qks])

    class V:
        pass
    a_lat = ab.t[:, 0, :]
    a_ctx = ab.t[:, 1, :]
    b_lat = modb.t[:, 0, 0, :]
    b_ctx = modb.t[:, 1, 0, :]

    for (t0, tn) in SUBCH:
        isctx = t0 >= TPC
        a, b = (a_ctx, b_ctx) if isctx else (a_lat, b_lat)
        xs = xs_rot.next()
        P.dma("sp", xs[:, :, 0:tn], xT[:, t0:t0 + tn].rearrange("(kc p) t -> p kc t", p=128), writes=[xs])
        sq = sq_rot.next()
        P.emit("act", lambda e, sq=sq, xs=xs, tn=tn: e.activation(out=sq[:, :, 0:tn], in_=xs[:, :, 0:tn], func=AF.Square),
               reads=[xs], writes=[sq])
        ps_ = pss.next()
        for kc in range(KC):
            P.emit("pe", lambda e, ps_=ps_, sq=sq, kc=kc, tn=tn: e.matmul(
                ps_[:, 0:tn], lhsT=ones[:], rhs=sq[:, kc, 0:tn], start=(kc == 0), stop=(kc == KC - 1)),
                reads=[sq, ones], writes=[ps_], inc=(kc == KC - 1))
        rs = rs_rot.next()
        P.emit("dve", lambda e, rs=rs, ps_=ps_, tn=tn: e.tensor_scalar(
            out=rs[:, 0, 0:tn], in0=ps_[:, 0:tn], scalar1=float(D * EPS), scalar2=None, op0=ALU.add),
            reads=[ps_], writes=[rs])
        P.emit("pool", lambda e, rs=rs, tn=tn: e.tensor_tensor(
            out=rs[:, 1, 0:tn], in0=rs[:, 0, 0:tn], in1=rs[:, 2, 0:tn], op=ALU.pow), reads=[rs], writes=[rs])
        for kc in range(KC):
            eng = "dve" if kc % 2 == 0 else "pool"
            P.emit(eng, lambda e, kc=kc, rs=rs, xs=xs, tn=tn: e.tensor_tensor(
                out=xs[:, kc, 0:tn], in0=xs[:, kc, 0:tn], in1=rs[:, 1, 0:tn], op=ALU.mult), reads=[xs, rs], writes=[xs])
            P.emit("act", lambda e, xs=xs, kc=kc, a=a, b=b, t0=t0, tn=tn: e.activation(
                out=hT[:, kc, t0:t0 + tn], in_=xs[:, kc, 0:tn], func=AF.Identity,
                bias=b[:, kc:kc + 1], scale=a[:, kc:kc + 1]), reads=[xs, ab, modb], writes=[hT])

    pending = []

    def flush(n_keep):
        while len(pending) > n_keep:
            st = pending.pop(0)
            st()

    def head_epilogue(ps_, j, t0, tn, is_q):
        gcol = 0 if is_q else 1
        st = {}

        def stageA():
            sq = sqh.next()
            st["sq"] = sq
            P.emit("act", lambda e: e.activation(out=sq[:, 0:tn], in_=ps_[:, 0:tn], func=AF.Square),
                   reads=[ps_], writes=[sq])

        def stageB():
            sq = st["sq"]
            p2 = pss.next()
            P.emit("pe", lambda e: e.matmul(p2[:, 0:tn], lhsT=ones[:], rhs=sq[:, 0:tn], start=True, stop=True),
                   reads=[sq, ones], writes=[p2])
            rs = rs_rot.next()
            P.emit("act", lambda e: e.activation(out=rs[:, 0, 0:tn], in_=p2[:, 0:tn], func=AF.Sqrt, bias=epsb[:, 0:1]),
                   reads=[p2, epsb], writes=[rs])
            P.emit("dve", lambda e: e.reciprocal(out=rs[:, 1, 0:tn], in_=rs[:, 0, 0:tn]), reads=[rs], writes=[rs])
            qn = qn_rot.next()
            st["qn"] = qn
            P.emit("dve", lambda e: e.scalar_tensor_tensor(
                out=qn[:, 0:tn], in0=ps_[:, 0:tn], scalar=qks[:, gcol:gcol + 1], in1=rs[:, 1, 0:tn],
                op0=ALU.mult, op1=ALU.mult), reads=[ps_, qks, rs], writes=[qn])

        def stageC():
            qn = st["qn"]
            p3 = psr.next()
            P.emit("pe", lambda e: e.matmul(p3[:, 0:tn], lhsT=rmb[:], rhs=qn[:, 0:tn], start=True, stop=True),
                   reads=[qn, rmb], writes=[p3])
            t1 = t1_rot.next()
            P.emit("dve", lambda e: e.tensor_tensor(out=t1[:, 0:tn], in0=qn[:, 0:tn], in1=cosb[:, t0:t0 + tn],
                                                    op=ALU.mult), reads=[qn, cosb], writes=[t1])
            t2 = t2_rot.next()
            P.emit("dve", lambda e: e.tensor_tensor(out=t2[:, 0:tn], in0=p3[:, 0:tn], in1=sinb[:, t0:t0 + tn],
                                                    op=ALU.mult), reads=[p3, sinb], writes=[t2])
            ob = ob_rot.next()
            P.emit("dve", lambda e: e.tensor_tensor(out=ob[:, 0:tn], in0=t1[:, 0:tn], in1=t2[:, 0:tn], op=ALU.add),
                   reads=[t1, t2], writes=[ob])
            dst = qT[j * 128:(j + 1) * 128, t0:t0 + tn] if is_q else kT[(j - 16) * 128:(j - 15) * 128, t0:t0 + tn]
            P.dma("sp", dst, ob[:, 0:tn], reads=[ob], is_output=True)

        stageA()
        pending.append(stageB)
        pending.append(stageC)

    for pn in range(16):
        wbuf = wrot.next()
        for h in range(2):
            P.dma("pool", wbuf[:, h * 8:(h + 1) * 8, :],
                  w[h * 1024:(h + 1) * 1024, pn * 512:(pn + 1) * 512].rearrange("(kc p) n -> p kc n", p=128),
                  writes=[wbuf], acc=(h > 0))
        if pn == 5:
            tiles = [(i * 128, 128) for i in range(16)] + [(2048, CPC)]
            for (t0, tn) in tiles:
                ps_ = psm.next()
                for kc in range(KC):
                    P.emit("pe", lambda e, ps_=ps_, kc=kc, t0=t0, tn=tn, wbuf=wbuf: e.matmul(
                        ps_[0:tn, :], lhsT=hT[:, kc, t0:t0 + tn], rhs=wbuf[:, kc, :], start=(kc == 0), stop=(kc == KC - 1)),
                        reads=[hT, wbuf], writes=[ps_], inc=(kc == KC - 1))
                ob = ob_rot.next()
                P.emit("act", lambda e, ob=ob, ps_=ps_, tn=tn: e.activation(out=ob[0:tn, :], in_=ps_[0:tn, :], func=AF.Copy),
                       reads=[ps_], writes=[ob])
                P.dma("sp", vo[t0:t0 + tn, :], ob[0:tn, :], reads=[ob], is_output=True)
                flush(1)
            continue
        for jj in range(4):
            j = pn * 4 + jj
            for (t0, tn) in CHUNKS:
                ps_ = psm.next()
                for kc in range(KC):
                    P.emit("pe", lambda e, ps_=ps_, kc=kc, t0=t0, tn=tn, wbuf=wbuf, jj=jj: e.matmul(
                        ps_[:, 0:tn], lhsT=wbuf[:, kc, jj * 128:(jj + 1) * 128], rhs=hT[:, kc, t0:t0 + tn],
                        start=(kc == 0), stop=(kc == KC - 1)),
                        reads=[hT, wbuf], writes=[ps_], inc=(kc == KC - 1))
                flush(1)
                if j < 20:
                    head_epilogue(ps_, j, t0, tn, j < 16)
                elif j < 32:
                    ob = ob_rot.next()
                    P.emit("act", lambda e, ob=ob, ps_=ps_, tn=tn: e.activation(out=ob[:, 0:tn], in_=ps_[:, 0:tn], func=AF.Copy),
                           reads=[ps_], writes=[ob])
                    P.dma("sp", fT[(j - 24) * 128:(j - 23) * 128, t0:t0 + tn], ob[:, 0:tn], reads=[ob], is_output=True)
                else:
                    g = j - 32
                    ob = ob_rot.next()
                    P.emit("act", lambda e, ob=ob, ps_=ps_, tn=tn, g=g: e.activation(
                        out=ob[:, 0:tn], in_=ps_[:, 0:tn], func=AF.Sigmoid, bias=bgb[:, g:g + 1]),
                        reads=[ps_, bgb], writes=[ob])
                    P.dma("sp", gT[g * 128:(g + 1) * 128, t0:t0 + tn], ob[:, 0:tn], reads=[ob], is_output=True)
    flush(0)
    P.finish()
    return nc


def rope_tables():
    t = np.arange(SEQ)
    row = (t // GRID_W).astype(np.float32)
    col = (t % GRID_W).astype(np.float32)
    inv = (10000.0 ** (-np.arange(32, dtype=np.float32) / 32)).astype(np.float32)
    ar = row[:, None] * inv
    ac = col[:, None] * inv
    ang = np.concatenate([ar, ar, ac, ac], axis=-1)
    return np.cos(ang).T.astype(np.float32), np.sin(ang).T.astype(np.float32)


def rot_matrix():
    R = np.zeros((128, 128), np.float32)
    for base in (0, 64):
        for i in range(32):
            R[base + 32 + i, base + i] = -1.0
            R[base + i, base + 32 + i] = 1.0
    return R.astype(NPBF)


def run_p1(nc, l, inp, xT_cores, mod):
    cosT, sinT = rope_tables()
    rm = rot_matrix()
    maps = []
    modv = np.stack([np.stack([mod[:, l, 0:16, wi], mod[:, l, 16:32, wi]], axis=1) for wi in range(2)], axis=1)
    modv = np.ascontiguousarray(modv.astype(np.float32))
    for i in range(NCORES):
        cs = np.concatenate([cosT[:, i * TPC:(i + 1) * TPC], np.ones((128, CPC), np.float32)], axis=1)
        sn = np.concatenate([sinT[:, i * TPC:(i + 1) * TPC], np.zeros((128, CPC), np.float32)], axis=1)
        maps.append({
            "xT": xT_cores[i], "w": inp["w_in"][l], "modv": modv, "gain": fm(inp["norm_attn_g"][l]),
            "bgate": fm(inp["b_gate"][l]),
            "qkg": np.ascontiguousarray(np.stack([inp["q_norm_g"][l], inp["k_norm_g"][l]], axis=1)),
            "cosT": np.ascontiguousarray(cs), "sinT": np.ascontiguousarray(sn), "rm": rm})
    return run(nc, maps)


NKEY = CTX + SEQ
NKC = NKEY // 128
ATTN_SCALE = HD ** -0.5


def build_p2(with_ctx):
    nc = new_nc()
    nq = SEQ + (CTX if with_ctx else 0)
    qT = din(nc, "qT", [2, 128, nq], BF16)
    kT = din(nc, "kT", [128, NKEY], BF16)
    v = din(nc, "v", [NKEY, 128], BF16)
    oT = dout(nc, "oT", [2, 128, nq], BF16)
    P = Prog(nc)
    kb = P.sbuf("kb", [128, NKEY], BF16)
    vb = P.sbuf("vb", [128, NKC, 128], BF16)
    ones = P.sbuf("ones", [128, 128], BF16)
    qrot = P.rot("qc", 3, [128, 512], BF16)
    prot = P.rot("pb", 4, [128, 512], BF16)
    rrot = P.rot("ri", 2, [128, 512], F32)
    orot = P.rot("ob", 2, [128, 512], BF16)
    psS = P.rot("psS", 4, [128, 512], F32, psum=True)
    psO = P.rot("psO", 2, [128, 512], F32, psum=True)
    psL = P.rot("psL", 2, [128, 512], F32, psum=True)
    hlrot = P.rot("hl", 2, [128, 2, 512], BF16)

    class _AccRot:
        def __init__(self):
            self.items = []
            for i in range(2):
                b0 = P.sbuf("acc%d" % i, [128, 2, 512], F32)
                b1 = Buf(b0.t, "acc%db" % i)
                self.items.append((b0, b1))
            self.i = 0

        def next(self):
            it = self.items[self.i % 2]
            self.i += 1
            return it
    accrot = _AccRot()
    P.emit("pool", lambda e: e.memset(ones[:], 1.0), writes=[ones])
    for h in range(4):
        c0, c1 = h * 4160, (h + 1) * 4160
        P.dma("sp", kb[:, c0:c1], kT[:, c0:c1], writes=[kb], acc=(h > 0))
    for h in range(5):
        c0, c1 = h * 26, (h + 1) * 26
        P.dma("sp", vb[:, c0:c1, :], v[c0 * 128:c1 * 128, :].rearrange("(c p) d -> p c d", p=128), writes=[vb], acc=(h > 0))
    qchunks = [(i * 512, 512, NKC) for i in range(SEQ // 512)]
    if with_ctx:
        qchunks.append((SEQ, CTX, CTX // 128))
    def do_chunk(hh, t0, tn, nk):
        qc = qrot.next()
        P.dma("sp", qc[:, 0:tn], qT[hh, :, t0:t0 + tn], writes=[qc])
        po = psO.next()
        pl = psL.next()
        accb = accrot.next()
        acc = accb[0].t
        accs = accb
        sbufs = {}

        def S(kc):
            ps = psS.next()
            sbufs[kc] = ps
            P.emit("pe", lambda e: e.matmul(ps[:, 0:tn], lhsT=kb[:, kc * 128:(kc + 1) * 128],
                                            rhs=qc[:, 0:tn], start=True, stop=True),
                   reads=[kb, qc], writes=[ps])

        def step(kc):
            ps = sbufs.pop(kc)
            pb = prot.next()
            P.emit("act", lambda e: e.activation(out=pb[:, 0:tn], in_=ps[:, 0:tn], func=AF.Exp,
                                                 scale=float(ATTN_SCALE)), reads=[ps], writes=[pb])
            P.emit("pe", lambda e: e.matmul(po[:, 0:tn], lhsT=vb[:, kc, :], rhs=pb[:, 0:tn],
                                            start=(kc == 0), stop=(kc == nk - 1)),
                   reads=[vb, pb], writes=[po], inc=(kc == nk - 1))
            par = kc % 2
            eng = "dve" if par == 0 else "pool"
            if kc < 2:
                P.emit(eng, lambda e: e.tensor_copy(out=acc[:, par, 0:tn], in_=pb[:, 0:tn]), reads=[pb], writes=[accs[par]])
            else:
                P.emit(eng, lambda e: e.tensor_tensor(out=acc[:, par, 0:tn], in0=acc[:, par, 0:tn], in1=pb[:, 0:tn], op=ALU.add),
                       reads=[pb, accs[par]], writes=[accs[par]])
        S(0)
        for kc in range(nk):
            if kc + 1 < nk:
                S(kc + 1)
            step(kc)
        P.emit("dve", lambda e: e.tensor_tensor(out=acc[:, 0, 0:tn], in0=acc[:, 0, 0:tn], in1=acc[:, 1, 0:tn], op=ALU.add),
               reads=[accs[0], accs[1]], writes=[accs[0]])
        hl = hlrot.next()
        P.emit("dve", lambda e: e.tensor_copy(out=hl[:, 0, 0:tn], in_=acc[:, 0, 0:tn]), reads=[accs[0]], writes=[hl])
        P.emit("dve", lambda e: e.tensor_tensor(out=hl[:, 1, 0:tn], in0=acc[:, 0, 0:tn], in1=hl[:, 0, 0:tn], op=ALU.subtract),
               reads=[accs[0], hl], writes=[hl])
        P.emit("pe", lambda e: e.matmul(pl[:, 0:tn], lhsT=ones[:], rhs=hl[:, 0, 0:tn], start=True, stop=False),
               reads=[ones, hl], writes=[pl], inc=False)
        P.emit("pe", lambda e: e.matmul(pl[:, 0:tn], lhsT=ones[:], rhs=hl[:, 1, 0:tn], start=False, stop=True),
               reads=[ones, hl], writes=[pl])
        ri = rrot.next()
        P.emit("dve", lambda e: e.reciprocal(out=ri[:, 0:tn], in_=pl[:, 0:tn]), reads=[pl], writes=[ri])
        ob = orot.next()
        P.emit("dve", lambda e: e.tensor_tensor(out=ob[:, 0:tn], in0=po[:, 0:tn], in1=ri[:, 0:tn],
                                                op=ALU.mult), reads=[po, ri], writes=[ob])
        P.dma("sp", oT[hh, :, t0:t0 + tn], ob[:, 0:tn], reads=[ob], is_output=True)

    for hh in range(2):
        for (t0, tn, nk) in qchunks:
            do_chunk(hh, t0, tn, nk)
    P.finish()
    return nc


def run_p2(nc, with_ctx, qT_full, kT_full, v_full):
    maps = []
    for i in range(NCORES):
        kv = i // 2
        maps.append({"qT": np.ascontiguousarray(qT_full[i * 256:(i + 1) * 256].reshape(2, 128, -1)),
                     "kT": np.ascontiguousarray(kT_full[kv * 128:(kv + 1) * 128]),
                     "v": np.ascontiguousarray(v_full[:, kv * 128:(kv + 1) * 128])})
    res = run(nc, maps)
    return np.concatenate([r["oT"].reshape(256, -1) for r in res], axis=0)


def build_p3a():
    nc = new_nc()
    fT = din(nc, "fT", [256, SEQ], BF16)
    ccs_d = din(nc, "ccs", [128, 2, 256], BF16)
    YT = dout(nc, "YT", [2, 128, SEQ], BF16)
    P = Prog(nc)
    fb = P.sbuf("fb", [128, 2, SEQ], BF16)
    ccs = P.sbuf("ccs_s", [128, 2, 256], BF16)
    yb = P.sbuf("yb", [128, 2, SEQ], BF16)
    psr = P.rot("ps", 4, [128, 512], F32, psum=True)
    P.dma("sp", ccs[:], ccs_d, writes=[ccs])
    for cc in range(2):
        for h in range(4):
            P.dma("sp", fb[:, cc, h * 4096:(h + 1) * 4096], fT[cc * 128:(cc + 1) * 128, h * 4096:(h + 1) * 4096],
                  writes=[fb], acc=(cc + h > 0))
    n = 0
    for tc in range(SEQ // 512):
        for ri in range(2):
            ps = psr.next()
            for cc in range(2):
                P.emit("pe", lambda e, ps=ps, tc=tc, ri=ri, cc=cc: e.matmul(
                    ps[:], lhsT=ccs[:, cc, ri * 128:(ri + 1) * 128], rhs=fb[:, cc, tc * 512:(tc + 1) * 512],
                    start=(cc == 0), stop=(cc == 1)), reads=[ccs, fb], writes=[ps], inc=(cc == 1))
            if n % 2 == 0:
                P.emit("act", lambda e, ps=ps, tc=tc, ri=ri: e.activation(out=yb[:, ri, tc * 512:(tc + 1) * 512], in_=ps[:], func=AF.Copy),
                       reads=[ps], writes=[yb])
            else:
                P.emit("dve", lambda e, ps=ps, tc=tc, ri=ri: e.tensor_copy(out=yb[:, ri, tc * 512:(tc + 1) * 512], in_=ps[:]),
                       reads=[ps], writes=[yb])
            n += 1
    for ri in range(2):
        for h in range(4):
            P.dma("sp", YT[ri, :, h * 4096:(h + 1) * 4096], yb[:, ri, h * 4096:(h + 1) * 4096], reads=[yb], is_output=True)
    P.finish()
    return nc


def build_p3b(with_ctx):
    nc = new_nc()
    Yd = din(nc, "Y", [2, 128, SEQ], BF16)
    fc_d = din(nc, "fc", [256, CTX], BF16)
    ccs_d = din(nc, "ccs", [128, 2, 256], BF16)
    w1_d = din(nc, "w1", [128, 2, 256], BF16)
    tw_d = din(nc, "tw", [128, 2, 512])
    w2_d = din(nc, "w2", [128, 2, 128], BF16)
    w256_d = din(nc, "w256", [128, 2, 2, 256], BF16)
    Fo = dout(nc, "Fo", [128, SEQ], BF16)
    Fc = dout(nc, "Fc", [128, CTX], BF16)
    P = Prog(nc)
    fb = P.sbuf("fb", [128, 2, SEQ], BF16)
    yr = P.sbuf("yr", [128, SEQ], BF16)
    yi = P.sbuf("yi", [128, SEQ], BF16)
    ccs = P.sbuf("ccs_s", [128, 2, 256], BF16)
    w1 = P.sbuf("w1_s", [128, 2, 256], BF16)
    tw = P.sbuf("tw_s", [128, 2, 512], F32)
    w2 = P.sbuf("w2_s", [128, 2, 128], BF16)
    w256 = P.sbuf("w256_s", [128, 2, 2, 256], BF16)
    fcb = P.sbuf("fcb", [128, 2, CTX], BF16)
    ycb = P.sbuf("ycb", [128, 2, 256], BF16)
    ocb = P.sbuf("ocb", [128, CTX], BF16)
    arot = P.rot("A", 2, [128, 512], F32)
    brot = P.rot("B", 2, [128, 512], F32)
    psr = P.rot("ps", 4, [128, 512], F32, psum=True)
    for (dst, src) in ((ccs, ccs_d), (w1, w1_d), (tw, tw_d), (w2, w2_d), (w256, w256_d)):
        P.dma("sp", dst[:], src, writes=[dst])
    for h in range(4):
        P.dma("sp", yr[:, h * 4096:(h + 1) * 4096], Yd[0, :, h * 4096:(h + 1) * 4096], writes=[yr], acc=(h > 0))
    for h in range(4):
        P.dma("sp", yi[:, h * 4096:(h + 1) * 4096], Yd[1, :, h * 4096:(h + 1) * 4096], writes=[yi], acc=(h > 0))
    yrv = yr.t[:, :].rearrange("p (j n) -> p j n", n=128)
    yiv = yi.t[:, :].rearrange("p (j n) -> p j n", n=128)
    for jp in range(64):
        ps = psr.next()
        psv = ps.t[:, :].rearrange("p (a c) -> p a c", c=256)
        for q in range(2):
            j = jp * 2 + q
            P.emit("pe", lambda e, psv=psv, q=q, j=j: e.matmul(psv[:, q, :], lhsT=yrv[:, j, :], rhs=w1[:, 0, :],
                                                               start=True, stop=False), reads=[yr, w1], writes=[ps], inc=False)
            P.emit("pe", lambda e, psv=psv, q=q, j=j: e.matmul(psv[:, q, :], lhsT=yiv[:, j, :], rhs=w1[:, 1, :],
                                                               start=False, stop=True), reads=[yi, w1], writes=[ps], inc=(q == 1))
        A = arot.next()
        B = brot.next()
        P.emit("dve", lambda e, A=A, ps=ps: e.tensor_tensor(out=A[:], in0=ps[:], in1=tw[:, 0, :], op=ALU.mult),
               reads=[ps, tw], writes=[A])
        P.emit("dve", lambda e, B=B, ps=ps: e.tensor_tensor(out=B[:], in0=ps[:], in1=tw[:, 1, :], op=ALU.mult),
               reads=[ps, tw], writes=[B])
        for q in range(2):
            o0 = jp * 256 + q * 128
            P.emit("pool", lambda e, A=A, B=B, q=q, o0=o0: e.tensor_tensor(
                out=fb[:, 0, o0:o0 + 128], in0=A[:, q * 256:q * 256 + 128], in1=B[:, q * 256 + 128:q * 256 + 256],
                op=ALU.subtract), reads=[A, B], writes=[fb])
            P.emit("pool", lambda e, A=A, B=B, q=q, o0=o0: e.tensor_tensor(
                out=fb[:, 1, o0:o0 + 128], in0=B[:, q * 256:q * 256 + 128], in1=A[:, q * 256 + 128:q * 256 + 256],
                op=ALU.add), reads=[A, B], writes=[fb])
    for cq in range(32):
        ps = psr.next()
        P.emit("pe", lambda e, ps=ps, cq=cq: e.matmul(ps[:], lhsT=w2[:, 0, :], rhs=fb[:, 0, cq * 512:(cq + 1) * 512],
                                                      start=True, stop=False), reads=[fb, w2], writes=[ps], inc=False)
        P.emit("pe", lambda e, ps=ps, cq=cq: e.matmul(ps[:], lhsT=w2[:, 1, :], rhs=fb[:, 1, cq * 512:(cq + 1) * 512],
                                                      start=False, stop=True), reads=[fb, w2], writes=[ps])
        if cq % 2 == 0:
            P.emit("act", lambda e, ps=ps, cq=cq: e.activation(out=yr[:, cq * 512:(cq + 1) * 512], in_=ps[:], func=AF.Copy,
                                                               scale=float(1.0 / 2048.0)), reads=[ps], writes=[yr])
        else:
            P.emit("dve", lambda e, ps=ps, cq=cq: e.tensor_scalar(out=yr[:, cq * 512:(cq + 1) * 512], in0=ps[:],
                                                                  scalar1=float(1.0 / 2048.0), scalar2=None, op0=ALU.mult),
                   reads=[ps], writes=[yr])
    for h in range(4):
        P.dma("sp", Fo[:, h * 4096:(h + 1) * 4096], yr[:, h * 4096:(h + 1) * 4096], reads=[yr], is_output=True)
    if with_ctx:
        for cc in range(2):
            P.dma("sp", fcb[:, cc, :], fc_d[cc * 128:(cc + 1) * 128, :], writes=[fcb], acc=(cc > 0))
        for tt in range(2):
            ps = psr.next()
            for cc in range(2):
                P.emit("pe", lambda e, ps=ps, tt=tt, cc=cc: e.matmul(
                    ps[:, 0:256], lhsT=fcb[:, cc, tt * 128:(tt + 1) * 128], rhs=ccs[:, cc, :], start=(cc == 0), stop=(cc == 1)),
                    reads=[fcb, ccs], writes=[ps], inc=(cc == 1))
            P.emit("act", lambda e, ps=ps, tt=tt: e.activation(out=ycb[:, tt, :], in_=ps[:, 0:256], func=AF.Copy),
                   reads=[ps], writes=[ycb])
        ps = psr.next()
        n = 0
        for tt in range(2):
            for ri in range(2):
                P.emit("pe", lambda e, ps=ps, tt=tt, ri=ri, n=n: e.matmul(
                    ps[:, 0:256], lhsT=ycb[:, tt, ri * 128:(ri + 1) * 128], rhs=w256[:, tt, ri, :], start=(n == 0), stop=(n == 3)),
                    reads=[ycb, w256], writes=[ps], inc=(n == 3))
                n += 1
        P.emit("act", lambda e, ps=ps: e.activation(out=ocb[:], in_=ps[:, 0:256], func=AF.Copy, scale=float(1.0 / 256.0)),
               reads=[ps], writes=[ocb])
    else:
        P.emit("pool", lambda e: e.memset(ocb[:], 0.0), writes=[ocb])
    P.dma("sp", Fc, ocb[:], reads=[ocb], is_output=True)
    P.finish()
    return nc


def p3_consts(half):
    p = np.arange(128)
    out = {}
    ccs = np.zeros((128, 2, 256), np.float64)
    j = 128 * half + np.arange(128)
    for cc in range(2):
        c = cc * 128 + p
        ang = 2 * np.pi * np.outer(c, j) / 256.0
        ccs[:, cc, 0:128] = np.cos(ang)
        ccs[:, cc, 128:256] = np.sin(ang)
    out["ccs"] = ccs.astype(NPBF)
    a128 = 2 * np.pi * np.outer(p, p) / 128.0
    C, S = np.cos(a128), np.sin(a128)
    w1 = np.zeros((128, 2, 256))
    w1[:, 0, 0:128] = C
    w1[:, 0, 128:256] = S
    w1[:, 1, 0:128] = -S
    w1[:, 1, 128:256] = C
    out["w1"] = w1.astype(NPBF)
    psi = 2 * np.pi * np.outer(p, p) / float(SEQ)
    tw = np.zeros((128, 2, 512))
    tw[:, 0, :] = np.tile(np.cos(psi), (1, 4))
    tw[:, 1, :] = np.tile(np.sin(psi), (1, 4))
    out["tw"] = tw.astype(np.float32)
    w2 = np.zeros((128, 2, 128))
    w2[:, 0] = C
    w2[:, 1] = -S
    out["w2"] = w2.astype(NPBF)
    w256 = np.zeros((128, 2, 2, 256))
    for tt in range(2):
        n = tt * 128 + p
        ang = 2 * np.pi * np.outer(n, np.arange(256)) / 256.0
        w256[:, tt, 0] = np.cos(ang)
        w256[:, tt, 1] = -np.sin(ang)
    out["w256"] = w256.astype(NPBF)
    out["ident"] = np.eye(128).astype(NPBF)
    return out


def run_p3(nca, ncb, fT_full):
    consts = [p3_consts(h) for h in range(2)]
    maps = []
    for i in range(NCORES):
        g, half = i // 2, i % 2
        maps.append({"fT": np.ascontiguousarray(fT_full[g * 256:(g + 1) * 256, :SEQ]), "ccs": consts[half]["ccs"]})
    ra = run(nca, maps)
    maps = []
    for i in range(NCORES):
        g, half = i // 2, i % 2
        c = consts[half]
        YT = ra[i]["YT"]
        Y = np.ascontiguousarray(YT.reshape(2, 128, 128, 128).transpose(0, 2, 1, 3)).reshape(2, 128, SEQ)
        maps.append({"Y": Y, "fc": np.ascontiguousarray(fT_full[g * 256:(g + 1) * 256, SEQ:]), "ccs": c["ccs"], "w1": c["w1"],
                     "tw": c["tw"], "w2": c["w2"], "w256": c["w256"]})
    rb = run(ncb, maps)
    outs = []
    for i in range(NCORES):
        Fo = rb[i]["Fo"].reshape(128, 128, 128)
        lat = np.ascontiguousarray(Fo.transpose(1, 0, 2)).reshape(128, SEQ)
        outs.append(np.concatenate([lat, rb[i]["Fc"]], axis=1))
    return np.concatenate(outs, axis=0)


DBG = {}


def build_p4(moe, with_ctx, last):
    nc = new_nc()
    ntok = TT if with_ctx else TPC
    xT = din(nc, "xT", [D, ntok])
    at_d = din(nc, "attnT", [QW, ntok], BF16)
    ft_d = din(nc, "FT", [FD, ntok], BF16)
    gt_d = din(nc, "gT", [2 * D, ntok], BF16)
    wp = din(nc, "wp", [QW, D])
    wf = din(nc, "wf", [FD, D])
    wo = din(nc, "wo", [D, D])
    if moe:
        rw_d = din(nc, "rw", [128, KC, 8])
        mg = din(nc, "mg", [NE, D, DFE])
        mu = din(nc, "mu", [NE, D, DFE])
        md = din(nc, "md", [NE, DFE, D])
        id_d = din(nc, "ident", [128, 128], BF16)
    else:
        wg = din(nc, "wg", [D, DFF])
        wu = din(nc, "wu", [D, DFF])
        wd = din(nc, "wd", [DFF, D])
    modv = din(nc, "modv", [128, 2, 4, KC])
    gain2 = din(nc, "gain2", [128, KC])
    fgain = din(nc, "fgain", [128, KC])
    xo = dout(nc, "xo", [D, ntok])
    P = Prog(nc)
    xs = P.sbuf("xs", [128, KC, 512], F32)
    atb = P.sbuf("atb", [128, KC, 512], BF16)
    fg = P.sbuf("fg", [128, (40 if moe else 44) * 512], BF16)
    yT = P.sbuf("yT", [128, KC, 512], BF16)
    ones = P.sbuf("ones", [128, 128], BF16)
    modb = P.sbuf("modb", [128, 2, 4, KC], F32)
    g2b = P.sbuf("g2b", [128, KC], F32)
    fgb = P.sbuf("fgb", [128, KC], F32)
    a2b = P.sbuf("a2b", [128, 2, KC], F32)
    wrot = P.rot("wb", 3, [128, 8192], BF16)
    t1r = P.rot("t1", 2, [128, 512], F32)
    t2r = P.rot("t2", 2, [128, 512], F32)
    sr = P.rot("sl", 2, [128, 512], F32)
    rsr = P.rot("rs", 2, [128, 3, 512], F32)
    psm = P.rot("psm", 6, [128, 512], F32, psum=True)
    pss = P.rot("pss", 2, [128, 512], F32, psum=True)
    ftv = fg.t[:, 0:8 * 512].rearrange("p (k t) -> p k t", t=512)
    gtv = fg.t[:, 8 * 512:40 * 512].rearrange("p (k t) -> p k t", t=512)
    aTv = fg.t[:, :].rearrange("p (k t) -> p k t", t=512)
    for (dst, src) in ((modb, modv), (g2b, gain2), (fgb, fgain)):
        P.dma("sp", dst[:], src, writes=[dst])
    P.emit("pool", lambda e: e.memset(ones[:], 1.0), writes=[ones])
    for r in rsr.bufs:
        P.emit("pool", lambda e, r=r: e.memset(r[:, 2, :], -0.5), writes=[r])
    for wi in range(2):
        P.emit("dve", lambda e, wi=wi: e.tensor_scalar(out=a2b[:, wi, :], in0=modb[:, wi, 2, :], scalar1=1.0,
                                                       scalar2=float(math.sqrt(D)), op0=ALU.add, op1=ALU.mult),
               reads=[modb], writes=[a2b])
        P.emit("dve", lambda e, wi=wi: e.tensor_tensor(out=a2b[:, wi, :], in0=a2b[:, wi, :], in1=g2b[:], op=ALU.mult),
               reads=[a2b, g2b], writes=[a2b])
    P.emit("dve", lambda e: e.tensor_scalar(out=fgb[:], in0=fgb[:], scalar1=float(math.sqrt(D)), scalar2=None, op0=ALU.mult),
           reads=[fgb], writes=[fgb])
    if moe:
        rwf = P.sbuf("rwf", [128, KC, 8], F32)
        rwb = P.sbuf("rwb", [128, KC, 16], BF16)
        ident = P.sbuf("ident_s", [128, 128], BF16)
        bcs = P.sbuf("bcs", [128, NE, 512], F32)
        l16 = P.sbuf("l16", [128, 16], F32)
        sm = P.sbuf("sm", [128, 8, 8], F32)
        sc1 = P.sbuf("sc1", [128, 8], F32)
        rep = P.rot("rep", 2, [128, 2, NE, 128], BF16)
        u2r = P.rot("u2", 2, [128, 512], F32)
        P.dma("sp", rwf[:], rw_d, writes=[rwf])
        P.dma("sp", ident[:], id_d, writes=[ident])
        P.emit("act", lambda e: e.activation(out=rwb[:, :, 0:8], in_=rwf[:], func=AF.Copy), reads=[rwf], writes=[rwb])
        P.emit("dve", lambda e: e.tensor_tensor(out=rwb[:, :, 8:16], in0=rwf[:], in1=rwb[:, :, 0:8], op=ALU.subtract),
               reads=[rwf, rwb], writes=[rwb])

    def load_w(src2d, k0, nkc, n0, ncols):
        wb = wrot.next()
        view = wb.t[:, 0:nkc * ncols].rearrange("p (k n) -> p k n", n=ncols)
        first = True
        for h0 in range(0, nkc, 8):
            h1 = min(nkc, h0 + 8)
            P.dma("pool", view[:, h0:h1, :],
                  src2d[k0 + h0 * 128:k0 + h1 * 128, n0:n0 + ncols].rearrange("(kc p) n -> p kc n", p=128),
                  writes=[wb], acc=(not first))
            first = False
        return wb, view

    def mm_group(ps, tn, parts, inc_last=True):
        n = len(parts)
        for i, (wbuf, lhsT, rbuf, rhs) in enumerate(parts):
            P.emit("pe", lambda e, lhsT=lhsT, rhs=rhs, i=i: e.matmul(ps[:, 0:tn], lhsT=lhsT, rhs=rhs, start=(i == 0), stop=(i == n - 1)),
                   reads=[wbuf, rbuf], writes=[ps], inc=(inc_last and i == n - 1))

    def resid(ps, j, tn, gcol, wi):
        P.emit("dve", lambda e: e.scalar_tensor_tensor(out=xs[:, j, 0:tn], in0=ps[:, 0:tn], scalar=modb[:, wi, gcol, j:j + 1],
                                                       in1=xs[:, j, 0:tn], op0=ALU.mult, op1=ALU.add),
               reads=[ps, modb, xs], writes=[xs])

    def rstd_of_xs(tn):
        P.emit("act", lambda e: e.activation(out=yT[:, :, 0:tn], in_=xs[:, :, 0:tn], func=AF.Square), reads=[xs], writes=[yT])
        ps_ = pss.next()
        mm_group(ps_, tn, [(ones, ones[:], yT, yT[:, kc, 0:tn]) for kc in range(KC)])
        rs = rsr.next()
        P.emit("dve", lambda e: e.tensor_scalar(out=rs[:, 0, 0:tn], in0=ps_[:, 0:tn], scalar1=float(D * EPS), scalar2=None,
                                                op0=ALU.add), reads=[ps_], writes=[rs])
        P.emit("pool", lambda e: e.tensor_tensor(out=rs[:, 1, 0:tn], in0=rs[:, 0, 0:tn], in1=rs[:, 2, 0:tn], op=ALU.pow),
               reads=[rs], writes=[rs])
        return rs

    def ffn_gu(c, tn, wgb, wgv, wub, wuv, jj, bc_e=None):
        psG = psm.next()
        mm_group(psG, tn, [(wgb, wgv[:, kc, jj * 128:(jj + 1) * 128], atb, atb[:, kc, 0:tn]) for kc in range(KC)])
        psU = psm.next()
        mm_group(psU, tn, [(wub, wuv[:, kc, jj * 128:(jj + 1) * 128], atb, atb[:, kc, 0:tn]) for kc in range(KC)])
        s_ = sr.next()
        P.emit("act", lambda e: e.activation(out=s_[:, 0:tn], in_=psG[:, 0:tn], func=AF.Silu), reads=[psG], writes=[s_])
        if bc_e is None:
            P.emit("dve", lambda e: e.tensor_tensor(out=aTv[:, c, 0:tn], in0=s_[:, 0:tn], in1=psU[:, 0:tn], op=ALU.mult),
                   reads=[s_, psU], writes=[fg])
        else:
            u2 = u2r.next()
            P.emit("dve", lambda e: e.tensor_tensor(out=u2[:, 0:tn], in0=psU[:, 0:tn], in1=bcs[:, bc_e, 0:tn], op=ALU.mult),
                   reads=[psU, bcs], writes=[u2])
            P.emit("pool", lambda e: e.tensor_tensor(out=aTv[:, c, 0:tn], in0=s_[:, 0:tn], in1=u2[:, 0:tn], op=ALU.mult),
                   reads=[s_, u2], writes=[fg])

    def router_tile(tt, rstage=9):
        psR = pss.next()
        parts = [(atb, atb[:, kc, tt * 128:(tt + 1) * 128], rwb, rwb[:, kc, 0:16]) for kc in range(KC)]
        n = len(parts)
        for i, (wbuf, lhsT, rbuf, rhs) in enumerate(parts):
            P.emit("pe", lambda e, lhsT=lhsT, rhs=rhs, i=i: e.matmul(psR[:, 0:16], lhsT=lhsT, rhs=rhs, start=(i == 0), stop=False),
                   reads=[wbuf, rbuf], writes=[psR], inc=False)
        for kc in range(KC):
            P.emit("pe", lambda e, kc=kc: e.matmul(psR[:, 0:8], lhsT=yT[:, kc, tt * 128:(tt + 1) * 128], rhs=rwb[:, kc, 0:8],
                                                   start=False, stop=(kc == KC - 1)),
                   reads=[yT, rwb], writes=[psR], inc=(kc == KC - 1))
        P.emit("dve", lambda e: e.tensor_copy(out=l16[:], in_=psR[:, 0:16]), reads=[psR], writes=[l16])
        if rstage < 1:
            if tt == 0:
                P.emit("pool", lambda e: e.memset(bcs[:], 0.5), writes=[bcs])
            return
        lg, m1, mk1, l2, m2, mk2, dd = (sm[:, 0, :], sm[:, 1, 0:1], sm[:, 2, :], sm[:, 3, :], sm[:, 1, 1:2], sm[:, 4, :], sm[:, 1, 2:3])
        ee, den, w1, w2 = sm[:, 1, 3:4], sm[:, 1, 4:5], sm[:, 1, 5:6], sm[:, 1, 6:7]
        comb = sm[:, 5, :]
        D_ = lambda fn: P.emit("dve", fn, reads=[sm, l16], writes=[sm])
        D_(lambda e: e.tensor_tensor(out=lg, in0=l16[:, 0:8], in1=l16[:, 8:16], op=ALU.add))
        D_(lambda e: e.reduce_max(out=m1, in_=lg, axis=AX.X))
        D_(lambda e: e.tensor_scalar(out=mk1, in0=lg, scalar1=m1, scalar2=None, op0=ALU.is_equal))
        D_(lambda e: e.scalar_tensor_tensor(out=l2, in0=mk1, scalar=-1e30, in1=lg, op0=ALU.mult, op1=ALU.add))
        D_(lambda e: e.reduce_max(out=m2, in_=l2, axis=AX.X))
        D_(lambda e: e.tensor_scalar(out=mk2, in0=l2, scalar1=m2, scalar2=None, op0=ALU.is_equal))
        D_(lambda e: e.tensor_tensor(out=dd, in0=m2, in1=m1, op=ALU.subtract))
        P.emit("act", lambda e: e.activation(out=ee, in_=dd, func=AF.Exp), reads=[sm], writes=[sm])
        D_(lambda e: e.tensor_scalar(out=den, in0=ee, scalar1=1.0, scalar2=None, op0=ALU.add))
        D_(lambda e: e.reciprocal(out=w1, in_=den))
        D_(lambda e: e.tensor_tensor(out=w2, in0=ee, in1=w1, op=ALU.mult))
        D_(lambda e: e.tensor_scalar(out=comb, in0=mk1, scalar1=w1, scalar2=None, op0=ALU.mult))
        D_(lambda e: e.scalar_tensor_tensor(out=comb, in0=mk2, scalar=w2, in1=comb, op0=ALU.mult, op1=ALU.add))
        if rstage < 2:
            if tt == 0:
                P.emit("pool", lambda e: e.memset(bcs[:], 0.5), writes=[bcs])
            return
        rp = rep.next()
        cb = comb.unsqueeze(2).to_broadcast([128, NE, 128])
        P.emit("dve", lambda e: e.tensor_copy(out=rp[:, 0], in_=cb), reads=[sm], writes=[rp])
        P.emit("dve", lambda e: e.tensor_tensor(out=rp[:, 1], in0=cb, in1=rp[:, 0], op=ALU.subtract), reads=[sm, rp], writes=[rp])
        if rstage < 3:
            if tt == 0:
                P.emit("pool", lambda e: e.memset(bcs[:], 0.5), writes=[bcs])
            return
        for e_ in range(NE):
            def bc_one(e_=e_):
                pb_ = psm.next()
                P.emit("pe", lambda e: e.matmul(pb_[:, 0:128], lhsT=rp[:, 0, e_, :], rhs=ident[:], start=True, stop=False),
                       reads=[rp, ident], writes=[pb_], inc=False)
                P.emit("pe", lambda e: e.matmul(pb_[:, 0:128], lhsT=rp[:, 1, e_, :], rhs=ident[:], start=False, stop=True),
                       reads=[rp, ident], writes=[pb_])
                if e_ % 2 == 0:
                    P.emit("act", lambda e: e.activation(out=bcs[:, e_, tt * 128:(tt + 1) * 128], in_=pb_[:, 0:128], func=AF.Copy),
                           reads=[pb_], writes=[bcs])
                else:
                    P.emit("dve", lambda e: e.tensor_copy(out=bcs[:, e_, tt * 128:(tt + 1) * 128], in_=pb_[:, 0:128]),
                           reads=[pb_], writes=[bcs])
            bc_one()

    def do_chunk(t0, tn, wi):
        P.dma("sp", xs[:, :, 0:tn], xT[:, t0:t0 + tn].rearrange("(kc p) t -> p kc t", p=128), writes=[xs])
        for h in range(2):
            P.dma("sp", atb[:, h * 8:(h + 1) * 8, 0:tn], at_d[h * 1024:(h + 1) * 1024, t0:t0 + tn].rearrange("(kc p) t -> p kc t", p=128),
                  writes=[atb], acc=(h > 0))
        P.dma("sp", ftv[:, :, 0:tn], ft_d[:, t0:t0 + tn].rearrange("(kc p) t -> p kc t", p=128), writes=[fg])
        for h in range(4):
            P.dma("sp", gtv[:, h * 8:(h + 1) * 8, 0:tn], gt_d[h * 1024:(h + 1) * 1024, t0:t0 + tn].rearrange("(kc p) t -> p kc t", p=128),
                  writes=[fg], acc=True)
        for pn in range(4):
            wpb, wpv = load_w(wp, 0, KC, pn * 512, 512)
            wfb, wfv = load_w(wf, 0, 8, pn * 512, 512)
            for jj in range(4):
                j = pn * 4 + jj

                def ya(j=j, jj=jj, wpb=wpb, wpv=wpv, wfb=wfb, wfv=wfv):
                    psA = psm.next()
                    mm_group(psA, tn, [(wpb, wpv[:, kc, jj * 128:(jj + 1) * 128], atb, atb[:, kc, 0:tn]) for kc in range(KC)])
                    psB = psm.next()
                    mm_group(psB, tn, [(wfb, wfv[:, kc, jj * 128:(jj + 1) * 128], fg, ftv[:, kc, 0:tn]) for kc in range(8)])
                    t1 = t1r.next()
                    t2 = t2r.next()
                    P.emit("dve", lambda e: e.tensor_tensor(out=t1[:, 0:tn], in0=psA[:, 0:tn], in1=gtv[:, j, 0:tn], op=ALU.mult),
                           reads=[psA, fg], writes=[t1])
                    P.emit("dve", lambda e: e.tensor_tensor(out=t2[:, 0:tn], in0=psB[:, 0:tn], in1=gtv[:, 16 + j, 0:tn], op=ALU.mult),
                           reads=[psB, fg], writes=[t2])
                    P.emit("pool", lambda e: e.tensor_tensor(out=yT[:, j, 0:tn], in0=t1[:, 0:tn], in1=t2[:, 0:tn], op=ALU.add),
                           reads=[t1, t2], writes=[yT])
                ya()
        for pn in range(4):
            wob, wov = load_w(wo, 0, KC, pn * 512, 512)
            for jj in range(4):
                j = pn * 4 + jj
                psZ = psm.next()
                mm_group(psZ, tn, [(wob, wov[:, kc, jj * 128:(jj + 1) * 128], yT, yT[:, kc, 0:tn]) for kc in range(KC)])
                resid(psZ, j, tn, 0, wi)
        rs = rstd_of_xs(tn)
        for kc in range(KC):
            def hk(kc=kc):
                t1 = t1r.next()
                P.emit("dve" if kc % 2 == 0 else "pool", lambda e: e.tensor_tensor(out=t1[:, 0:tn], in0=xs[:, kc, 0:tn], in1=rs[:, 1, 0:tn],
                                                                                   op=ALU.mult), reads=[xs, rs], writes=[t1])
                if not moe:
                    P.emit("act", lambda e: e.activation(out=atb[:, kc, 0:tn], in_=t1[:, 0:tn], func=AF.Identity,
                                                         bias=modb[:, wi, 1, kc:kc + 1], scale=a2b[:, wi, kc:kc + 1]),
                           reads=[t1, modb, a2b], writes=[atb])
                else:
                    t2 = t2r.next()
                    P.emit("act", lambda e: e.activation(out=t2[:, 0:tn], in_=t1[:, 0:tn], func=AF.Identity,
                                                         bias=modb[:, wi, 1, kc:kc + 1], scale=a2b[:, wi, kc:kc + 1]),
                           reads=[t1, modb, a2b], writes=[t2])
                    P.emit("dve", lambda e: e.tensor_copy(out=atb[:, kc, 0:tn], in_=t2[:, 0:tn]), reads=[t2], writes=[atb])
                    P.emit("pool", lambda e: e.tensor_tensor(out=yT[:, kc, 0:tn], in0=t2[:, 0:tn], in1=atb[:, kc, 0:tn], op=ALU.subtract),
                           reads=[t2, atb], writes=[yT])
            hk()
        if not moe:
            for pn in range(DFF // 512):
                wgb, wgv = load_w(wg, 0, KC, pn * 512, 512)
                wub, wuv = load_w(wu, 0, KC, pn * 512, 512)
                for jj in range(4):
                    ffn_gu(pn * 4 + jj, tn, wgb, wgv, wub, wuv, jj)
            for np_ in range(8):
                wab, wav = load_w(wd, 0, 22, np_ * 256, 256)
                wbb, wbv = load_w(wd, 22 * 128, 22, np_ * 256, 256)
                for jj in range(2):
                    j = np_ * 2 + jj
                    psD = psm.next()
                    parts = [(wab, wav[:, c, jj * 128:(jj + 1) * 128], fg, aTv[:, c, 0:tn]) for c in range(22)]
                    parts += [(wbb, wbv[:, c, jj * 128:(jj + 1) * 128], fg, aTv[:, 22 + c, 0:tn]) for c in range(22)]
                    mm_group(psD, tn, parts)
                    resid(psD, j, tn, 3, wi)
        else:
            if DBG.get("norouter"):
                P.emit("pool", lambda e: e.memset(bcs[:], 0.5), writes=[bcs])
            else:
                for tt in range(tn // 128):
                    router_tile(tt, DBG.get("rstage", 9))
            for e_ in range(DBG.get("nexp", NE)):
                for pn in range(6):
                    ncols = 512 if pn < 5 else 256
                    wgb, wgv = load_w(mg[e_], 0, KC, pn * 512, ncols)
                    wub, wuv = load_w(mu[e_], 0, KC, pn * 512, ncols)
                    for jj in range(ncols // 128):
                        ffn_gu(pn * 4 + jj, tn, wgb, wgv, wub, wuv, jj, bc_e=e_)
                for np_ in range(8):
                    wab, wav = load_w(md[e_], 0, 22, np_ * 256, 256)
                    for jj in range(2):
                        j = np_ * 2 + jj
                        psD = psm.next()
                        mm_group(psD, tn, [(wab, wav[:, c, jj * 128:(jj + 1) * 128], fg, aTv[:, c, 0:tn]) for c in range(22)])
                        resid(psD, j, tn, 3, wi)
        if last:
            rs2 = rstd_of_xs(tn)
            for kc in range(KC):
                P.emit("dve", lambda e, kc=kc: e.scalar_tensor_tensor(out=xs[:, kc, 0:tn], in0=xs[:, kc, 0:tn], scalar=fgb[:, kc:kc + 1],
                                                                      in1=rs2[:, 1, 0:tn], op0=ALU.mult, op1=ALU.mult),
                       reads=[xs, fgb, rs2], writes=[xs])
        P.dma("sp", xo[:, t0:t0 + tn].rearrange("(kc p) t -> p kc t", p=128), xs[:, :, 0:tn], reads=[xs], is_output=True)

    for (t0, tn) in CHUNKS[:DBG.get("nchunk", 9)]:
        if t0 >= TPC and not with_ctx:
            continue
        do_chunk(t0, tn, 1 if t0 >= TPC else 0)
    P.finish()
    return nc


def run_p4(nc, l, moe, with_ctx, inp, xT_cores, attnT_cores, FT_cores, gT_cores, mod):
    modv = np.stack([np.stack([mod[:, l, 32:48, wi], mod[:, l, 48:64, wi], mod[:, l, 64:80, wi], mod[:, l, 80:96, wi]], axis=1)
                     for wi in range(2)], axis=1)
    modv = np.ascontiguousarray(modv.astype(np.float32))
    maps = []
    for i in range(NCORES):
        m = {"xT": xT_cores[i], "attnT": attnT_cores[i], "FT": FT_cores[i], "gT": gT_cores[i],
             "wp": inp["w_attn_proj"][l], "wf": inp["w_four_proj"][l], "wo": inp["w_out"][l],
             "modv": modv, "gain2": fm(inp["norm_ffn_g"][l]), "fgain": fm(inp["final_norm_g"])}
        if moe:
            li = l // 2
            m["rw"] = np.ascontiguousarray(inp["router_w"][li].reshape(KC, 128, NE).transpose(1, 0, 2))
            m["mg"] = inp["moe_w_gate"][li]
            m["mu"] = inp["moe_w_up"][li]
            m["md"] = inp["moe_w_down"][li]
            m["ident"] = np.eye(128).astype(NPBF)
        else:
            li = l // 2
            m["wg"] = inp["ffn_w_gate"][li]
            m["wu"] = inp["ffn_w_up"][li]
            m["wd"] = inp["ffn_w_down"][li]
        maps.append(m)
    res = run(nc, maps)
    return [r["xo"] for r in res]


def kernel(**inp):
    inp = {k: np.asarray(v) for k, v in inp.items()}
    mod = run_p0(inp)
    x = inp["x"][0]
    ctx = inp["ctx"][0]
    xT_cores = [np.ascontiguousarray(np.concatenate([x[i * TPC:(i + 1) * TPC].T, ctx[i * CPC:(i + 1) * CPC].T], axis=1))
                for i in range(NCORES)]
    out = None
    for l in range(2):
        with_ctx = (l == 0)
        moe = (l % 2 == 1)
        last = (l == 1)
        r1 = run_p1(build_p1(), l, inp, xT_cores, mod)

        def gather(name, axis_tok):
            lat = np.concatenate([np.take(r[name], range(0, TPC), axis=axis_tok) for r in r1], axis=axis_tok)
            cx = np.concatenate([np.take(r[name], range(TPC, TT), axis=axis_tok) for r in r1], axis=axis_tok)
            return lat, cx
        q_lat, q_ctx = gather("qT", 1)
        k_lat, k_ctx = gather("kT", 1)
        v_lat, v_ctx = gather("v", 0)
        f_lat, f_ctx = gather("fT", 1)
        q_full = np.concatenate([q_lat, q_ctx], axis=1) if with_ctx else q_lat
        kT_full = np.concatenate([k_ctx, k_lat], axis=1)
        v_full = np.concatenate([v_ctx, v_lat], axis=0)
        oT = run_p2(build_p2(with_ctx), with_ctx, q_full, kT_full, v_full)
        fT_full = np.concatenate([f_lat, f_ctx], axis=1)
        FT = run_p3(build_p3a(), build_p3b(with_ctx), fT_full)

        def percore(a):
            res = []
            for i in range(NCORES):
                if with_ctx:
                    res.append(np.ascontiguousarray(np.concatenate(
                        [a[:, i * TPC:(i + 1) * TPC], a[:, SEQ + i * CPC:SEQ + (i + 1) * CPC]], axis=1)))
                else:
                    res.append(np.ascontiguousarray(a[:, i * TPC:(i + 1) * TPC]))
            return res
        attn_c = percore(oT)
        FT_c = percore(FT)
        ntok = TT if with_ctx else TPC
        g_c = [np.ascontiguousarray(r["gT"][:, :ntok]) for r in r1]
        x_c = [np.ascontiguousarray(xc[:, :ntok]) for xc in xT_cores]
        xo = run_p4(build_p4(moe, with_ctx, last), l, moe, with_ctx, inp, x_c, attn_c, FT_c, g_c, mod)
        if last:
            out = np.concatenate([xo[i][:, :TPC].T for i in range(NCORES)], axis=0)
        else:
            xT_cores = xo
    return np.ascontiguousarray(out.reshape(1, SEQ, D).astype(np.float32))
```

```python
import math
import numpy as np
import ml_dtypes
import concourse.bass as bass
import concourse.mybir as mybir
from concourse.bass_utils import run_bass_kernel_spmd

F32 = mybir.dt.float32
BF16 = mybir.dt.bfloat16
ALU = mybir.AluOpType
AF = mybir.ActivationFunctionType
AX = mybir.AxisListType
NPBF = ml_dtypes.bfloat16

NCORES = 8
D = 2048
KC = 16
SEQ = 16384
CTX = 256
TPC = SEQ // NCORES
CPC = CTX // NCORES
TT = TPC + CPC
NH, NKV, HD = 16, 4, 128
QW, KVW, FD = 2048, 512, 1024
INC = 8192
DFF = 5632
NE = 8
DFE = 2816
EPS = 1e-6
GRID_W = 64
ENGS = ("pe", "act", "dve", "pool", "sp")


class Buf:
    __slots__ = ("t", "last_w", "readers", "name", "ws")

    def __init__(self, t, name=""):
        self.t = t
        self.last_w = None
        self.ws = []
        self.readers = {}
        self.name = name

    def __getitem__(self, idx):
        return self.t[idx]


class Rot:
    def __init__(self, bufs):
        self.bufs = bufs
        self.i = 0

    def next(self):
        b = self.bufs[self.i % len(self.bufs)]
        self.i += 1
        return b


class Prog:
    def __init__(self, nc, same_engine_sync=True, n_dma_sems=8):
        self.nc = nc
        self.streams = {e: [] for e in ENGS}
        self.cnt = {e: 0 for e in ENGS}
        self.waited = {e: {} for e in ENGS}
        self.same_engine_sync = same_engine_sync
        self.sems = {}
        self.ctx = []
        for e in ENGS:
            self.sems[("eng", e)] = self._sem("s_" + e)
        self.dma_rot = {}
        self.dma_val = {}
        for q in ("sp", "act", "pool"):
            lst = []
            for i in range(n_dma_sems):
                k = ("dma", q + str(i))
                self.sems[k] = self._sem("d_%s%d" % (q, i))
                self.dma_val[k] = 0
                lst.append(k)
            self.dma_rot[q] = [lst, 0]
        self.out_tokens = []

    def _sem(self, name):
        cm = self.nc.semaphore(name)
        s = cm.__enter__()
        self.ctx.append(cm)
        return s

    def sbuf(self, name, shape, dtype):
        cm = self.nc.sbuf_tensor(name, shape, dtype)
        t = cm.__enter__()
        self.ctx.append(cm)
        return Buf(t, name)

    def psum(self, name, shape, dtype=F32):
        cm = self.nc.psum_tensor(name, shape, dtype)
        t = cm.__enter__()
        self.ctx.append(cm)
        return Buf(t, name)

    def rot(self, name, n, shape, dtype, psum=False):
        return Rot([(self.psum if psum else self.sbuf)("%s%d" % (name, i), shape, dtype) for i in range(n)])

    def _collect(self, e, reads, writes, acc=False):
        waits = {}

        def need(tok):
            if tok is None:
                return
            k, v = tok
            if k == ("eng", e):
                if e == "pe" or not self.same_engine_sync:
                    return
            if waits.get(k, 0) < v:
                waits[k] = v

        for b in reads:
            need(b.last_w)
            for t_ in b.ws:
                need(t_)
        for b in writes:
            need(b.last_w)
            for t_ in b.ws:
                if acc and t_[0][0] == "dma":
                    continue
                need(t_)
            for k, v in b.readers.items():
                if k == ("eng", e):
                    continue
                need((k, v))
        wl = []
        for k, v in waits.items():
            if self.waited[e].get(k, 0) >= v:
                continue
            self.waited[e][k] = v
            wl.append((k, v))
        return wl

    def _mark(self, tok, reads, writes, acc=False):
        k, v = tok
        for b in writes:
            if acc:
                b.ws.append(tok)
                continue
            b.last_w = tok
            b.ws = []
            b.readers = {}
        for b in reads:
            if b.readers.get(k, 0) < v:
                b.readers[k] = v

    def emit(self, e, fn, reads=(), writes=(), inc=True):
        wl = self._collect(e, reads, writes)
        if inc:
            self.cnt[e] += 1
            tok = (("eng", e), self.cnt[e])
        else:
            tok = (("eng", e), self.cnt[e] + 1)
        self.streams[e].append((wl, fn, ("eng", e) if inc else None, 1))
        self._mark(tok, reads, writes)
        return tok

    def dma(self, q, out, in_, reads=(), writes=(), is_output=False, acc=False):
        lst, i = self.dma_rot[q]
        k = lst[i % len(lst)]
        self.dma_rot[q][1] = i + 1
        wl = self._collect(q, reads, writes, acc)
        prev = self.dma_val[k]
        if prev > 0 and self.waited[q].get(k, 0) < prev:
            self.waited[q][k] = prev
            wl.append((k, prev))
        self.dma_val[k] = prev + 16
        tok = (k, prev + 16)
        self.streams[q].append((wl, lambda e: e.dma_start(out=out, in_=in_), k, 16))
        self._mark(tok, reads, writes, acc)
        if is_output:
            self.out_tokens.append(tok)
        return tok

    def finish(self):
        fin = {}
        for k, v in self.out_tokens:
            fin[k] = max(fin.get(k, 0), v)
        nc = self.nc
        sems = self.sems
        streams = self.streams
        emap = {"pe": "tensor", "act": "scalar", "dve": "vector", "pool": "gpsimd", "sp": "sync"}
        with nc.Block() as block:
            for e in ENGS:
                def body(eng, e=e):
                    for wl, fn, inc_k, inc_v in streams[e]:
                        for k, v in wl:
                            eng.wait_ge(sems[k], v)
                        ins = fn(eng)
                        if inc_k is not None:
                            ins.then_inc(sems[inc_k], inc_v)
                    if e == "sp":
                        for k, v in fin.items():
                            eng.wait_ge(sems[k], v)
                getattr(block, emap[e])(body)
        for cm in reversed(self.ctx):
            cm.__exit__(None, None, None)
        self.ctx = []


def new_nc():
    return bass.Bass("TRN2", target_bir_lowering=False)


def din(nc, name, shape, dt=F32):
    return nc.dram_tensor(name, list(shape), dt, kind="ExternalInput").ap()


def dout(nc, name, shape, dt=F32):
    return nc.dram_tensor(name, list(shape), dt, kind="ExternalOutput").ap()


def run(nc, in_maps):
    res = run_bass_kernel_spmd(nc, in_maps, core_ids=list(range(NCORES)))
    return res.results


def fm(v):
    v = np.asarray(v)
    return np.ascontiguousarray(v.reshape(-1, 128).T)


NMC = 12


def build_p0():
    nc = new_nc()
    cond = din(nc, "cond", [128, KC, 2])
    adaw = din(nc, "adaw", [2, D, NMC * 128])
    adab = din(nc, "adab", [128, 2, NMC])
    o = dout(nc, "mod", [128, 2, NMC, 2])
    P = Prog(nc)
    cs = P.sbuf("cs", [128, KC, 2], F32)
    cb = P.sbuf("cb", [128, KC, 2], BF16)
    bs = P.sbuf("bs", [128, 2, NMC], F32)
    ob = P.sbuf("ob", [128, 2, NMC, 2], F32)
    wb = [P.sbuf("w%d" % l, [128, KC, NMC * 128], BF16) for l in range(2)]
    ps = P.psum("ps", [128, 2, NMC, 2], F32)
    P.dma("sp", cs[:], cond, writes=[cs])
    P.dma("sp", bs[:], adab, writes=[bs])
    for l in range(2):
        for h in range(2):
            P.dma("pool", wb[l][:, h * 8:(h + 1) * 8, :],
                  adaw[l, h * 1024:(h + 1) * 1024, :].rearrange("(kc p) n -> p kc n", p=128), writes=[wb[l]], acc=(h > 0))
    P.emit("act", lambda e: e.activation(out=cb[:], in_=cs[:], func=AF.Silu), reads=[cs], writes=[cb])
    for l in range(2):
        for j in range(NMC):
            for kc in range(KC):
                P.emit("pe", lambda e, l=l, j=j, kc=kc: e.matmul(
                    ps[:, l, j, :], lhsT=wb[l][:, kc, j * 128:(j + 1) * 128], rhs=cb[:, kc, :],
                    start=(kc == 0), stop=(kc == KC - 1)), reads=[wb[l], cb], writes=[ps],
                    inc=(kc == KC - 1 and j == NMC - 1))
        P.emit("dve", lambda e, l=l: e.tensor_tensor(
            out=ob[:, l], in0=ps[:, l], in1=bs[:, l].unsqueeze(2).to_broadcast([128, NMC, 2]), op=ALU.add),
            reads=[ps, bs], writes=[ob])
    P.dma("sp", o, ob[:], reads=[ob], is_output=True)
    P.finish()
    return nc


def run_p0(inp):
    nc = build_p0()
    cond = np.stack([fm(inp["c"][0]), fm(inp["c_ctx"])], axis=-1).astype(np.float32)
    maps = []
    for i in range(NCORES):
        sl = slice(i * NMC * 128, (i + 1) * NMC * 128)
        adab = np.stack([fm(inp["ada_b"][l, sl]) for l in range(2)], axis=1)
        maps.append({"cond": cond, "adaw": np.ascontiguousarray(inp["ada_w"][:, :, sl]),
                     "adab": np.ascontiguousarray(adab)})
    res = run(nc, maps)
    full = np.concatenate([r["mod"] for r in res], axis=2)
    return full


CHUNKS = [(0, 512), (512, 512), (1024, 512), (1536, 512), (2048, CPC)]
SUBCH = [(i * 128, 128) for i in range(16)] + [(2048, CPC)]


def build_p1():
    nc = new_nc()
    xT = din(nc, "xT", [D, TT])
    w = din(nc, "w", [D, INC])
    modv = din(nc, "modv", [128, 2, 2, KC])
    gain = din(nc, "gain", [128, KC])
    bgate = din(nc, "bgate", [128, 32])
    qkg = din(nc, "qkg", [128, 2])
    cosd = din(nc, "cosT", [128, TT])
    sind = din(nc, "sinT", [128, TT])
    rmd = din(nc, "rm", [128, 128], BF16)
    qT = dout(nc, "qT", [QW, TT], BF16)
    kT = dout(nc, "kT", [KVW, TT], BF16)
    vo = dout(nc, "v", [TT, KVW], BF16)
    fT = dout(nc, "fT", [FD, TT], BF16)
    gT = dout(nc, "gT", [2 * D, TT], BF16)
    P = Prog(nc)
    hT = P.sbuf("hT", [128, KC, TT], BF16)
    cosb = P.sbuf("cosb", [128, TT], F32)
    sinb = P.sbuf("sinb", [128, TT], F32)
    modb = P.sbuf("modb", [128, 2, 2, KC], F32)
    gb = P.sbuf("gb", [128, KC], F32)
    ab = P.sbuf("ab", [128, 2, KC], F32)
    bgb = P.sbuf("bgb", [128, 32], F32)
    qkb = P.sbuf("qkb", [128, 2], F32)
    qks = P.sbuf("qks", [128, 2], F32)
    rmb = P.sbuf("rmb", [128, 128], BF16)
    ones = P.sbuf("ones", [128, 128], BF16)
    xs_rot = P.rot("xs", 2, [128, KC, 128], F32)
    sq_rot = P.rot("sqc", 2, [128, KC, 128], BF16)
    rs_rot = P.rot("rs", 3, [128, 3, 512], F32)
    wrot = P.rot("wp", 3, [128, KC, 512], BF16)
    psm = P.rot("psm", 3, [128, 512], F32, psum=True)
    pss = P.rot("pss", 2, [128, 512], F32, psum=True)
    psr = P.rot("psr", 2, [128, 512], F32, psum=True)
    sqh = P.rot("sqh", 2, [128, 512], BF16)
    qn_rot = P.rot("qn", 3, [128, 512], BF16)
    t1_rot = P.rot("t1", 2, [128, 512], F32)
    t2_rot = P.rot("t2", 2, [128, 512], F32)
    ob_rot = P.rot("ob", 4, [128, 512], BF16)

    for (dst, src) in ((cosb, cosd), (sinb, sind), (modb, modv), (gb, gain), (bgb, bgate), (qkb, qkg), (rmb, rmd)):
        P.dma("sp", dst[:], src, writes=[dst])
    P.emit("pool", lambda e: e.memset(ones[:], 1.0), writes=[ones])
    epsb = P.sbuf("epsb", [128, 1], F32)
    P.emit("pool", lambda e: e.memset(epsb[:], float(HD * EPS)), writes=[epsb])
    for r in rs_rot.bufs:
        P.emit("pool", lambda e, r=r: e.memset(r[:, 2, :], -0.5), writes=[r])
    for wi in range(2):
        P.emit("dve", lambda e, wi=wi: e.tensor_scalar(out=ab[:, wi, :], in0=modb[:, wi, 1, :], scalar1=1.0,
                                                       scalar2=float(math.sqrt(D)), op0=ALU.add, op1=ALU.mult),
               reads=[modb], writes=[ab])
        P.emit("dve", lambda e, wi=wi: e.tensor_tensor(out=ab[:, wi, :], in0=ab[:, wi, :], in1=gb[:], op=ALU.mult),
               reads=[ab, gb], writes=[ab])
    P.emit("dve", lambda e: e.tensor_scalar(out=qks[:], in0=qkb[:], scalar1=float(math.sqrt(HD)), scalar2=None,
                                            op0=ALU.mult), reads=[qkb], writes=[qks])

    class V:
        pass
    a_lat = ab.t[:, 0, :]
    a_ctx = ab.t[:, 1, :]
    b_lat = modb.t[:, 0, 0, :]
    b_ctx = modb.t[:, 1, 0, :]

    for (t0, tn) in SUBCH:
        isctx = t0 >= TPC
        a, b = (a_ctx, b_ctx) if isctx else (a_lat, b_lat)
        xs = xs_rot.next()
        P.dma("sp", xs[:, :, 0:tn], xT[:, t0:t0 + tn].rearrange("(kc p) t -> p kc t", p=128), writes=[xs])
        sq = sq_rot.next()
        P.emit("act", lambda e, sq=sq, xs=xs, tn=tn: e.activation(out=sq[:, :, 0:tn], in_=xs[:, :, 0:tn], func=AF.Square),
               reads=[xs], writes=[sq])
        ps_ = pss.next()
        for kc in range(KC):
            P.emit("pe", lambda e, ps_=ps_, sq=sq, kc=kc, tn=tn: e.matmul(
                ps_[:, 0:tn], lhsT=ones[:], rhs=sq[:, kc, 0:tn], start=(kc == 0), stop=(kc == KC - 1)),
                reads=[sq, ones], writes=[ps_], inc=(kc == KC - 1))
        rs = rs_rot.next()
        P.emit("dve", lambda e, rs=rs, ps_=ps_, tn=tn: e.tensor_scalar(
            out=rs[:, 0, 0:tn], in0=ps_[:, 0:tn], scalar1=float(D * EPS), scalar2=None, op0=ALU.add),
            reads=[ps_], writes=[rs])
        P.emit("pool", lambda e, rs=rs, tn=tn: e.tensor_tensor(
            out=rs[:, 1, 0:tn], in0=rs[:, 0, 0:tn], in1=rs[:, 2, 0:tn], op=ALU.pow), reads=[rs], writes=[rs])
        for kc in range(KC):
            eng = "dve" if kc % 2 == 0 else "pool"
            P.emit(eng, lambda e, kc=kc, rs=rs, xs=xs, tn=tn: e.tensor_tensor(
                out=xs[:, kc, 0:tn], in0=xs[:, kc, 0:tn], in1=rs[:, 1, 0:tn], op=ALU.mult), reads=[xs, rs], writes=[xs])
            P.emit("act", lambda e, xs=xs, kc=kc, a=a, b=b, t0=t0, tn=tn: e.activation(
                out=hT[:, kc, t0:t0 + tn], in_=xs[:, kc, 0:tn], func=AF.Identity,
                bias=b[:, kc:kc + 1], scale=a[:, kc:kc + 1]), reads=[xs, ab, modb], writes=[hT])

    pending = []

    def flush(n_keep):
        while len(pending) > n_keep:
            st = pending.pop(0)
            st()

    def head_epilogue(ps_, j, t0, tn, is_q):
        gcol = 0 if is_q else 1
        st = {}

        def stageA():
            sq = sqh.next()
            st["sq"] = sq
            P.emit("act", lambda e: e.activation(out=sq[:, 0:tn], in_=ps_[:, 0:tn], func=AF.Square),
                   reads=[ps_], writes=[sq])

        def stageB():
            sq = st["sq"]
            p2 = pss.next()
            P.emit("pe", lambda e: e.matmul(p2[:, 0:tn], lhsT=ones[:], rhs=sq[:, 0:tn], start=True, stop=True),
                   reads=[sq, ones], writes=[p2])
            rs = rs_rot.next()
            P.emit("act", lambda e: e.activation(out=rs[:, 0, 0:tn], in_=p2[:, 0:tn], func=AF.Sqrt, bias=epsb[:, 0:1]),
                   reads=[p2, epsb], writes=[rs])
            P.emit("dve", lambda e: e.reciprocal(out=rs[:, 1, 0:tn], in_=rs[:, 0, 0:tn]), reads=[rs], writes=[rs])
            qn = qn_rot.next()
            st["qn"] = qn
            P.emit("dve", lambda e: e.scalar_tensor_tensor(
                out=qn[:, 0:tn], in0=ps_[:, 0:tn], scalar=qks[:, gcol:gcol + 1], in1=rs[:, 1, 0:tn],
                op0=ALU.mult, op1=ALU.mult), reads=[ps_, qks, rs], writes=[qn])

        def stageC():
            qn = st["qn"]
            p3 = psr.next()
            P.emit("pe", lambda e: e.matmul(p3[:, 0:tn], lhsT=rmb[:], rhs=qn[:, 0:tn], start=True, stop=True),
                   reads=[qn, rmb], writes=[p3])
            t1 = t1_rot.next()
            P.emit("dve", lambda e: e.tensor_tensor(out=t1[:, 0:tn], in0=qn[:, 0:tn], in1=cosb[:, t0:t0 + tn],
                                                    op=ALU.mult), reads=[qn, cosb], writes=[t1])
            t2 = t2_rot.next()
            P.emit("dve", lambda e: e.tensor_tensor(out=t2[:, 0:tn], in0=p3[:, 0:tn], in1=sinb[:, t0:t0 + tn],
                                                    op=ALU.mult), reads=[p3, sinb], writes=[t2])
            ob = ob_rot.next()
            P.emit("dve", lambda e: e.tensor_tensor(out=ob[:, 0:tn], in0=t1[:, 0:tn], in1=t2[:, 0:tn], op=ALU.add),
                   reads=[t1, t2], writes=[ob])
            dst = qT[j * 128:(j + 1) * 128, t0:t0 + tn] if is_q else kT[(j - 16) * 128:(j - 15) * 128, t0:t0 + tn]
            P.dma("sp", dst, ob[:, 0:tn], reads=[ob], is_output=True)

        stageA()
        pending.append(stageB)
        pending.append(stageC)

    for pn in range(16):
        wbuf = wrot.next()
        for h in range(2):
            P.dma("pool", wbuf[:, h * 8:(h + 1) * 8, :],
                  w[h * 1024:(h + 1) * 1024, pn * 512:(pn + 1) * 512].rearrange("(kc p) n -> p kc n", p=128),
                  writes=[wbuf], acc=(h > 0))
        if pn == 5:
            tiles = [(i * 128, 128) for i in range(16)] + [(2048, CPC)]
            for (t0, tn) in tiles:
                ps_ = psm.next()
                for kc in range(KC):
                    P.emit("pe", lambda e, ps_=ps_, kc=kc, t0=t0, tn=tn, wbuf=wbuf: e.matmul(
                        ps_[0:tn, :], lhsT=hT[:, kc, t0:t0 + tn], rhs=wbuf[:, kc, :], start=(kc == 0), stop=(kc == KC - 1)),
                        reads=[hT, wbuf], writes=[ps_], inc=(kc == KC - 1))
                ob = ob_rot.next()
                P.emit("act", lambda e, ob=ob, ps_=ps_, tn=tn: e.activation(out=ob[0:tn, :], in_=ps_[0:tn, :], func=AF.Copy),
                       reads=[ps_], writes=[ob])
                P.dma("sp", vo[t0:t0 + tn, :], ob[0:tn, :], reads=[ob], is_output=True)
                flush(1)
            continue
        for jj in range(4):
            j = pn * 4 + jj
            for (t0, tn) in CHUNKS:
                ps_ = psm.next()
                for kc in range(KC):
                    P.emit("pe", lambda e, ps_=ps_, kc=kc, t0=t0, tn=tn, wbuf=wbuf, jj=jj: e.matmul(
                        ps_[:, 0:tn], lhsT=wbuf[:, kc, jj * 128:(jj + 1) * 128], rhs=hT[:, kc, t0:t0 + tn],
                        start=(kc == 0), stop=(kc == KC - 1)),
                        reads=[hT, wbuf], writes=[ps_], inc=(kc == KC - 1))
                flush(1)
                if j < 20:
                    head_epilogue(ps_, j, t0, tn, j < 16)
                elif j < 32:
                    ob = ob_rot.next()
                    P.emit("act", lambda e, ob=ob, ps_=ps_, tn=tn: e.activation(out=ob[:, 0:tn], in_=ps_[:, 0:tn], func=AF.Copy),
                           reads=[ps_], writes=[ob])
                    P.dma("sp", fT[(j - 24) * 128:(j - 23) * 128, t0:t0 + tn], ob[:, 0:tn], reads=[ob], is_output=True)
                else:
                    g = j - 32
                    ob = ob_rot.next()
                    P.emit("act", lambda e, ob=ob, ps_=ps_, tn=tn, g=g: e.activation(
                        out=ob[:, 0:tn], in_=ps_[:, 0:tn], func=AF.Sigmoid, bias=bgb[:, g:g + 1]),
                        reads=[ps_, bgb], writes=[ob])
                    P.dma("sp", gT[g * 128:(g + 1) * 128, t0:t0 + tn], ob[:, 0:tn], reads=[ob], is_output=True)
    flush(0)
    P.finish()
    return nc


def rope_tables():
    t = np.arange(SEQ)
    row = (t // GRID_W).astype(np.float32)
    col = (t % GRID_W).astype(np.float32)
    inv = (10000.0 ** (-np.arange(32, dtype=np.float32) / 32)).astype(np.float32)
    ar = row[:, None] * inv
    ac = col[:, None] * inv
    ang = np.concatenate([ar, ar, ac, ac], axis=-1)
    return np.cos(ang).T.astype(np.float32), np.sin(ang).T.astype(np.float32)


def rot_matrix():
    R = np.zeros((128, 128), np.float32)
    for base in (0, 64):
        for i in range(32):
            R[base + 32 + i, base + i] = -1.0
            R[base + i, base + 32 + i] = 1.0
    return R.astype(NPBF)


def run_p1(nc, l, inp, xT_cores, mod):
    cosT, sinT = rope_tables()
    rm = rot_matrix()
    maps = []
    modv = np.stack([np.stack([mod[:, l, 0:16, wi], mod[:, l, 16:32, wi]], axis=1) for wi in range(2)], axis=1)
    modv = np.ascontiguousarray(modv.astype(np.float32))
    for i in range(NCORES):
        cs = np.concatenate([cosT[:, i * TPC:(i + 1) * TPC], np.ones((128, CPC), np.float32)], axis=1)
        sn = np.concatenate([sinT[:, i * TPC:(i + 1) * TPC], np.zeros((128, CPC), np.float32)], axis=1)
        maps.append({
            "xT": xT_cores[i], "w": inp["w_in"][l], "modv": modv, "gain": fm(inp["norm_attn_g"][l]),
            "bgate": fm(inp["b_gate"][l]),
            "qkg": np.ascontiguousarray(np.stack([inp["q_norm_g"][l], inp["k_norm_g"][l]], axis=1)),
            "cosT": np.ascontiguousarray(cs), "sinT": np.ascontiguousarray(sn), "rm": rm})
    return run(nc, maps)


NKEY = CTX + SEQ
NKC = NKEY // 128
ATTN_SCALE = HD ** -0.5


def build_p2(with_ctx):
    nc = new_nc()
    nq = SEQ + (CTX if with_ctx else 0)
    qT = din(nc, "qT", [2, 128, nq], BF16)
    kT = din(nc, "kT", [128, NKEY], BF16)
    v = din(nc, "v", [NKEY, 128], BF16)
    oT = dout(nc, "oT", [2, 128, nq], BF16)
    P = Prog(nc)
    kb = P.sbuf("kb", [128, NKEY], BF16)
    vb = P.sbuf("vb", [128, NKC, 128], BF16)
    ones = P.sbuf("ones", [128, 128], BF16)
    qrot = P.rot("qc", 3, [128, 512], BF16)
    prot = P.rot("pb", 4, [128, 512], BF16)
    rrot = P.rot("ri", 2, [128, 512], F32)
    orot = P.rot("ob", 2, [128, 512], BF16)
    psS = P.rot("psS", 4, [128, 512], F32, psum=True)
    psO = P.rot("psO", 2, [128, 512], F32, psum=True)
    psL = P.rot("psL", 2, [128, 512], F32, psum=True)
    hlrot = P.rot("hl", 2, [128, 2, 512], BF16)

    class _AccRot:
        def __init__(self):
            self.items = []
            for i in range(2):
                b0 = P.sbuf("acc%d" % i, [128, 2, 512], F32)
                b1 = Buf(b0.t, "acc%db" % i)
                self.items.append((b0, b1))
            self.i = 0

        def next(self):
            it = self.items[self.i % 2]
            self.i += 1
            return it
    accrot = _AccRot()
    P.emit("pool", lambda e: e.memset(ones[:], 1.0), writes=[ones])
    for h in range(4):
        c0, c1 = h * 4160, (h + 1) * 4160
        P.dma("sp", kb[:, c0:c1], kT[:, c0:c1], writes=[kb], acc=(h > 0))
    for h in range(5):
        c0, c1 = h * 26, (h + 1) * 26
        P.dma("sp", vb[:, c0:c1, :], v[c0 * 128:c1 * 128, :].rearrange("(c p) d -> p c d", p=128), writes=[vb], acc=(h > 0))
    qchunks = [(i * 512, 512, NKC) for i in range(SEQ // 512)]
    if with_ctx:
        qchunks.append((SEQ, CTX, CTX // 128))
    def do_chunk(hh, t0, tn, nk):
        qc = qrot.next()
        P.dma("sp", qc[:, 0:tn], qT[hh, :, t0:t0 + tn], writes=[qc])
        po = psO.next()
        pl = psL.next()
        accb = accrot.next()
        acc = accb[0].t
        accs = accb
        sbufs = {}

        def S(kc):
            ps = psS.next()
            sbufs[kc] = ps
            P.emit("pe", lambda e: e.matmul(ps[:, 0:tn], lhsT=kb[:, kc * 128:(kc + 1) * 128],
                                            rhs=qc[:, 0:tn], start=True, stop=True),
                   reads=[kb, qc], writes=[ps])

        def step(kc):
            ps = sbufs.pop(kc)
            pb = prot.next()
            P.emit("act", lambda e: e.activation(out=pb[:, 0:tn], in_=ps[:, 0:tn], func=AF.Exp,
                                                 scale=float(ATTN_SCALE)), reads=[ps], writes=[pb])
            P.emit("pe", lambda e: e.matmul(po[:, 0:tn], lhsT=vb[:, kc, :], rhs=pb[:, 0:tn],
                                            start=(kc == 0), stop=(kc == nk - 1)),
                   reads=[vb, pb], writes=[po], inc=(kc == nk - 1))
            par = kc % 2
            eng = "dve" if par == 0 else "pool"
            if kc < 2:
                P.emit(eng, lambda e: e.tensor_copy(out=acc[:, par, 0:tn], in_=pb[:, 0:tn]), reads=[pb], writes=[accs[par]])
            else:
                P.emit(eng, lambda e: e.tensor_tensor(out=acc[:, par, 0:tn], in0=acc[:, par, 0:tn], in1=pb[:, 0:tn], op=ALU.add),
                       reads=[pb, accs[par]], writes=[accs[par]])
        S(0)
        for kc in range(nk):
            if kc + 1 < nk:
                S(kc + 1)
            step(kc)
        P.emit("dve", lambda e: e.tensor_tensor(out=acc[:, 0, 0:tn], in0=acc[:, 0, 0:tn], in1=acc[:, 1, 0:tn], op=ALU.add),
               reads=[accs[0], accs[1]], writes=[accs[0]])
        hl = hlrot.next()
        P.emit("dve", lambda e: e.tensor_copy(out=hl[:, 0, 0:tn], in_=acc[:, 0, 0:tn]), reads=[accs[0]], writes=[hl])
        P.emit("dve", lambda e: e.tensor_tensor(out=hl[:, 1, 0:tn], in0=acc[:, 0, 0:tn], in1=hl[:, 0, 0:tn], op=ALU.subtract),
               reads=[accs[0], hl], writes=[hl])
        P.emit("pe", lambda e: e.matmul(pl[:, 0:tn], lhsT=ones[:], rhs=hl[:, 0, 0:tn], start=True, stop=False),
               reads=[ones, hl], writes=[pl], inc=False)
        P.emit("pe", lambda e: e.matmul(pl[:, 0:tn], lhsT=ones[:], rhs=hl[:, 1, 0:tn], start=False, stop=True),
               reads=[ones, hl], writes=[pl])
        ri = rrot.next()
        P.emit("dve", lambda e: e.reciprocal(out=ri[:, 0:tn], in_=pl[:, 0:tn]), reads=[pl], writes=[ri])
        ob = orot.next()
        P.emit("dve", lambda e: e.tensor_tensor(out=ob[:, 0:tn], in0=po[:, 0:tn], in1=ri[:, 0:tn],
                                                op=ALU.mult), reads=[po, ri], writes=[ob])
        P.dma("sp", oT[hh, :, t0:t0 + tn], ob[:, 0:tn], reads=[ob], is_output=True)

    for hh in range(2):
        for (t0, tn, nk) in qchunks:
            do_chunk(hh, t0, tn, nk)
    P.finish()
    return nc


def run_p2(nc, with_ctx, qT_full, kT_full, v_full):
    maps = []
    for i in range(NCORES):
        kv = i // 2
        maps.append({"qT": np.ascontiguousarray(qT_full[i * 256:(i + 1) * 256].reshape(2, 128, -1)),
                     "kT": np.ascontiguousarray(kT_full[kv * 128:(kv + 1) * 128]),
                     "v": np.ascontiguousarray(v_full[:, kv * 128:(kv + 1) * 128])})
    res = run(nc, maps)
    return np.concatenate([r["oT"].reshape(256, -1) for r in res], axis=0)


def build_p3a():
    nc = new_nc()
    fT = din(nc, "fT", [256, SEQ], BF16)
    ccs_d = din(nc, "ccs", [128, 2, 256], BF16)
    YT = dout(nc, "YT", [2, 128, SEQ], BF16)
    P = Prog(nc)
    fb = P.sbuf("fb", [128, 2, SEQ], BF16)
    ccs = P.sbuf("ccs_s", [128, 2, 256], BF16)
    yb = P.sbuf("yb", [128, 2, SEQ], BF16)
    psr = P.rot("ps", 4, [128, 512], F32, psum=True)
    P.dma("sp", ccs[:], ccs_d, writes=[ccs])
    for cc in range(2):
        for h in range(4):
            P.dma("sp", fb[:, cc, h * 4096:(h + 1) * 4096], fT[cc * 128:(cc + 1) * 128, h * 4096:(h + 1) * 4096],
                  writes=[fb], acc=(cc + h > 0))
    n = 0
    for tc in range(SEQ // 512):
        for ri in range(2):
            ps = psr.next()
            for cc in range(2):
                P.emit("pe", lambda e, ps=ps, tc=tc, ri=ri, cc=cc: e.matmul(
                    ps[:], lhsT=ccs[:, cc, ri * 128:(ri + 1) * 128], rhs=fb[:, cc, tc * 512:(tc + 1) * 512],
                    start=(cc == 0), stop=(cc == 1)), reads=[ccs, fb], writes=[ps], inc=(cc == 1))
            if n % 2 == 0:
                P.emit("act", lambda e, ps=ps, tc=tc, ri=ri: e.activation(out=yb[:, ri, tc * 512:(tc + 1) * 512], in_=ps[:], func=AF.Copy),
                       reads=[ps], writes=[yb])
            else:
                P.emit("dve", lambda e, ps=ps, tc=tc, ri=ri: e.tensor_copy(out=yb[:, ri, tc * 512:(tc + 1) * 512], in_=ps[:]),
                       reads=[ps], writes=[yb])
            n += 1
    for ri in range(2):
        for h in range(4):
            P.dma("sp", YT[ri, :, h * 4096:(h + 1) * 4096], yb[:, ri, h * 4096:(h + 1) * 4096], reads=[yb], is_output=True)
    P.finish()
    return nc


def build_p3b(with_ctx):
    nc = new_nc()
    Yd = din(nc, "Y", [2, 128, SEQ], BF16)
    fc_d = din(nc, "fc", [256, CTX], BF16)
    ccs_d = din(nc, "ccs", [128, 2, 256], BF16)
    w1_d = din(nc, "w1", [128, 2, 256], BF16)
    tw_d = din(nc, "tw", [128, 2, 512])
    w2_d = din(nc, "w2", [128, 2, 128], BF16)
    w256_d = din(nc, "w256", [128, 2, 2, 256], BF16)
    Fo = dout(nc, "Fo", [128, SEQ], BF16)
    Fc = dout(nc, "Fc", [128, CTX], BF16)
    P = Prog(nc)
    fb = P.sbuf("fb", [128, 2, SEQ], BF16)
    yr = P.sbuf("yr", [128, SEQ], BF16)
    yi = P.sbuf("yi", [128, SEQ], BF16)
    ccs = P.sbuf("ccs_s", [128, 2, 256], BF16)
    w1 = P.sbuf("w1_s", [128, 2, 256], BF16)
    tw = P.sbuf("tw_s", [128, 2, 512], F32)
    w2 = P.sbuf("w2_s", [128, 2, 128], BF16)
    w256 = P.sbuf("w256_s", [128, 2, 2, 256], BF16)
    fcb = P.sbuf("fcb", [128, 2, CTX], BF16)
    ycb = P.sbuf("ycb", [128, 2, 256], BF16)
    ocb = P.sbuf("ocb", [128, CTX], BF16)
    arot = P.rot("A", 2, [128, 512], F32)
    brot = P.rot("B", 2, [128, 512], F32)
    psr = P.rot("ps", 4, [128, 512], F32, psum=True)
    for (dst, src) in ((ccs, ccs_d), (w1, w1_d), (tw, tw_d), (w2, w2_d), (w256, w256_d)):
        P.dma("sp", dst[:], src, writes=[dst])
    for h in range(4):
        P.dma("sp", yr[:, h * 4096:(h + 1) * 4096], Yd[0, :, h * 4096:(h + 1) * 4096], writes=[yr], acc=(h > 0))
    for h in range(4):
        P.dma("sp", yi[:, h * 4096:(h + 1) * 4096], Yd[1, :, h * 4096:(h + 1) * 4096], writes=[yi], acc=(h > 0))
    yrv = yr.t[:, :].rearrange("p (j n) -> p j n", n=128)
    yiv = yi.t[:, :].rearrange("p (j n) -> p j n", n=128)
    for jp in range(64):
        ps = psr.next()
        psv = ps.t[:, :].rearrange("p (a c) -> p a c", c=256)
        for q in range(2):
            j = jp * 2 + q
            P.emit("pe", lambda e, psv=psv, q=q, j=j: e.matmul(psv[:, q, :], lhsT=yrv[:, j, :], rhs=w1[:, 0, :],
                                                               start=True, stop=False), reads=[yr, w1], writes=[ps], inc=False)
            P.emit("pe", lambda e, psv=psv, q=q, j=j: e.matmul(psv[:, q, :], lhsT=yiv[:, j, :], rhs=w1[:, 1, :],
                                                               start=False, stop=True), reads=[yi, w1], writes=[ps], inc=(q == 1))
        A = arot.next()
        B = brot.next()
        P.emit("dve", lambda e, A=A, ps=ps: e.tensor_tensor(out=A[:], in0=ps[:], in1=tw[:, 0, :], op=ALU.mult),
               reads=[ps, tw], writes=[A])
        P.emit("dve", lambda e, B=B, ps=ps: e.tensor_tensor(out=B[:], in0=ps[:], in1=tw[:, 1, :], op=ALU.mult),
               reads=[ps, tw], writes=[B])
        for q in range(2):
            o0 = jp * 256 + q * 128
            P.emit("pool", lambda e, A=A, B=B, q=q, o0=o0: e.tensor_tensor(
                out=fb[:, 0, o0:o0 + 128], in0=A[:, q * 256:q * 256 + 128], in1=B[:, q * 256 + 128:q * 256 + 256],
                op=ALU.subtract), reads=[A, B], writes=[fb])
            P.emit("pool", lambda e, A=A, B=B, q=q, o0=o0: e.tensor_tensor(
                out=fb[:, 1, o0:o0 + 128], in0=B[:, q * 256:q * 256 + 128], in1=A[:, q * 256 + 128:q * 256 + 256],
                op=ALU.add), reads=[A, B], writes=[fb])
    for cq in range(32):
        ps = psr.next()
        P.emit("pe", lambda e, ps=ps, cq=cq: e.matmul(ps[:], lhsT=w2[:, 0, :], rhs=fb[:, 0, cq * 512:(cq + 1) * 512],
                                                      start=True, stop=False), reads=[fb, w2], writes=[ps], inc=False)
        P.emit("pe", lambda e, ps=ps, cq=cq: e.matmul(ps[:], lhsT=w2[:, 1, :], rhs=fb[:, 1, cq * 512:(cq + 1) * 512],
                                                      start=False, stop=True), reads=[fb, w2], writes=[ps])
        if cq % 2 == 0:
            P.emit("act", lambda e, ps=ps, cq=cq: e.activation(out=yr[:, cq * 512:(cq + 1) * 512], in_=ps[:], func=AF.Copy,
                                                               scale=float(1.0 / 2048.0)), reads=[ps], writes=[yr])
        else:
            P.emit("dve", lambda e, ps=ps, cq=cq: e.tensor_scalar(out=yr[:, cq * 512:(cq + 1) * 512], in0=ps[:],
                                                                  scalar1=float(1.0 / 2048.0), scalar2=None, op0=ALU.mult),
                   reads=[ps], writes=[yr])
    for h in range(4):
        P.dma("sp", Fo[:, h * 4096:(h + 1) * 4096], yr[:, h * 4096:(h + 1) * 4096], reads=[yr], is_output=True)
    if with_ctx:
        for cc in range(2):
            P.dma("sp", fcb[:, cc, :], fc_d[cc * 128:(cc + 1) * 128, :], writes=[fcb], acc=(cc > 0))
        for tt in range(2):
            ps = psr.next()
            for cc in range(2):
                P.emit("pe", lambda e, ps=ps, tt=tt, cc=cc: e.matmul(
                    ps[:, 0:256], lhsT=fcb[:, cc, tt * 128:(tt + 1) * 128], rhs=ccs[:, cc, :], start=(cc == 0), stop=(cc == 1)),
                    reads=[fcb, ccs], writes=[ps], inc=(cc == 1))
            P.emit("act", lambda e, ps=ps, tt=tt: e.activation(out=ycb[:, tt, :], in_=ps[:, 0:256], func=AF.Copy),
                   reads=[ps], writes=[ycb])
        ps = psr.next()
        n = 0
        for tt in range(2):
            for ri in range(2):
                P.emit("pe", lambda e, ps=ps, tt=tt, ri=ri, n=n: e.matmul(
                    ps[:, 0:256], lhsT=ycb[:, tt, ri * 128:(ri + 1) * 128], rhs=w256[:, tt, ri, :], start=(n == 0), stop=(n == 3)),
                    reads=[ycb, w256], writes=[ps], inc=(n == 3))
                n += 1
        P.emit("act", lambda e, ps=ps: e.activation(out=ocb[:], in_=ps[:, 0:256], func=AF.Copy, scale=float(1.0 / 256.0)),
               reads=[ps], writes=[ocb])
    else:
        P.emit("pool", lambda e: e.memset(ocb[:], 0.0), writes=[ocb])
    P.dma("sp", Fc, ocb[:], reads=[ocb], is_output=True)
    P.finish()
    return nc


def p3_consts(half):
    p = np.arange(128)
    out = {}
    ccs = np.zeros((128, 2, 256), np.float64)
    j = 128 * half + np.arange(128)
    for cc in range(2):
        c = cc * 128 + p
        ang = 2 * np.pi * np.outer(c, j) / 256.0
        ccs[:, cc, 0:128] = np.cos(ang)
        ccs[:, cc, 128:256] = np.sin(ang)
    out["ccs"] = ccs.astype(NPBF)
    a128 = 2 * np.pi * np.outer(p, p) / 128.0
    C, S = np.cos(a128), np.sin(a128)
    w1 = np.zeros((128, 2, 256))
    w1[:, 0, 0:128] = C
    w1[:, 0, 128:256] = S
    w1[:, 1, 0:128] = -S
    w1[:, 1, 128:256] = C
    out["w1"] = w1.astype(NPBF)
    psi = 2 * np.pi * np.outer(p, p) / float(SEQ)
    tw = np.zeros((128, 2, 512))
    tw[:, 0, :] = np.tile(np.cos(psi), (1, 4))
    tw[:, 1, :] = np.tile(np.sin(psi), (1, 4))
    out["tw"] = tw.astype(np.float32)
    w2 = np.zeros((128, 2, 128))
    w2[:, 0] = C
    w2[:, 1] = -S
    out["w2"] = w2.astype(NPBF)
    w256 = np.zeros((128, 2, 2, 256))
    for tt in range(2):
        n = tt * 128 + p
        ang = 2 * np.pi * np.outer(n, np.arange(256)) / 256.0
        w256[:, tt, 0] = np.cos(ang)
        w256[:, tt, 1] = -np.sin(ang)
    out["w256"] = w256.astype(NPBF)
    out["ident"] = np.eye(128).astype(NPBF)
    return out


def run_p3(nca, ncb, fT_full):
    consts = [p3_consts(h) for h in range(2)]
    maps = []
    for i in range(NCORES):
        g, half = i // 2, i % 2
        maps.append({"fT": np.ascontiguousarray(fT_full[g * 256:(g + 1) * 256, :SEQ]), "ccs": consts[half]["ccs"]})
    ra = run(nca, maps)
    maps = []
    for i in range(NCORES):
        g, half = i // 2, i % 2
        c = consts[half]
        YT = ra[i]["YT"]
        Y = np.ascontiguousarray(YT.reshape(2, 128, 128, 128).transpose(0, 2, 1, 3)).reshape(2, 128, SEQ)
        maps.append({"Y": Y, "fc": np.ascontiguousarray(fT_full[g * 256:(g + 1) * 256, SEQ:]), "ccs": c["ccs"], "w1": c["w1"],
                     "tw": c["tw"], "w2": c["w2"], "w256": c["w256"]})
    rb = run(ncb, maps)
    outs = []
    for i in range(NCORES):
        Fo = rb[i]["Fo"].reshape(128, 128, 128)
        lat = np.ascontiguousarray(Fo.transpose(1, 0, 2)).reshape(128, SEQ)
        outs.append(np.concatenate([lat, rb[i]["Fc"]], axis=1))
    return np.concatenate(outs, axis=0)


DBG = {}


def build_p4(moe, with_ctx, last):
    nc = new_nc()
    ntok = TT if with_ctx else TPC
    xT = din(nc, "xT", [D, ntok])
    at_d = din(nc, "attnT", [QW, ntok], BF16)
    ft_d = din(nc, "FT", [FD, ntok], BF16)
    gt_d = din(nc, "gT", [2 * D, ntok], BF16)
    wp = din(nc, "wp", [QW, D])
    wf = din(nc, "wf", [FD, D])
    wo = din(nc, "wo", [D, D])
    if moe:
        rw_d = din(nc, "rw", [128, KC, 8])
        mg = din(nc, "mg", [NE, D, DFE])
        mu = din(nc, "mu", [NE, D, DFE])
        md = din(nc, "md", [NE, DFE, D])
        id_d = din(nc, "ident", [128, 128], BF16)
    else:
        wg = din(nc, "wg", [D, DFF])
        wu = din(nc, "wu", [D, DFF])
        wd = din(nc, "wd", [DFF, D])
    modv = din(nc, "modv", [128, 2, 4, KC])
    gain2 = din(nc, "gain2", [128, KC])
    fgain = din(nc, "fgain", [128, KC])
    xo = dout(nc, "xo", [D, ntok])
    P = Prog(nc)
    xs = P.sbuf("xs", [128, KC, 512], F32)
    atb = P.sbuf("atb", [128, KC, 512], BF16)
    fg = P.sbuf("fg", [128, (40 if moe else 44) * 512], BF16)
    yT = P.sbuf("yT", [128, KC, 512], BF16)
    ones = P.sbuf("ones", [128, 128], BF16)
    modb = P.sbuf("modb", [128, 2, 4, KC], F32)
    g2b = P.sbuf("g2b", [128, KC], F32)
    fgb = P.sbuf("fgb", [128, KC], F32)
    a2b = P.sbuf("a2b", [128, 2, KC], F32)
    wrot = P.rot("wb", 3, [128, 8192], BF16)
    t1r = P.rot("t1", 2, [128, 512], F32)
    t2r = P.rot("t2", 2, [128, 512], F32)
    sr = P.rot("sl", 2, [128, 512], F32)
    rsr = P.rot("rs", 2, [128, 3, 512], F32)
    psm = P.rot("psm", 6, [128, 512], F32, psum=True)
    pss = P.rot("pss", 2, [128, 512], F32, psum=True)
    ftv = fg.t[:, 0:8 * 512].rearrange("p (k t) -> p k t", t=512)
    gtv = fg.t[:, 8 * 512:40 * 512].rearrange("p (k t) -> p k t", t=512)
    aTv = fg.t[:, :].rearrange("p (k t) -> p k t", t=512)
    for (dst, src) in ((modb, modv), (g2b, gain2), (fgb, fgain)):
        P.dma("sp", dst[:], src, writes=[dst])
    P.emit("pool", lambda e: e.memset(ones[:], 1.0), writes=[ones])
    for r in rsr.bufs:
        P.emit("pool", lambda e, r=r: e.memset(r[:, 2, :], -0.5), writes=[r])
    for wi in range(2):
        P.emit("dve", lambda e, wi=wi: e.tensor_scalar(out=a2b[:, wi, :], in0=modb[:, wi, 2, :], scalar1=1.0,
                                                       scalar2=float(math.sqrt(D)), op0=ALU.add, op1=ALU.mult),
               reads=[modb], writes=[a2b])
        P.emit("dve", lambda e, wi=wi: e.tensor_tensor(out=a2b[:, wi, :], in0=a2b[:, wi, :], in1=g2b[:], op=ALU.mult),
               reads=[a2b, g2b], writes=[a2b])
    P.emit("dve", lambda e: e.tensor_scalar(out=fgb[:], in0=fgb[:], scalar1=float(math.sqrt(D)), scalar2=None, op0=ALU.mult),
           reads=[fgb], writes=[fgb])
    if moe:
        rwf = P.sbuf("rwf", [128, KC, 8], F32)
        rwb = P.sbuf("rwb", [128, KC, 16], BF16)
        ident = P.sbuf("ident_s", [128, 128], BF16)
        bcs = P.sbuf("bcs", [128, NE, 512], F32)
        l16 = P.sbuf("l16", [128, 16], F32)
        sm = P.sbuf("sm", [128, 8, 8], F32)
        sc1 = P.sbuf("sc1", [128, 8], F32)
        rep = P.rot("rep", 2, [128, 2, NE, 128], BF16)
        u2r = P.rot("u2", 2, [128, 512], F32)
        P.dma("sp", rwf[:], rw_d, writes=[rwf])
        P.dma("sp", ident[:], id_d, writes=[ident])
        P.emit("act", lambda e: e.activation(out=rwb[:, :, 0:8], in_=rwf[:], func=AF.Copy), reads=[rwf], writes=[rwb])
        P.emit("dve", lambda e: e.tensor_tensor(out=rwb[:, :, 8:16], in0=rwf[:], in1=rwb[:, :, 0:8], op=ALU.subtract),
               reads=[rwf, rwb], writes=[rwb])

    def load_w(src2d, k0, nkc, n0, ncols):
        wb = wrot.next()
        view = wb.t[:, 0:nkc * ncols].rearrange("p (k n) -> p k n", n=ncols)
        first = True
        for h0 in range(0, nkc, 8):
            h1 = min(nkc, h0 + 8)
            P.dma("pool", view[:, h0:h1, :],
                  src2d[k0 + h0 * 128:k0 + h1 * 128, n0:n0 + ncols].rearrange("(kc p) n -> p kc n", p=128),
                  writes=[wb], acc=(not first))
            first = False
        return wb, view

    def mm_group(ps, tn, parts, inc_last=True):
        n = len(parts)
        for i, (wbuf, lhsT, rbuf, rhs) in enumerate(parts):
            P.emit("pe", lambda e, lhsT=lhsT, rhs=rhs, i=i: e.matmul(ps[:, 0:tn], lhsT=lhsT, rhs=rhs, start=(i == 0), stop=(i == n - 1)),
                   reads=[wbuf, rbuf], writes=[ps], inc=(inc_last and i == n - 1))

    def resid(ps, j, tn, gcol, wi):
        P.emit("dve", lambda e: e.scalar_tensor_tensor(out=xs[:, j, 0:tn], in0=ps[:, 0:tn], scalar=modb[:, wi, gcol, j:j + 1],
                                                       in1=xs[:, j, 0:tn], op0=ALU.mult, op1=ALU.add),
               reads=[ps, modb, xs], writes=[xs])

    def rstd_of_xs(tn):
        P.emit("act", lambda e: e.activation(out=yT[:, :, 0:tn], in_=xs[:, :, 0:tn], func=AF.Square), reads=[xs], writes=[yT])
        ps_ = pss.next()
        mm_group(ps_, tn, [(ones, ones[:], yT, yT[:, kc, 0:tn]) for kc in range(KC)])
        rs = rsr.next()
        P.emit("dve", lambda e: e.tensor_scalar(out=rs[:, 0, 0:tn], in0=ps_[:, 0:tn], scalar1=float(D * EPS), scalar2=None,
                                                op0=ALU.add), reads=[ps_], writes=[rs])
        P.emit("pool", lambda e: e.tensor_tensor(out=rs[:, 1, 0:tn], in0=rs[:, 0, 0:tn], in1=rs[:, 2, 0:tn], op=ALU.pow),
               reads=[rs], writes=[rs])
        return rs

    def ffn_gu(c, tn, wgb, wgv, wub, wuv, jj, bc_e=None):
        psG = psm.next()
        mm_group(psG, tn, [(wgb, wgv[:, kc, jj * 128:(jj + 1) * 128], atb, atb[:, kc, 0:tn]) for kc in range(KC)])
        psU = psm.next()
        mm_group(psU, tn, [(wub, wuv[:, kc, jj * 128:(jj + 1) * 128], atb, atb[:, kc, 0:tn]) for kc in range(KC)])
        s_ = sr.next()
        P.emit("act", lambda e: e.activation(out=s_[:, 0:tn], in_=psG[:, 0:tn], func=AF.Silu), reads=[psG], writes=[s_])
        if bc_e is None:
            P.emit("dve", lambda e: e.tensor_tensor(out=aTv[:, c, 0:tn], in0=s_[:, 0:tn], in1=psU[:, 0:tn], op=ALU.mult),
                   reads=[s_, psU], writes=[fg])
        else:
            u2 = u2r.next()
            P.emit("dve", lambda e: e.tensor_tensor(out=u2[:, 0:tn], in0=psU[:, 0:tn], in1=bcs[:, bc_e, 0:tn], op=ALU.mult),
                   reads=[psU, bcs], writes=[u2])
            P.emit("dve", lambda e: e.tensor_tensor(out=aTv[:, c, 0:tn], in0=s_[:, 0:tn], in1=u2[:, 0:tn], op=ALU.mult),
                   reads=[s_, u2], writes=[fg])

    def router_tile(tt, rstage=9):
        psR = pss.next()
        parts = [(atb, atb[:, kc, tt * 128:(tt + 1) * 128], rwb, rwb[:, kc, 0:16]) for kc in range(KC)]
        n = len(parts)
        for i, (wbuf, lhsT, rbuf, rhs) in enumerate(parts):
            P.emit("pe", lambda e, lhsT=lhsT, rhs=rhs, i=i: e.matmul(psR[:, 0:16], lhsT=lhsT, rhs=rhs, start=(i == 0), stop=False),
                   reads=[wbuf, rbuf], writes=[psR], inc=False)
        for kc in range(KC):
            P.emit("pe", lambda e, kc=kc: e.matmul(psR[:, 0:8], lhsT=yT[:, kc, tt * 128:(tt + 1) * 128], rhs=rwb[:, kc, 0:8],
                                                   start=False, stop=(kc == KC - 1)),
                   reads=[yT, rwb], writes=[psR], inc=(kc == KC - 1))
        P.emit("dve", lambda e: e.tensor_copy(out=l16[:], in_=psR[:, 0:16]), reads=[psR], writes=[l16])
        if rstage < 1:
            if tt == 0:
                P.emit("pool", lambda e: e.memset(bcs[:], 0.5), writes=[bcs])
            return
        lg, m1, mk1, l2, m2, mk2, dd = (sm[:, 0, :], sm[:, 1, 0:1], sm[:, 2, :], sm[:, 3, :], sm[:, 1, 1:2], sm[:, 4, :], sm[:, 1, 2:3])
        ee, den, w1, w2 = sm[:, 1, 3:4], sm[:, 1, 4:5], sm[:, 1, 5:6], sm[:, 1, 6:7]
        comb = sm[:, 5, :]
        D_ = lambda fn: P.emit("dve", fn, reads=[sm, l16], writes=[sm])
        D_(lambda e: e.tensor_tensor(out=lg, in0=l16[:, 0:8], in1=l16[:, 8:16], op=ALU.add))
        D_(lambda e: e.reduce_max(out=m1, in_=lg, axis=AX.X))
        D_(lambda e: e.tensor_scalar(out=mk1, in0=lg, scalar1=m1, scalar2=None, op0=ALU.is_equal))
        D_(lambda e: e.scalar_tensor_tensor(out=l2, in0=mk1, scalar=-1e30, in1=lg, op0=ALU.mult, op1=ALU.add))
        D_(lambda e: e.reduce_max(out=m2, in_=l2, axis=AX.X))
        D_(lambda e: e.tensor_scalar(out=mk2, in0=l2, scalar1=m2, scalar2=None, op0=ALU.is_equal))
        D_(lambda e: e.tensor_tensor(out=dd, in0=m2, in1=m1, op=ALU.subtract))
        P.emit("act", lambda e: e.activation(out=ee, in_=dd, func=AF.Exp), reads=[sm], writes=[sm])
        D_(lambda e: e.tensor_scalar(out=den, in0=ee, scalar1=1.0, scalar2=None, op0=ALU.add))
        D_(lambda e: e.reciprocal(out=w1, in_=den))
        D_(lambda e: e.tensor_tensor(out=w2, in0=ee, in1=w1, op=ALU.mult))
        D_(lambda e: e.tensor_scalar(out=comb, in0=mk1, scalar1=w1, scalar2=None, op0=ALU.mult))
        D_(lambda e: e.scalar_tensor_tensor(out=comb, in0=mk2, scalar=w2, in1=comb, op0=ALU.mult, op1=ALU.add))
        if rstage < 2:
            if tt == 0:
                P.emit("pool", lambda e: e.memset(bcs[:], 0.5), writes=[bcs])
            return
        rp = rep.next()
        cb = comb.unsqueeze(2).to_broadcast([128, NE, 128])
        P.emit("dve", lambda e: e.tensor_copy(out=rp[:, 0], in_=cb), reads=[sm], writes=[rp])
        P.emit("dve", lambda e: e.tensor_tensor(out=rp[:, 1], in0=cb, in1=rp[:, 0], op=ALU.subtract), reads=[sm, rp], writes=[rp])
        if rstage < 3:
            if tt == 0:
                P.emit("pool", lambda e: e.memset(bcs[:], 0.5), writes=[bcs])
            return
        for e_ in range(NE):
            def bc_one(e_=e_):
                pb_ = psm.next()
                P.emit("pe", lambda e: e.matmul(pb_[:, 0:128], lhsT=rp[:, 0, e_, :], rhs=ident[:], start=True, stop=False),
                       reads=[rp, ident], writes=[pb_], inc=False)
                P.emit("pe", lambda e: e.matmul(pb_[:, 0:128], lhsT=rp[:, 1, e_, :], rhs=ident[:], start=False, stop=True),
                       reads=[rp, ident], writes=[pb_])
                if e_ % 2 == 0:
                    P.emit("act", lambda e: e.activation(out=bcs[:, e_, tt * 128:(tt + 1) * 128], in_=pb_[:, 0:128], func=AF.Copy),
                           reads=[pb_], writes=[bcs])
                else:
                    P.emit("dve", lambda e: e.tensor_copy(out=bcs[:, e_, tt * 128:(tt + 1) * 128], in_=pb_[:, 0:128]),
                           reads=[pb_], writes=[bcs])
            bc_one()

    def do_chunk(t0, tn, wi):
        P.dma("sp", xs[:, :, 0:tn], xT[:, t0:t0 + tn].rearrange("(kc p) t -> p kc t", p=128), writes=[xs])
        for h in range(2):
            P.dma("sp", atb[:, h * 8:(h + 1) * 8, 0:tn], at_d[h * 1024:(h + 1) * 1024, t0:t0 + tn].rearrange("(kc p) t -> p kc t", p=128),
                  writes=[atb], acc=(h > 0))
        P.dma("sp", ftv[:, :, 0:tn], ft_d[:, t0:t0 + tn].rearrange("(kc p) t -> p kc t", p=128), writes=[fg])
        for h in range(4):
            P.dma("sp", gtv[:, h * 8:(h + 1) * 8, 0:tn], gt_d[h * 1024:(h + 1) * 1024, t0:t0 + tn].rearrange("(kc p) t -> p kc t", p=128),
                  writes=[fg], acc=True)
        for pn in range(4):
            wpb, wpv = load_w(wp, 0, KC, pn * 512, 512)
            wfb, wfv = load_w(wf, 0, 8, pn * 512, 512)
            for jj in range(4):
                j = pn * 4 + jj

                def ya(j=j, jj=jj, wpb=wpb, wpv=wpv, wfb=wfb, wfv=wfv):
                    psA = psm.next()
                    mm_group(psA, tn, [(wpb, wpv[:, kc, jj * 128:(jj + 1) * 128], atb, atb[:, kc, 0:tn]) for kc in range(KC)])
                    psB = psm.next()
                    mm_group(psB, tn, [(wfb, wfv[:, kc, jj * 128:(jj + 1) * 128], fg, ftv[:, kc, 0:tn]) for kc in range(8)])
                    t1 = t1r.next()
                    t2 = t2r.next()
                    P.emit("dve", lambda e: e.tensor_tensor(out=t1[:, 0:tn], in0=psA[:, 0:tn], in1=gtv[:, j, 0:tn], op=ALU.mult),
                           reads=[psA, fg], writes=[t1])
                    P.emit("dve", lambda e: e.tensor_tensor(out=t2[:, 0:tn], in0=psB[:, 0:tn], in1=gtv[:, 16 + j, 0:tn], op=ALU.mult),
                           reads=[psB, fg], writes=[t2])
                    P.emit("dve", lambda e: e.tensor_tensor(out=yT[:, j, 0:tn], in0=t1[:, 0:tn], in1=t2[:, 0:tn], op=ALU.add),
                           reads=[t1, t2], writes=[yT])
                ya()
        for pn in range(4):
            wob, wov = load_w(wo, 0, KC, pn * 512, 512)
            for jj in range(4):
                j = pn * 4 + jj
                psZ = psm.next()
                mm_group(psZ, tn, [(wob, wov[:, kc, jj * 128:(jj + 1) * 128], yT, yT[:, kc, 0:tn]) for kc in range(KC)])
                resid(psZ, j, tn, 0, wi)
        rs = rstd_of_xs(tn)
        for kc in range(KC):
            def hk(kc=kc):
                t1 = t1r.next()
                P.emit("dve", lambda e: e.tensor_tensor(out=t1[:, 0:tn], in0=xs[:, kc, 0:tn], in1=rs[:, 1, 0:tn],
                                                                                   op=ALU.mult), reads=[xs, rs], writes=[t1])
                if not moe:
                    P.emit("act", lambda e: e.activation(out=atb[:, kc, 0:tn], in_=t1[:, 0:tn], func=AF.Identity,
                                                         bias=modb[:, wi, 1, kc:kc + 1], scale=a2b[:, wi, kc:kc + 1]),
                           reads=[t1, modb, a2b], writes=[atb])
                else:
                    t2 = t2r.next()
                    P.emit("act", lambda e: e.activation(out=t2[:, 0:tn], in_=t1[:, 0:tn], func=AF.Identity,
                                                         bias=modb[:, wi, 1, kc:kc + 1], scale=a2b[:, wi, kc:kc + 1]),
                           reads=[t1, modb, a2b], writes=[t2])
                    P.emit("dve", lambda e: e.tensor_copy(out=atb[:, kc, 0:tn], in_=t2[:, 0:tn]), reads=[t2], writes=[atb])
                    P.emit("dve", lambda e: e.tensor_tensor(out=yT[:, kc, 0:tn], in0=t2[:, 0:tn], in1=atb[:, kc, 0:tn], op=ALU.subtract),
                           reads=[t2, atb], writes=[yT])
            hk()
        if not moe:
            for pn in range(DFF // 512):
                wgb, wgv = load_w(wg, 0, KC, pn * 512, 512)
                wub, wuv = load_w(wu, 0, KC, pn * 512, 512)
                for jj in range(4):
                    ffn_gu(pn * 4 + jj, tn, wgb, wgv, wub, wuv, jj)
            for np_ in range(8):
                wab, wav = load_w(wd, 0, 22, np_ * 256, 256)
                wbb, wbv = load_w(wd, 22 * 128, 22, np_ * 256, 256)
                for jj in range(2):
                    j = np_ * 2 + jj
                    psD = psm.next()
                    parts = [(wab, wav[:, c, jj * 128:(jj + 1) * 128], fg, aTv[:, c, 0:tn]) for c in range(22)]
                    parts += [(wbb, wbv[:, c, jj * 128:(jj + 1) * 128], fg, aTv[:, 22 + c, 0:tn]) for c in range(22)]
                    mm_group(psD, tn, parts)
                    resid(psD, j, tn, 3, wi)
        else:
            if DBG.get("norouter"):
                P.emit("pool", lambda e: e.memset(bcs[:], 0.5), writes=[bcs])
            else:
                for tt in range(tn // 128):
                    router_tile(tt, DBG.get("rstage", 9))
            for e_ in range(DBG.get("nexp", NE)):
                for pn in range(6):
                    ncols = 512 if pn < 5 else 256
                    wgb, wgv = load_w(mg[e_], 0, KC, pn * 512, ncols)
                    wub, wuv = load_w(mu[e_], 0, KC, pn * 512, ncols)
                    for jj in range(ncols // 128):
                        ffn_gu(pn * 4 + jj, tn, wgb, wgv, wub, wuv, jj, bc_e=e_)
                for np_ in range(8):
                    wab, wav = load_w(md[e_], 0, 22, np_ * 256, 256)
                    for jj in range(2):
                        j = np_ * 2 + jj
                        psD = psm.next()
                        mm_group(psD, tn, [(wab, wav[:, c, jj * 128:(jj + 1) * 128], fg, aTv[:, c, 0:tn]) for c in range(22)])
                        resid(psD, j, tn, 3, wi)
        if last:
            rs2 = rstd_of_xs(tn)
            for kc in range(KC):
                P.emit("dve", lambda e, kc=kc: e.scalar_tensor_tensor(out=xs[:, kc, 0:tn], in0=xs[:, kc, 0:tn], scalar=fgb[:, kc:kc + 1],
                                                                      in1=rs2[:, 1, 0:tn], op0=ALU.mult, op1=ALU.mult),
                       reads=[xs, fgb, rs2], writes=[xs])
        P.dma("sp", xo[:, t0:t0 + tn].rearrange("(kc p) t -> p kc t", p=128), xs[:, :, 0:tn], reads=[xs], is_output=True)

    for (t0, tn) in CHUNKS[:DBG.get("nchunk", 9)]:
        if t0 >= TPC and not with_ctx:
            continue
        do_chunk(t0, tn, 1 if t0 >= TPC else 0)
    P.finish()
    return nc


def run_p4(nc, l, moe, with_ctx, inp, xT_cores, attnT_cores, FT_cores, gT_cores, mod):
    modv = np.stack([np.stack([mod[:, l, 32:48, wi], mod[:, l, 48:64, wi], mod[:, l, 64:80, wi], mod[:, l, 80:96, wi]], axis=1)
                     for wi in range(2)], axis=1)
    modv = np.ascontiguousarray(modv.astype(np.float32))
    maps = []
    for i in range(NCORES):
        m = {"xT": xT_cores[i], "attnT": attnT_cores[i], "FT": FT_cores[i], "gT": gT_cores[i],
             "wp": inp["w_attn_proj"][l], "wf": inp["w_four_proj"][l], "wo": inp["w_out"][l],
             "modv": modv, "gain2": fm(inp["norm_ffn_g"][l]), "fgain": fm(inp["final_norm_g"])}
        if moe:
            li = l // 2
            m["rw"] = np.ascontiguousarray(inp["router_w"][li].reshape(KC, 128, NE).transpose(1, 0, 2))
            m["mg"] = inp["moe_w_gate"][li]
            m["mu"] = inp["moe_w_up"][li]
            m["md"] = inp["moe_w_down"][li]
            m["ident"] = np.eye(128).astype(NPBF)
        else:
            li = l // 2
            m["wg"] = inp["ffn_w_gate"][li]
            m["wu"] = inp["ffn_w_up"][li]
            m["wd"] = inp["ffn_w_down"][li]
        maps.append(m)
    res = run(nc, maps)
    return [r["xo"] for r in res]


def kernel(**inp):
    inp = {k: np.asarray(v) for k, v in inp.items()}
    mod = run_p0(inp)
    x = inp["x"][0]
    ctx = inp["ctx"][0]
    xT_cores = [np.ascontiguousarray(np.concatenate([x[i * TPC:(i + 1) * TPC].T, ctx[i * CPC:(i + 1) * CPC].T], axis=1))
                for i in range(NCORES)]
    out = None
    for l in range(2):
        with_ctx = (l == 0)
        moe = (l % 2 == 1)
        last = (l == 1)
        r1 = run_p1(build_p1(), l, inp, xT_cores, mod)

        def gather(name, axis_tok):
            lat = np.concatenate([np.take(r[name], range(0, TPC), axis=axis_tok) for r in r1], axis=axis_tok)
            cx = np.concatenate([np.take(r[name], range(TPC, TT), axis=axis_tok) for r in r1], axis=axis_tok)
            return lat, cx
        q_lat, q_ctx = gather("qT", 1)
        k_lat, k_ctx = gather("kT", 1)
        v_lat, v_ctx = gather("v", 0)
        f_lat, f_ctx = gather("fT", 1)
        q_full = np.concatenate([q_lat, q_ctx], axis=1) if with_ctx else q_lat
        kT_full = np.concatenate([k_ctx, k_lat], axis=1)
        v_full = np.concatenate([v_ctx, v_lat], axis=0)
        oT = run_p2(build_p2(with_ctx), with_ctx, q_full, kT_full, v_full)
        fT_full = np.concatenate([f_lat, f_ctx], axis=1)
        FT = run_p3(build_p3a(), build_p3b(with_ctx), fT_full)

        def percore(a):
            res = []
            for i in range(NCORES):
                if with_ctx:
                    res.append(np.ascontiguousarray(np.concatenate(
                        [a[:, i * TPC:(i + 1) * TPC], a[:, SEQ + i * CPC:SEQ + (i + 1) * CPC]], axis=1)))
                else:
                    res.append(np.ascontiguousarray(a[:, i * TPC:(i + 1) * TPC]))
            return res
        attn_c = percore(oT)
        FT_c = percore(FT)
        ntok = TT if with_ctx else TPC
        g_c = [np.ascontiguousarray(r["gT"][:, :ntok]) for r in r1]
        x_c = [np.ascontiguousarray(xc[:, :ntok]) for xc in xT_cores]
        xo = run_p4(build_p4(moe, with_ctx, last), l, moe, with_ctx, inp, x_c, attn_c, FT_c, g_c, mod)
        if last:
            out = np.concatenate([xo[i][:, :TPC].T for i in range(NCORES)], axis=0)
        else:
            xT_cores = xo
    return np.ascontiguousarray(out.reshape(1, SEQ, D).astype(np.float32))
```

```python
import math
import numpy as np
import ml_dtypes
import concourse.bass as bass
import concourse.mybir as mybir
from concourse.bass_utils import run_bass_kernel_spmd

F32 = mybir.dt.float32
BF16 = mybir.dt.bfloat16
ALU = mybir.AluOpType
AF = mybir.ActivationFunctionType
AX = mybir.AxisListType
NPBF = ml_dtypes.bfloat16

NCORES = 8
D = 2048
KC = 16
SEQ = 16384
CTX = 256
TPC = SEQ // NCORES
CPC = CTX // NCORES
TT = TPC + CPC
NH, NKV, HD = 16, 4, 128
QW, KVW, FD = 2048, 512, 1024
INC = 8192
DFF = 5632
NE = 8
DFE = 2816
EPS = 1e-6
GRID_W = 64
ENGS = ("pe", "act", "dve", "pool", "sp")


class Buf:
    __slots__ = ("t", "last_w", "readers", "name", "ws")

    def __init__(self, t, name=""):
        self.t = t
        self.last_w = None
        self.ws = []
        self.readers = {}
        self.name = name

    def __getitem__(self, idx):
        return self.t[idx]


class Rot:
    def __init__(self, bufs):
        self.bufs = bufs
        self.i = 0

    def next(self):
        b = self.bufs[self.i % len(self.bufs)]
        self.i += 1
        return b


class Prog:
    def __init__(self, nc, same_engine_sync=True, n_dma_sems=8):
        self.nc = nc
        self.streams = {e: [] for e in ENGS}
        self.cnt = {e: 0 for e in ENGS}
        self.waited = {e: {} for e in ENGS}
        self.same_engine_sync = same_engine_sync
        self.sems = {}
        self.ctx = []
        for e in ENGS:
            self.sems[("eng", e)] = self._sem("s_" + e)
        self.dma_rot = {}
        self.dma_val = {}
        for q in ("sp", "act", "pool"):
            lst = []
            for i in range(n_dma_sems):
                k = ("dma", q + str(i))
                self.sems[k] = self._sem("d_%s%d" % (q, i))
                self.dma_val[k] = 0
                lst.append(k)
            self.dma_rot[q] = [lst, 0]
        self.out_tokens = []

    def _sem(self, name):
        cm = self.nc.semaphore(name)
        s = cm.__enter__()
        self.ctx.append(cm)
        return s

    def sbuf(self, name, shape, dtype):
        cm = self.nc.sbuf_tensor(name, shape, dtype)
        t = cm.__enter__()
        self.ctx.append(cm)
        return Buf(t, name)

    def psum(self, name, shape, dtype=F32):
        cm = self.nc.psum_tensor(name, shape, dtype)
        t = cm.__enter__()
        self.ctx.append(cm)
        return Buf(t, name)

    def rot(self, name, n, shape, dtype, psum=False):
        return Rot([(self.psum if psum else self.sbuf)("%s%d" % (name, i), shape, dtype) for i in range(n)])

    def _collect(self, e, reads, writes, acc=False):
        waits = {}

        def need(tok):
            if tok is None:
                return
            k, v = tok
            if k == ("eng", e):
                if e == "pe" or not self.same_engine_sync:
                    return
            if waits.get(k, 0) < v:
                waits[k] = v

        for b in reads:
            need(b.last_w)
            for t_ in b.ws:
                need(t_)
        for b in writes:
            need(b.last_w)
            for t_ in b.ws:
                if acc and t_[0][0] == "dma":
                    continue
                need(t_)
            for k, v in b.readers.items():
                if k == ("eng", e):
                    continue
                need((k, v))
        wl = []
        for k, v in waits.items():
            if self.waited[e].get(k, 0) >= v:
                continue
            self.waited[e][k] = v
            wl.append((k, v))
        return wl

    def _mark(self, tok, reads, writes, acc=False):
        k, v = tok
        for b in writes:
            if acc:
                b.ws.append(tok)
                continue
            b.last_w = tok
            b.ws = []
            b.readers = {}
        for b in reads:
            if b.readers.get(k, 0) < v:
                b.readers[k] = v

    def emit(self, e, fn, reads=(), writes=(), inc=True):
        wl = self._collect(e, reads, writes)
        if inc:
            self.cnt[e] += 1
            tok = (("eng", e), self.cnt[e])
        else:
            tok = (("eng", e), self.cnt[e] + 1)
        self.streams[e].append((wl, fn, ("eng", e) if inc else None, 1))
        self._mark(tok, reads, writes)
        return tok

    def dma(self, q, out, in_, reads=(), writes=(), is_output=False, acc=False):
        lst, i = self.dma_rot[q]
        k = lst[i % len(lst)]
        self.dma_rot[q][1] = i + 1
        wl = self._collect(q, reads, writes, acc)
        prev = self.dma_val[k]
        if prev > 0 and self.waited[q].get(k, 0) < prev:
            self.waited[q][k] = prev
            wl.append((k, prev))
        self.dma_val[k] = prev + 16
        tok = (k, prev + 16)
        self.streams[q].append((wl, lambda e: e.dma_start(out=out, in_=in_), k, 16))
        self._mark(tok, reads, writes, acc)
        if is_output:
            self.out_tokens.append(tok)
        return tok

    def finish(self):
        fin = {}
        for k, v in self.out_tokens:
            fin[k] = max(fin.get(k, 0), v)
        nc = self.nc
        sems = self.sems
        streams = self.streams
        emap = {"pe": "tensor", "act": "scalar", "dve": "vector", "pool": "gpsimd", "sp": "sync"}
        with nc.Block() as block:
            for e in ENGS:
                def body(eng, e=e):
                    for wl, fn, inc_k, inc_v in streams[e]:
                        for k, v in wl:
                            eng.wait_ge(sems[k], v)
                        ins = fn(eng)
                        if inc_k is not None:
                            ins.then_inc(sems[inc_k], inc_v)
                    if e == "sp":
                        for k, v in fin.items():
                            eng.wait_ge(sems[k], v)
                getattr(block, emap[e])(body)
        for cm in reversed(self.ctx):
            cm.__exit__(None, None, None)
        self.ctx = []


def new_nc():
    return bass.Bass("TRN2", target_bir_lowering=False)


def din(nc, name, shape, dt=F32):
    return nc.dram_tensor(name, list(shape), dt, kind="ExternalInput").ap()


def dout(nc, name, shape, dt=F32):
    return nc.dram_tensor(name, list(shape), dt, kind="ExternalOutput").ap()


def run(nc, in_maps):
    res = run_bass_kernel_spmd(nc, in_maps, core_ids=list(range(NCORES)))
    return res.results


def fm(v):
    v = np.asarray(v)
    return np.ascontiguousarray(v.reshape(-1, 128).T)


NMC = 12


def build_p0():
    nc = new_nc()
    cond = din(nc, "cond", [128, KC, 2])
    adaw = din(nc, "adaw", [2, D, NMC * 128])
    adab = din(nc, "adab", [128, 2, NMC])
    o = dout(nc, "mod", [128, 2, NMC, 2])
    P = Prog(nc)
    cs = P.sbuf("cs", [128, KC, 2], F32)
    cb = P.sbuf("cb", [128, KC, 2], BF16)
    bs = P.sbuf("bs", [128, 2, NMC], F32)
    ob = P.sbuf("ob", [128, 2, NMC, 2], F32)
    wb = [P.sbuf("w%d" % l, [128, KC, NMC * 128], BF16) for l in range(2)]
    ps = P.psum("ps", [128, 2, NMC, 2], F32)
    P.dma("sp", cs[:], cond, writes=[cs])
    P.dma("sp", bs[:], adab, writes=[bs])
    for l in range(2):
        for h in range(2):
            P.dma("pool", wb[l][:, h * 8:(h + 1) * 8, :],
                  adaw[l, h * 1024:(h + 1) * 1024, :].rearrange("(kc p) n -> p kc n", p=128), writes=[wb[l]], acc=(h > 0))
    P.emit("act", lambda e: e.activation(out=cb[:], in_=cs[:], func=AF.Silu), reads=[cs], writes=[cb])
    for l in range(2):
        for j in range(NMC):
            for kc in range(KC):
                P.emit("pe", lambda e, l=l, j=j, kc=kc: e.matmul(
                    ps[:, l, j, :], lhsT=wb[l][:, kc, j * 128:(j + 1) * 128], rhs=cb[:, kc, :],
                    start=(kc == 0), stop=(kc == KC - 1)), reads=[wb[l], cb], writes=[ps],
                    inc=(kc == KC - 1 and j == NMC - 1))
        P.emit("dve", lambda e, l=l: e.tensor_tensor(
            out=ob[:, l], in0=ps[:, l], in1=bs[:, l].unsqueeze(2).to_broadcast([128, NMC, 2]), op=ALU.add),
            reads=[ps, bs], writes=[ob])
    P.dma("sp", o, ob[:], reads=[ob], is_output=True)
    P.finish()
    return nc


def run_p0(inp):
    nc = build_p0()
    cond = np.stack([fm(inp["c"][0]), fm(inp["c_ctx"])], axis=-1).astype(np.float32)
    maps = []
    for i in range(NCORES):
        sl = slice(i * NMC * 128, (i + 1) * NMC * 128)
        adab = np.stack([fm(inp["ada_b"][l, sl]) for l in range(2)], axis=1)
        maps.append({"cond": cond, "adaw": np.ascontiguousarray(inp["ada_w"][:, :, sl]),
                     "adab": np.ascontiguousarray(adab)})
    res = run(nc, maps)
    full = np.concatenate([r["mod"] for r in res], axis=2)
    return full


CHUNKS = [(0, 512), (512, 512), (1024, 512), (1536, 512), (2048, CPC)]
SUBCH = [(i * 128, 128) for i in range(16)] + [(2048, CPC)]


def build_p1():
    nc = new_nc()
    xT = din(nc, "xT", [D, TT])
    w = din(nc, "w", [D, INC])
    modv = din(nc, "modv", [128, 2, 2, KC])
    gain = din(nc, "gain", [128, KC])
    bgate = din(nc, "bgate", [128, 32])
    qkg = din(nc, "qkg", [128, 2])
    cosd = din(nc, "cosT", [128, TT])
    sind = din(nc, "sinT", [128, TT])
    rmd = din(nc, "rm", [128, 128], BF16)
    qT = dout(nc, "qT", [QW, TT], BF16)
    kT = dout(nc, "kT", [KVW, TT], BF16)
    vo = dout(nc, "v", [TT, KVW], BF16)
    fT = dout(nc, "fT", [FD, TT], BF16)
    gT = dout(nc, "gT", [2 * D, TT], BF16)
    P = Prog(nc)
    hT = P.sbuf("hT", [128, KC, TT], BF16)
    cosb = P.sbuf("cosb", [128, TT], F32)
    sinb = P.sbuf("sinb", [128, TT], F32)
    modb = P.sbuf("modb", [128, 2, 2, KC], F32)
    gb = P.sbuf("gb", [128, KC], F32)
    ab = P.sbuf("ab", [128, 2, KC], F32)
    bgb = P.sbuf("bgb", [128, 32], F32)
    qkb = P.sbuf("qkb", [128, 2], F32)
    qks = P.sbuf("qks", [128, 2], F32)
    rmb = P.sbuf("rmb", [128, 128], BF16)
    ones = P.sbuf("ones", [128, 128], BF16)
    xs_rot = P.rot("xs", 2, [128, KC, 128], F32)
    sq_rot = P.rot("sqc", 2, [128, KC, 128], BF16)
    rs_rot = P.rot("rs", 3, [128, 3, 512], F32)
    wrot = P.rot("wp", 3, [128, KC, 512], BF16)
    psm = P.rot("psm", 3, [128, 512], F32, psum=True)
    pss = P.rot("pss", 2, [128, 512], F32, psum=True)
    psr = P.rot("psr", 2, [128, 512], F32, psum=True)
    sqh = P.rot("sqh", 2, [128, 512], BF16)
    qn_rot = P.rot("qn", 3, [128, 512], BF16)
    t1_rot = P.rot("t1", 2, [128, 512], F32)
    t2_rot = P.rot("t2", 2, [128, 512], F32)
    ob_rot = P.rot("ob", 4, [128, 512], BF16)

    for (dst, src) in ((cosb, cosd), (sinb, sind), (modb, modv), (gb, gain), (bgb, bgate), (qkb, qkg), (rmb, rmd)):
        P.dma("sp", dst[:], src, writes=[dst])
    P.emit("pool", lambda e: e.memset(ones[:], 1.0), writes=[ones])
    epsb = P.sbuf("epsb", [128, 1], F32)
    P.emit("pool", lambda e: e.memset(epsb[:], float(HD * EPS)), writes=[epsb])
    for r in rs_rot.bufs:
        P.emit("pool", lambda e, r=r: e.memset(r[:, 2, :], -0.5), writes=[r])
    for wi in range(2):
        P.emit("dve", lambda e, wi=wi: e.tensor_scalar(out=ab[:, wi, :], in0=modb[:, wi, 1, :], scalar1=1.0,
                                                       scalar2=float(math.sqrt(D)), op0=ALU.add, op1=ALU.mult),
               reads=[modb], writes=[ab])
        P.emit("dve", lambda e, wi=wi: e.tensor_tensor(out=ab[:, wi, :], in0=ab[:, wi, :], in1=gb[:], op=ALU.mult),
               reads=[ab, gb], writes=[ab])
    P.emit("dve", lambda e: e.tensor_scalar(out=qks[:], in0=qkb[:], scalar1=float(math.sqrt(HD)), scalar2=None,
                                            op0=ALU.mult), reads=[qkb], writes=[qks])

    class V:
        pass
    a_lat = ab.t[:, 0, :]
    a_ctx = ab.t[:, 1, :]
    b_lat = modb.t[:, 0, 0, :]
    b_ctx = modb.t[:, 1, 0, :]

    for (t0, tn) in SUBCH:
        isctx = t0 >= TPC
        a, b = (a_ctx, b_ctx) if isctx else (a_lat, b_lat)
        xs = xs_rot.next()
        P.dma("sp", xs[:, :, 0:tn], xT[:, t0:t0 + tn].rearrange("(kc p) t -> p kc t", p=128), writes=[xs])
        sq = sq_rot.next()
        P.emit("act", lambda e, sq=sq, xs=xs, tn=tn: e.activation(out=sq[:, :, 0:tn], in_=xs[:, :, 0:tn], func=AF.Square),
               reads=[xs], writes=[sq])
        ps_ = pss.next()
        for kc in range(KC):
            P.emit("pe", lambda e, ps_=ps_, sq=sq, kc=kc, tn=tn: e.matmul(
                ps_[:, 0:tn], lhsT=ones[:], rhs=sq[:, kc, 0:tn], start=(kc == 0), stop=(kc == KC - 1)),
                reads=[sq, ones], writes=[ps_], inc=(kc == KC - 1))
        rs = rs_rot.next()
        P.emit("dve", lambda e, rs=rs, ps_=ps_, tn=tn: e.tensor_scalar(
            out=rs[:, 0, 0:tn], in0=ps_[:, 0:tn], scalar1=float(D * EPS), scalar2=None, op0=ALU.add),
            reads=[ps_], writes=[rs])
        P.emit("pool", lambda e, rs=rs, tn=tn: e.tensor_tensor(
            out=rs[:, 1, 0:tn], in0=rs[:, 0, 0:tn], in1=rs[:, 2, 0:tn], op=ALU.pow), reads=[rs], writes=[rs])
        for kc in range(KC):
            eng = "dve" if kc % 2 == 0 else "pool"
            P.emit(eng, lambda e, kc=kc, rs=rs, xs=xs, tn=tn: e.tensor_tensor(
                out=xs[:, kc, 0:tn], in0=xs[:, kc, 0:tn], in1=rs[:, 1, 0:tn], op=ALU.mult), reads=[xs, rs], writes=[xs])
            P.emit("act", lambda e, xs=xs, kc=kc, a=a, b=b, t0=t0, tn=tn: e.activation(
                out=hT[:, kc, t0:t0 + tn], in_=xs[:, kc, 0:tn], func=AF.Identity,
                bias=b[:, kc:kc + 1], scale=a[:, kc:kc + 1]), reads=[xs, ab, modb], writes=[hT])

    pending = []

    def flush(n_keep):
        while len(pending) > n_keep:
            st = pending.pop(0)
            st()

    def head_epilogue(ps_, j, t0, tn, is_q):
        gcol = 0 if is_q else 1
        st = {}

        def stageA():
            sq = sqh.next()
            st["sq"] = sq
            P.emit("act", lambda e: e.activation(out=sq[:, 0:tn], in_=ps_[:, 0:tn], func=AF.Square),
                   reads=[ps_], writes=[sq])

        def stageB():
            sq = st["sq"]
            p2 = pss.next()
            P.emit("pe", lambda e: e.matmul(p2[:, 0:tn], lhsT=ones[:], rhs=sq[:, 0:tn], start=True, stop=True),
                   reads=[sq, ones], writes=[p2])
            rs = rs_rot.next()
            P.emit("act", lambda e: e.activation(out=rs[:, 0, 0:tn], in_=p2[:, 0:tn], func=AF.Sqrt, bias=epsb[:, 0:1]),
                   reads=[p2, epsb], writes=[rs])
            P.emit("dve", lambda e: e.reciprocal(out=rs[:, 1, 0:tn], in_=rs[:, 0, 0:tn]), reads=[rs], writes=[rs])
            qn = qn_rot.next()
            st["qn"] = qn
            P.emit("dve", lambda e: e.scalar_tensor_tensor(
                out=qn[:, 0:tn], in0=ps_[:, 0:tn], scalar=qks[:, gcol:gcol + 1], in1=rs[:, 1, 0:tn],
                op0=ALU.mult, op1=ALU.mult), reads=[ps_, qks, rs], writes=[qn])

        def stageC():
            qn = st["qn"]
            p3 = psr.next()
            P.emit("pe", lambda e: e.matmul(p3[:, 0:tn], lhsT=rmb[:], rhs=qn[:, 0:tn], start=True, stop=True),
                   reads=[qn, rmb], writes=[p3])
            t1 = t1_rot.next()
            P.emit("dve", lambda e: e.tensor_tensor(out=t1[:, 0:tn], in0=qn[:, 0:tn], in1=cosb[:, t0:t0 + tn],
                                                    op=ALU.mult), reads=[qn, cosb], writes=[t1])
            t2 = t2_rot.next()
            P.emit("dve", lambda e: e.tensor_tensor(out=t2[:, 0:tn], in0=p3[:, 0:tn], in1=sinb[:, t0:t0 + tn],
                                                    op=ALU.mult), reads=[p3, sinb], writes=[t2])
            ob = ob_rot.next()
            P.emit("dve", lambda e: e.tensor_tensor(out=ob[:, 0:tn], in0=t1[:, 0:tn], in1=t2[:, 0:tn], op=ALU.add),
                   reads=[t1, t2], writes=[ob])
            dst = qT[j * 128:(j + 1) * 128, t0:t0 + tn] if is_q else kT[(j - 16) * 128:(j - 15) * 128, t0:t0 + tn]
            P.dma("sp", dst, ob[:, 0:tn], reads=[ob], is_output=True)

        stageA()
        pending.append(stageB)
        pending.append(stageC)

    for pn in range(16):
        wbuf = wrot.next()
        for h in range(2):
            P.dma("pool", wbuf[:, h * 8:(h + 1) * 8, :],
                  w[h * 1024:(h + 1) * 1024, pn * 512:(pn + 1) * 512].rearrange("(kc p) n -> p kc n", p=128),
                  writes=[wbuf], acc=(h > 0))
        if pn == 5:
            tiles = [(i * 128, 128) for i in range(16)] + [(2048, CPC)]
            for (t0, tn) in tiles:
                ps_ = psm.next()
                for kc in range(KC):
                    P.emit("pe", lambda e, ps_=ps_, kc=kc, t0=t0, tn=tn, wbuf=wbuf: e.matmul(
                        ps_[0:tn, :], lhsT=hT[:, kc, t0:t0 + tn], rhs=wbuf[:, kc, :], start=(kc == 0), stop=(kc == KC - 1)),
                        reads=[hT, wbuf], writes=[ps_], inc=(kc == KC - 1))
                ob = ob_rot.next()
                P.emit("act", lambda e, ob=ob, ps_=ps_, tn=tn: e.activation(out=ob[0:tn, :], in_=ps_[0:tn, :], func=AF.Copy),
                       reads=[ps_], writes=[ob])
                P.dma("sp", vo[t0:t0 + tn, :], ob[0:tn, :], reads=[ob], is_output=True)
                flush(1)
            continue
        for jj in range(4):
            j = pn * 4 + jj
            for (t0, tn) in CHUNKS:
                ps_ = psm.next()
                for kc in range(KC):
                    P.emit("pe", lambda e, ps_=ps_, kc=kc, t0=t0, tn=tn, wbuf=wbuf, jj=jj: e.matmul(
                        ps_[:, 0:tn], lhsT=wbuf[:, kc, jj * 128:(jj + 1) * 128], rhs=hT[:, kc, t0:t0 + tn],
                        start=(kc == 0), stop=(kc == KC - 1)),
                        reads=[hT, wbuf], writes=[ps_], inc=(kc == KC - 1))
                flush(1)
                if j < 20:
                    head_epilogue(ps_, j, t0, tn, j < 16)
                elif j < 32:
                    ob = ob_rot.next()
                    P.emit("act", lambda e, ob=ob, ps_=ps_, tn=tn: e.activation(out=ob[:, 0:tn], in_=ps_[:, 0:tn], func=AF.Copy),
                           reads=[ps_], writes=[ob])
                    P.dma("sp", fT[(j - 24) * 128:(j - 23) * 128, t0:t0 + tn], ob[:, 0:tn], reads=[ob], is_output=True)
                else:
                    g = j - 32
                    ob = ob_rot.next()
                    P.emit("act", lambda e, ob=ob, ps_=ps_, tn=tn, g=g: e.activation(
                        out=ob[:, 0:tn], in_=ps_[:, 0:tn], func=AF.Sigmoid, bias=bgb[:, g:g + 1]),
                        reads=[ps_, bgb], writes=[ob])
                    P.dma("sp", gT[g * 128:(g + 1) * 128, t0:t0 + tn], ob[:, 0:tn], reads=[ob], is_output=True)
    flush(0)
    P.finish()
    return nc


def rope_tables():
    t = np.arange(SEQ)
    row = (t // GRID_W).astype(np.float32)
    col = (t % GRID_W).astype(np.float32)
    inv = (10000.0 ** (-np.arange(32, dtype=np.float32) / 32)).astype(np.float32)
    ar = row[:, None] * inv
    ac = col[:, None] * inv
    ang = np.concatenate([ar, ar, ac, ac], axis=-1)
    return np.cos(ang).T.astype(np.float32), np.sin(ang).T.astype(np.float32)


def rot_matrix():
    R = np.zeros((128, 128), np.float32)
    for base in (0, 64):
        for i in range(32):
            R[base + 32 + i, base + i] = -1.0
            R[base + i, base + 32 + i] = 1.0
    return R.astype(NPBF)


def run_p1(nc, l, inp, xT_cores, mod):
    cosT, sinT = rope_tables()
    rm = rot_matrix()
    maps = []
    modv = np.stack([np.stack([mod[:, l, 0:16, wi], mod[:, l, 16:32, wi]], axis=1) for wi in range(2)], axis=1)
    modv = np.ascontiguousarray(modv.astype(np.float32))
    for i in range(NCORES):
        cs = np.concatenate([cosT[:, i * TPC:(i + 1) * TPC], np.ones((128, CPC), np.float32)], axis=1)
        sn = np.concatenate([sinT[:, i * TPC:(i + 1) * TPC], np.zeros((128, CPC), np.float32)], axis=1)
        maps.append({
            "xT": xT_cores[i], "w": inp["w_in"][l], "modv": modv, "gain": fm(inp["norm_attn_g"][l]),
            "bgate": fm(inp["b_gate"][l]),
            "qkg": np.ascontiguousarray(np.stack([inp["q_norm_g"][l], inp["k_norm_g"][l]], axis=1)),
            "cosT": np.ascontiguousarray(cs), "sinT": np.ascontiguousarray(sn), "rm": rm})
    return run(nc, maps)


NKEY = CTX + SEQ
NKC = NKEY // 128
ATTN_SCALE = HD ** -0.5


def build_p2(with_ctx):
    nc = new_nc()
    nq = SEQ + (CTX if with_ctx else 0)
    qT = din(nc, "qT", [2, 128, nq], BF16)
    kT = din(nc, "kT", [128, NKEY], BF16)
    v = din(nc, "v", [NKEY, 128], BF16)
    oT = dout(nc, "oT", [2, 128, nq], BF16)
    P = Prog(nc)
    kb = P.sbuf("kb", [128, NKEY], BF16)
    vb = P.sbuf("vb", [128, NKC, 128], BF16)
    ones = P.sbuf("ones", [128, 128], BF16)
    qrot = P.rot("qc", 3, [128, 512], BF16)
    prot = P.rot("pb", 3, [128, 1024], BF16)
    rrot = P.rot("ri", 2, [128, 512], F32)
    orot = P.rot("ob", 2, [128, 512], BF16)
    psS = P.rot("psS", 2, [128, 1024], F32, psum=True)
    psO = P.rot("psO", 2, [128, 512], F32, psum=True)
    psL = P.rot("psL", 2, [128, 512], F32, psum=True)
    hlrot = P.rot("hl", 2, [128, 2, 512], BF16)

    class _AccRot:
        def __init__(self):
            self.items = []
            for i in range(2):
                b0 = P.sbuf("acc%d" % i, [128, 2, 512], F32)
                b1 = Buf(b0.t, "acc%db" % i)
                self.items.append((b0, b1))
            self.i = 0

        def next(self):
            it = self.items[self.i % 2]
            self.i += 1
            return it
    accrot = _AccRot()
    P.emit("pool", lambda e: e.memset(ones[:], 1.0), writes=[ones])
    for h in range(4):
        c0, c1 = h * 4160, (h + 1) * 4160
        P.dma("sp", kb[:, c0:c1], kT[:, c0:c1], writes=[kb], acc=(h > 0))
    for h in range(5):
        c0, c1 = h * 26, (h + 1) * 26
        P.dma("sp", vb[:, c0:c1, :], v[c0 * 128:c1 * 128, :].rearrange("(c p) d -> p c d", p=128), writes=[vb], acc=(h > 0))
    qchunks = [(i * 512, 512, NKC) for i in range(SEQ // 512)]
    if with_ctx:
        qchunks.append((SEQ, CTX, CTX // 128))
    def do_chunk(hh, t0, tn, nk):
        qc = qrot.next()
        P.dma("sp", qc[:, 0:tn], qT[hh, :, t0:t0 + tn], writes=[qc])
        po = psO.next()
        pl = psL.next()
        accb = accrot.next()
        acc = accb[0].t
        accs = accb
        sbufs = {}

        def S2(kp):
            ps = psS.next()
            sbufs[kp] = ps
            for h in range(2):
                kc = 2 * kp + h
                P.emit("pe", lambda e, h=h, kc=kc: e.matmul(ps[:, h * 512:h * 512 + tn], lhsT=kb[:, kc * 128:(kc + 1) * 128],
                                                            rhs=qc[:, 0:tn], start=True, stop=True),
                       reads=[kb, qc], writes=[ps], inc=(h == 1))

        def step2(kp):
            ps = sbufs.pop(kp)
            pb = prot.next()
            pv = pb.t[:, :].rearrange("p (h t) -> p h t", t=512)[:, :, 0:tn]
            sv = ps.t[:, :].rearrange("p (h t) -> p h t", t=512)[:, :, 0:tn]
            P.emit("act", lambda e: e.activation(out=pv, in_=sv, func=AF.Exp, scale=float(ATTN_SCALE)), reads=[ps], writes=[pb])
            for h in range(2):
                kc = 2 * kp + h

                def one(h=h, kc=kc):
                    P.emit("pe", lambda e: e.matmul(po[:, 0:tn], lhsT=vb[:, kc, :], rhs=pb[:, h * 512:h * 512 + tn],
                                                    start=(kc == 0), stop=(kc == nk - 1)),
                           reads=[vb, pb], writes=[po], inc=(kc == nk - 1))
                    eng = "dve" if h == 0 else "pool"
                    if kc < 2:
                        P.emit(eng, lambda e: e.tensor_copy(out=acc[:, h, 0:tn], in_=pb[:, h * 512:h * 512 + tn]),
                               reads=[pb], writes=[accs[h]])
                    else:
                        P.emit(eng, lambda e: e.tensor_tensor(out=acc[:, h, 0:tn], in0=acc[:, h, 0:tn],
                                                              in1=pb[:, h * 512:h * 512 + tn], op=ALU.add),
                               reads=[pb, accs[h]], writes=[accs[h]])
                one()
        npairs = nk // 2
        S2(0)
        for kp in range(npairs):
            if kp + 1 < npairs:
                S2(kp + 1)
            step2(kp)
        P.emit("dve", lambda e: e.tensor_tensor(out=acc[:, 0, 0:tn], in0=acc[:, 0, 0:tn], in1=acc[:, 1, 0:tn], op=ALU.add),
               reads=[accs[0], accs[1]], writes=[accs[0]])
        hl = hlrot.next()
        P.emit("dve", lambda e: e.tensor_copy(out=hl[:, 0, 0:tn], in_=acc[:, 0, 0:tn]), reads=[accs[0]], writes=[hl])
        P.emit("dve", lambda e: e.tensor_tensor(out=hl[:, 1, 0:tn], in0=acc[:, 0, 0:tn], in1=hl[:, 0, 0:tn], op=ALU.subtract),
               reads=[accs[0], hl], writes=[hl])
        P.emit("pe", lambda e: e.matmul(pl[:, 0:tn], lhsT=ones[:], rhs=hl[:, 0, 0:tn], start=True, stop=False),
               reads=[ones, hl], writes=[pl], inc=False)
        P.emit("pe", lambda e: e.matmul(pl[:, 0:tn], lhsT=ones[:], rhs=hl[:, 1, 0:tn], start=False, stop=True),
               reads=[ones, hl], writes=[pl])
        ri = rrot.next()
        P.emit("dve", lambda e: e.reciprocal(out=ri[:, 0:tn], in_=pl[:, 0:tn]), reads=[pl], writes=[ri])
        ob = orot.next()
        P.emit("dve", lambda e: e.tensor_tensor(out=ob[:, 0:tn], in0=po[:, 0:tn], in1=ri[:, 0:tn],
                                                op=ALU.mult), reads=[po, ri], writes=[ob])
        P.dma("sp", oT[hh, :, t0:t0 + tn], ob[:, 0:tn], reads=[ob], is_output=True)

    for hh in range(2):
        for (t0, tn, nk) in qchunks:
            do_chunk(hh, t0, tn, nk)
    P.finish()
    return nc


def run_p2(nc, with_ctx, qT_full, kT_full, v_full):
    maps = []
    for i in range(NCORES):
        kv = i // 2
        maps.append({"qT": np.ascontiguousarray(qT_full[i * 256:(i + 1) * 256].reshape(2, 128, -1)),
                     "kT": np.ascontiguousarray(kT_full[kv * 128:(kv + 1) * 128]),
                     "v": np.ascontiguousarray(v_full[:, kv * 128:(kv + 1) * 128])})
    res = run(nc, maps)
    return np.concatenate([r["oT"].reshape(256, -1) for r in res], axis=0)


def build_p3a():
    nc = new_nc()
    fT = din(nc, "fT", [256, SEQ], BF16)
    ccs_d = din(nc, "ccs", [128, 2, 256], BF16)
    YT = dout(nc, "YT", [2, 128, SEQ], BF16)
    P = Prog(nc)
    fb = P.sbuf("fb", [128, 2, SEQ], BF16)
    ccs = P.sbuf("ccs_s", [128, 2, 256], BF16)
    yb = P.sbuf("yb", [128, 2, SEQ], BF16)
    psr = P.rot("ps", 4, [128, 512], F32, psum=True)
    P.dma("sp", ccs[:], ccs_d, writes=[ccs])
    for cc in range(2):
        for h in range(4):
            P.dma("sp", fb[:, cc, h * 4096:(h + 1) * 4096], fT[cc * 128:(cc + 1) * 128, h * 4096:(h + 1) * 4096],
                  writes=[fb], acc=(cc + h > 0))
    n = 0
    for tc in range(SEQ // 512):
        for ri in range(2):
            ps = psr.next()
            for cc in range(2):
                P.emit("pe", lambda e, ps=ps, tc=tc, ri=ri, cc=cc: e.matmul(
                    ps[:], lhsT=ccs[:, cc, ri * 128:(ri + 1) * 128], rhs=fb[:, cc, tc * 512:(tc + 1) * 512],
                    start=(cc == 0), stop=(cc == 1)), reads=[ccs, fb], writes=[ps], inc=(cc == 1))
            if n % 2 == 0:
                P.emit("act", lambda e, ps=ps, tc=tc, ri=ri: e.activation(out=yb[:, ri, tc * 512:(tc + 1) * 512], in_=ps[:], func=AF.Copy),
                       reads=[ps], writes=[yb])
            else:
                P.emit("dve", lambda e, ps=ps, tc=tc, ri=ri: e.tensor_copy(out=yb[:, ri, tc * 512:(tc + 1) * 512], in_=ps[:]),
                       reads=[ps], writes=[yb])
            n += 1
    for ri in range(2):
        for h in range(4):
            P.dma("sp", YT[ri, :, h * 4096:(h + 1) * 4096], yb[:, ri, h * 4096:(h + 1) * 4096], reads=[yb], is_output=True)
    P.finish()
    return nc


def build_p3b(with_ctx):
    nc = new_nc()
    Yd = din(nc, "Y", [2, 128, SEQ], BF16)
    fc_d = din(nc, "fc", [256, CTX], BF16)
    ccs_d = din(nc, "ccs", [128, 2, 256], BF16)
    w1_d = din(nc, "w1", [128, 2, 256], BF16)
    tw_d = din(nc, "tw", [128, 2, 512])
    w2_d = din(nc, "w2", [128, 2, 128], BF16)
    w256_d = din(nc, "w256", [128, 2, 2, 256], BF16)
    Fo = dout(nc, "Fo", [128, SEQ], BF16)
    Fc = dout(nc, "Fc", [128, CTX], BF16)
    P = Prog(nc)
    fb = P.sbuf("fb", [128, 2, SEQ], BF16)
    yr = P.sbuf("yr", [128, SEQ], BF16)
    yi = P.sbuf("yi", [128, SEQ], BF16)
    ccs = P.sbuf("ccs_s", [128, 2, 256], BF16)
    w1 = P.sbuf("w1_s", [128, 2, 256], BF16)
    tw = P.sbuf("tw_s", [128, 2, 512], F32)
    w2 = P.sbuf("w2_s", [128, 2, 128], BF16)
    w256 = P.sbuf("w256_s", [128, 2, 2, 256], BF16)
    fcb = P.sbuf("fcb", [128, 2, CTX], BF16)
    ycb = P.sbuf("ycb", [128, 2, 256], BF16)
    ocb = P.sbuf("ocb", [128, CTX], BF16)
    arot = P.rot("A", 2, [128, 512], F32)
    brot = P.rot("B", 2, [128, 512], F32)
    psr = P.rot("ps", 4, [128, 512], F32, psum=True)
    for (dst, src) in ((ccs, ccs_d), (w1, w1_d), (tw, tw_d), (w2, w2_d), (w256, w256_d)):
        P.dma("sp", dst[:], src, writes=[dst])
    for h in range(4):
        P.dma("sp", yr[:, h * 4096:(h + 1) * 4096], Yd[0, :, h * 4096:(h + 1) * 4096], writes=[yr], acc=(h > 0))
    for h in range(4):
        P.dma("sp", yi[:, h * 4096:(h + 1) * 4096], Yd[1, :, h * 4096:(h + 1) * 4096], writes=[yi], acc=(h > 0))
    yrv = yr.t[:, :].rearrange("p (j n) -> p j n", n=128)
    yiv = yi.t[:, :].rearrange("p (j n) -> p j n", n=128)
    for jp in range(64):
        ps = psr.next()
        psv = ps.t[:, :].rearrange("p (a c) -> p a c", c=256)
        for q in range(2):
            j = jp * 2 + q
            P.emit("pe", lambda e, psv=psv, q=q, j=j: e.matmul(psv[:, q, :], lhsT=yrv[:, j, :], rhs=w1[:, 0, :],
                                                               start=True, stop=False), reads=[yr, w1], writes=[ps], inc=False)
            P.emit("pe", lambda e, psv=psv, q=q, j=j: e.matmul(psv[:, q, :], lhsT=yiv[:, j, :], rhs=w1[:, 1, :],
                                                               start=False, stop=True), reads=[yi, w1], writes=[ps], inc=(q == 1))
        A = arot.next()
        B = brot.next()
        P.emit("dve", lambda e, A=A, ps=ps: e.tensor_tensor(out=A[:], in0=ps[:], in1=tw[:, 0, :], op=ALU.mult),
               reads=[ps, tw], writes=[A])
        P.emit("dve", lambda e, B=B, ps=ps: e.tensor_tensor(out=B[:], in0=ps[:], in1=tw[:, 1, :], op=ALU.mult),
               reads=[ps, tw], writes=[B])
        for q in range(2):
            o0 = jp * 256 + q * 128
            P.emit("pool", lambda e, A=A, B=B, q=q, o0=o0: e.tensor_tensor(
                out=fb[:, 0, o0:o0 + 128], in0=A[:, q * 256:q * 256 + 128], in1=B[:, q * 256 + 128:q * 256 + 256],
                op=ALU.subtract), reads=[A, B], writes=[fb])
            P.emit("pool", lambda e, A=A, B=B, q=q, o0=o0: e.tensor_tensor(
                out=fb[:, 1, o0:o0 + 128], in0=B[:, q * 256:q * 256 + 128], in1=A[:, q * 256 + 128:q * 256 + 256],
                op=ALU.add), reads=[A, B], writes=[fb])
    for cq in range(32):
        ps = psr.next()
        P.emit("pe", lambda e, ps=ps, cq=cq: e.matmul(ps[:], lhsT=w2[:, 0, :], rhs=fb[:, 0, cq * 512:(cq + 1) * 512],
                                                      start=True, stop=False), reads=[fb, w2], writes=[ps], inc=False)
        P.emit("pe", lambda e, ps=ps, cq=cq: e.matmul(ps[:], lhsT=w2[:, 1, :], rhs=fb[:, 1, cq * 512:(cq + 1) * 512],
                                                      start=False, stop=True), reads=[fb, w2], writes=[ps])
        if cq % 2 == 0:
            P.emit("act", lambda e, ps=ps, cq=cq: e.activation(out=yr[:, cq * 512:(cq + 1) * 512], in_=ps[:], func=AF.Copy,
                                                               scale=float(1.0 / 2048.0)), reads=[ps], writes=[yr])
        else:
            P.emit("dve", lambda e, ps=ps, cq=cq: e.tensor_scalar(out=yr[:, cq * 512:(cq + 1) * 512], in0=ps[:],
                                                                  scalar1=float(1.0 / 2048.0), scalar2=None, op0=ALU.mult),
                   reads=[ps], writes=[yr])
    for h in range(4):
        P.dma("sp", Fo[:, h * 4096:(h + 1) * 4096], yr[:, h * 4096:(h + 1) * 4096], reads=[yr], is_output=True)
    if with_ctx:
        for cc in range(2):
            P.dma("sp", fcb[:, cc, :], fc_d[cc * 128:(cc + 1) * 128, :], writes=[fcb], acc=(cc > 0))
        for tt in range(2):
            ps = psr.next()
            for cc in range(2):
                P.emit("pe", lambda e, ps=ps, tt=tt, cc=cc: e.matmul(
                    ps[:, 0:256], lhsT=fcb[:, cc, tt * 128:(tt + 1) * 128], rhs=ccs[:, cc, :], start=(cc == 0), stop=(cc == 1)),
                    reads=[fcb, ccs], writes=[ps], inc=(cc == 1))
            P.emit("act", lambda e, ps=ps, tt=tt: e.activation(out=ycb[:, tt, :], in_=ps[:, 0:256], func=AF.Copy),
                   reads=[ps], writes=[ycb])
        ps = psr.next()
        n = 0
        for tt in range(2):
            for ri in range(2):
                P.emit("pe", lambda e, ps=ps, tt=tt, ri=ri, n=n: e.matmul(
                    ps[:, 0:256], lhsT=ycb[:, tt, ri * 128:(ri + 1) * 128], rhs=w256[:, tt, ri, :], start=(n == 0), stop=(n == 3)),
                    reads=[ycb, w256], writes=[ps], inc=(n == 3))
                n += 1
        P.emit("act", lambda e, ps=ps: e.activation(out=ocb[:], in_=ps[:, 0:256], func=AF.Copy, scale=float(1.0 / 256.0)),
               reads=[ps], writes=[ocb])
    else:
        P.emit("pool", lambda e: e.memset(ocb[:], 0.0), writes=[ocb])
    P.dma("sp", Fc, ocb[:], reads=[ocb], is_output=True)
    P.finish()
    return nc


def p3_consts(half):
    p = np.arange(128)
    out = {}
    ccs = np.zeros((128, 2, 256), np.float64)
    j = 128 * half + np.arange(128)
    for cc in range(2):
        c = cc * 128 + p
        ang = 2 * np.pi * np.outer(c, j) / 256.0
        ccs[:, cc, 0:128] = np.cos(ang)
        ccs[:, cc, 128:256] = np.sin(ang)
    out["ccs"] = ccs.astype(NPBF)
    a128 = 2 * np.pi * np.outer(p, p) / 128.0
    C, S = np.cos(a128), np.sin(a128)
    w1 = np.zeros((128, 2, 256))
    w1[:, 0, 0:128] = C
    w1[:, 0, 128:256] = S
    w1[:, 1, 0:128] = -S
    w1[:, 1, 128:256] = C
    out["w1"] = w1.astype(NPBF)
    psi = 2 * np.pi * np.outer(p, p) / float(SEQ)
    tw = np.zeros((128, 2, 512))
    tw[:, 0, :] = np.tile(np.cos(psi), (1, 4))
    tw[:, 1, :] = np.tile(np.sin(psi), (1, 4))
    out["tw"] = tw.astype(np.float32)
    w2 = np.zeros((128, 2, 128))
    w2[:, 0] = C
    w2[:, 1] = -S
    out["w2"] = w2.astype(NPBF)
    w256 = np.zeros((128, 2, 2, 256))
    for tt in range(2):
        n = tt * 128 + p
        ang = 2 * np.pi * np.outer(n, np.arange(256)) / 256.0
        w256[:, tt, 0] = np.cos(ang)
        w256[:, tt, 1] = -np.sin(ang)
    out["w256"] = w256.astype(NPBF)
    out["ident"] = np.eye(128).astype(NPBF)
    return out


def run_p3(nca, ncb, fT_full):
    consts = [p3_consts(h) for h in range(2)]
    maps = []
    for i in range(NCORES):
        g, half = i // 2, i % 2
        maps.append({"fT": np.ascontiguousarray(fT_full[g * 256:(g + 1) * 256, :SEQ]), "ccs": consts[half]["ccs"]})
    ra = run(nca, maps)
    maps = []
    for i in range(NCORES):
        g, half = i // 2, i % 2
        c = consts[half]
        YT = ra[i]["YT"]
        Y = np.ascontiguousarray(YT.reshape(2, 128, 128, 128).transpose(0, 2, 1, 3)).reshape(2, 128, SEQ)
        maps.append({"Y": Y, "fc": np.ascontiguousarray(fT_full[g * 256:(g + 1) * 256, SEQ:]), "ccs": c["ccs"], "w1": c["w1"],
                     "tw": c["tw"], "w2": c["w2"], "w256": c["w256"]})
    rb = run(ncb, maps)
    outs = []
    for i in range(NCORES):
        Fo = rb[i]["Fo"].reshape(128, 128, 128)
        lat = np.ascontiguousarray(Fo.transpose(1, 0, 2)).reshape(128, SEQ)
        outs.append(np.concatenate([lat, rb[i]["Fc"]], axis=1))
    return np.concatenate(outs, axis=0)


DBG = {}


def build_p4(moe, with_ctx, last):
    nc = new_nc()
    ntok = TT if with_ctx else TPC
    xT = din(nc, "xT", [D, ntok])
    at_d = din(nc, "attnT", [QW, ntok], BF16)
    ft_d = din(nc, "FT", [FD, ntok], BF16)
    gt_d = din(nc, "gT", [2 * D, ntok], BF16)
    wp = din(nc, "wp", [QW, D])
    wf = din(nc, "wf", [FD, D])
    wo = din(nc, "wo", [D, D])
    if moe:
        rw_d = din(nc, "rw", [128, KC, 8])
        mg = din(nc, "mg", [NE, D, DFE])
        mu = din(nc, "mu", [NE, D, DFE])
        md = din(nc, "md", [NE, DFE, D])
        id_d = din(nc, "ident", [128, 128], BF16)
    else:
        wg = din(nc, "wg", [D, DFF])
        wu = din(nc, "wu", [D, DFF])
        wd = din(nc, "wd", [DFF, D])
    modv = din(nc, "modv", [128, 2, 4, KC])
    gain2 = din(nc, "gain2", [128, KC])
    fgain = din(nc, "fgain", [128, KC])
    xo = dout(nc, "xo", [D, ntok])
    P = Prog(nc)
    xs = P.sbuf("xs", [128, KC, 512], F32)
    atb = P.sbuf("atb", [128, KC, 512], BF16)
    fg = P.sbuf("fg", [128, (40 if moe else 44) * 512], BF16)
    yT = P.sbuf("yT", [128, KC, 512], BF16)
    ones = P.sbuf("ones", [128, 128], BF16)
    modb = P.sbuf("modb", [128, 2, 4, KC], F32)
    g2b = P.sbuf("g2b", [128, KC], F32)
    fgb = P.sbuf("fgb", [128, KC], F32)
    a2b = P.sbuf("a2b", [128, 2, KC], F32)
    wrot = P.rot("wb", 3, [128, 8192], BF16)
    t1r = P.rot("t1", 2, [128, 512], F32)
    t2r = P.rot("t2", 2, [128, 512], F32)
    sr = P.rot("sl", 2, [128, 512], F32)
    rsr = P.rot("rs", 2, [128, 3, 512], F32)
    psm = P.rot("psm", 6, [128, 512], F32, psum=True)
    pss = P.rot("pss", 2, [128, 512], F32, psum=True)
    ftv = fg.t[:, 0:8 * 512].rearrange("p (k t) -> p k t", t=512)
    gtv = fg.t[:, 8 * 512:40 * 512].rearrange("p (k t) -> p k t", t=512)
    aTv = fg.t[:, :].rearrange("p (k t) -> p k t", t=512)
    for (dst, src) in ((modb, modv), (g2b, gain2), (fgb, fgain)):
        P.dma("sp", dst[:], src, writes=[dst])
    P.emit("pool", lambda e: e.memset(ones[:], 1.0), writes=[ones])
    for r in rsr.bufs:
        P.emit("pool", lambda e, r=r: e.memset(r[:, 2, :], -0.5), writes=[r])
    for wi in range(2):
        P.emit("dve", lambda e, wi=wi: e.tensor_scalar(out=a2b[:, wi, :], in0=modb[:, wi, 2, :], scalar1=1.0,
                                                       scalar2=float(math.sqrt(D)), op0=ALU.add, op1=ALU.mult),
               reads=[modb], writes=[a2b])
        P.emit("dve", lambda e, wi=wi: e.tensor_tensor(out=a2b[:, wi, :], in0=a2b[:, wi, :], in1=g2b[:], op=ALU.mult),
               reads=[a2b, g2b], writes=[a2b])
    P.emit("dve", lambda e: e.tensor_scalar(out=fgb[:], in0=fgb[:], scalar1=float(math.sqrt(D)), scalar2=None, op0=ALU.mult),
           reads=[fgb], writes=[fgb])
    if moe:
        rwf = P.sbuf("rwf", [128, KC, 8], F32)
        rwb = P.sbuf("rwb", [128, KC, 16], BF16)
        ident = P.sbuf("ident_s", [128, 128], BF16)
        bcs = P.sbuf("bcs", [128, NE, 512], F32)
        l16 = P.sbuf("l16", [128, 16], F32)
        sm = P.sbuf("sm", [128, 8, 8], F32)
        sc1 = P.sbuf("sc1", [128, 8], F32)
        rep = P.rot("rep", 2, [128, 2, NE, 128], BF16)
        u2r = P.rot("u2", 2, [128, 512], F32)
        P.dma("sp", rwf[:], rw_d, writes=[rwf])
        P.dma("sp", ident[:], id_d, writes=[ident])
        P.emit("act", lambda e: e.activation(out=rwb[:, :, 0:8], in_=rwf[:], func=AF.Copy), reads=[rwf], writes=[rwb])
        P.emit("dve", lambda e: e.tensor_tensor(out=rwb[:, :, 8:16], in0=rwf[:], in1=rwb[:, :, 0:8], op=ALU.subtract),
               reads=[rwf, rwb], writes=[rwb])

    def load_w(src2d, k0, nkc, n0, ncols):
        wb = wrot.next()
        view = wb.t[:, 0:nkc * ncols].rearrange("p (k n) -> p k n", n=ncols)
        first = True
        for h0 in range(0, nkc, 8):
            h1 = min(nkc, h0 + 8)
            P.dma("pool", view[:, h0:h1, :],
                  src2d[k0 + h0 * 128:k0 + h1 * 128, n0:n0 + ncols].rearrange("(kc p) n -> p kc n", p=128),
                  writes=[wb], acc=(not first))
            first = False
        return wb, view

    def mm_group(ps, tn, parts, inc_last=True):
        n = len(parts)
        for i, (wbuf, lhsT, rbuf, rhs) in enumerate(parts):
            P.emit("pe", lambda e, lhsT=lhsT, rhs=rhs, i=i: e.matmul(ps[:, 0:tn], lhsT=lhsT, rhs=rhs, start=(i == 0), stop=(i == n - 1)),
                   reads=[wbuf, rbuf], writes=[ps], inc=(inc_last and i == n - 1))

    def resid(ps, j, tn, gcol, wi):
        P.emit("dve", lambda e: e.scalar_tensor_tensor(out=xs[:, j, 0:tn], in0=ps[:, 0:tn], scalar=modb[:, wi, gcol, j:j + 1],
                                                       in1=xs[:, j, 0:tn], op0=ALU.mult, op1=ALU.add),
               reads=[ps, modb, xs], writes=[xs])

    def rstd_of_xs(tn):
        P.emit("act", lambda e: e.activation(out=yT[:, :, 0:tn], in_=xs[:, :, 0:tn], func=AF.Square), reads=[xs], writes=[yT])
        ps_ = pss.next()
        mm_group(ps_, tn, [(ones, ones[:], yT, yT[:, kc, 0:tn]) for kc in range(KC)])
        rs = rsr.next()
        P.emit("dve", lambda e: e.tensor_scalar(out=rs[:, 0, 0:tn], in0=ps_[:, 0:tn], scalar1=float(D * EPS), scalar2=None,
                                                op0=ALU.add), reads=[ps_], writes=[rs])
        P.emit("pool", lambda e: e.tensor_tensor(out=rs[:, 1, 0:tn], in0=rs[:, 0, 0:tn], in1=rs[:, 2, 0:tn], op=ALU.pow),
               reads=[rs], writes=[rs])
        return rs

    def ffn_gu(c, tn, wgb, wgv, wub, wuv, jj, bc_e=None):
        psG = psm.next()
        mm_group(psG, tn, [(wgb, wgv[:, kc, jj * 128:(jj + 1) * 128], atb, atb[:, kc, 0:tn]) for kc in range(KC)])
        psU = psm.next()
        mm_group(psU, tn, [(wub, wuv[:, kc, jj * 128:(jj + 1) * 128], atb, atb[:, kc, 0:tn]) for kc in range(KC)])
        s_ = sr.next()
        P.emit("act", lambda e: e.activation(out=s_[:, 0:tn], in_=psG[:, 0:tn], func=AF.Silu), reads=[psG], writes=[s_])
        if bc_e is None:
            P.emit("dve", lambda e: e.tensor_tensor(out=aTv[:, c, 0:tn], in0=s_[:, 0:tn], in1=psU[:, 0:tn], op=ALU.mult),
                   reads=[s_, psU], writes=[fg])
        else:
            u2 = u2r.next()
            P.emit("dve", lambda e: e.tensor_tensor(out=u2[:, 0:tn], in0=psU[:, 0:tn], in1=bcs[:, bc_e, 0:tn], op=ALU.mult),
                   reads=[psU, bcs], writes=[u2])
            P.emit("dve", lambda e: e.tensor_tensor(out=aTv[:, c, 0:tn], in0=s_[:, 0:tn], in1=u2[:, 0:tn], op=ALU.mult),
                   reads=[s_, u2], writes=[fg])

    def router_tile(tt, rstage=9):
        psR = pss.next()
        parts = [(atb, atb[:, kc, tt * 128:(tt + 1) * 128], rwb, rwb[:, kc, 0:16]) for kc in range(KC)]
        n = len(parts)
        for i, (wbuf, lhsT, rbuf, rhs) in enumerate(parts):
            P.emit("pe", lambda e, lhsT=lhsT, rhs=rhs, i=i: e.matmul(psR[:, 0:16], lhsT=lhsT, rhs=rhs, start=(i == 0), stop=False),
                   reads=[wbuf, rbuf], writes=[psR], inc=False)
        for kc in range(KC):
            P.emit("pe", lambda e, kc=kc: e.matmul(psR[:, 0:8], lhsT=yT[:, kc, tt * 128:(tt + 1) * 128], rhs=rwb[:, kc, 0:8],
                                                   start=False, stop=(kc == KC - 1)),
                   reads=[yT, rwb], writes=[psR], inc=(kc == KC - 1))
        P.emit("dve", lambda e: e.tensor_copy(out=l16[:], in_=psR[:, 0:16]), reads=[psR], writes=[l16])
        if rstage < 1:
            if tt == 0:
                P.emit("pool", lambda e: e.memset(bcs[:], 0.5), writes=[bcs])
            return
        lg, m1, mk1, l2, m2, mk2, dd = (sm[:, 0, :], sm[:, 1, 0:1], sm[:, 2, :], sm[:, 3, :], sm[:, 1, 1:2], sm[:, 4, :], sm[:, 1, 2:3])
        ee, den, w1, w2 = sm[:, 1, 3:4], sm[:, 1, 4:5], sm[:, 1, 5:6], sm[:, 1, 6:7]
        comb = sm[:, 5, :]
        D_ = lambda fn: P.emit("dve", fn, reads=[sm, l16], writes=[sm])
        D_(lambda e: e.tensor_tensor(out=lg, in0=l16[:, 0:8], in1=l16[:, 8:16], op=ALU.add))
        D_(lambda e: e.reduce_max(out=m1, in_=lg, axis=AX.X))
        D_(lambda e: e.tensor_scalar(out=mk1, in0=lg, scalar1=m1, scalar2=None, op0=ALU.is_equal))
        D_(lambda e: e.scalar_tensor_tensor(out=l2, in0=mk1, scalar=-1e30, in1=lg, op0=ALU.mult, op1=ALU.add))
        D_(lambda e: e.reduce_max(out=m2, in_=l2, axis=AX.X))
        D_(lambda e: e.tensor_scalar(out=mk2, in0=l2, scalar1=m2, scalar2=None, op0=ALU.is_equal))
        D_(lambda e: e.tensor_tensor(out=dd, in0=m2, in1=m1, op=ALU.subtract))
        P.emit("act", lambda e: e.activation(out=ee, in_=dd, func=AF.Exp), reads=[sm], writes=[sm])
        D_(lambda e: e.tensor_scalar(out=den, in0=ee, scalar1=1.0, scalar2=None, op0=ALU.add))
        D_(lambda e: e.reciprocal(out=w1, in_=den))
        D_(lambda e: e.tensor_tensor(out=w2, in0=ee, in1=w1, op=ALU.mult))
        D_(lambda e: e.tensor_scalar(out=comb, in0=mk1, scalar1=w1, scalar2=None, op0=ALU.mult))
        D_(lambda e: e.scalar_tensor_tensor(out=comb, in0=mk2, scalar=w2, in1=comb, op0=ALU.mult, op1=ALU.add))
        if rstage < 2:
            if tt == 0:
                P.emit("pool", lambda e: e.memset(bcs[:], 0.5), writes=[bcs])
            return
        rp = rep.next()
        cb = comb.unsqueeze(2).to_broadcast([128, NE, 128])
        P.emit("dve", lambda e: e.tensor_copy(out=rp[:, 0], in_=cb), reads=[sm], writes=[rp])
        P.emit("dve", lambda e: e.tensor_tensor(out=rp[:, 1], in0=cb, in1=rp[:, 0], op=ALU.subtract), reads=[sm, rp], writes=[rp])
        if rstage < 3:
            if tt == 0:
                P.emit("pool", lambda e: e.memset(bcs[:], 0.5), writes=[bcs])
            return
        for e_ in range(NE):
            def bc_one(e_=e_):
                pb_ = psm.next()
                P.emit("pe", lambda e: e.matmul(pb_[:, 0:128], lhsT=rp[:, 0, e_, :], rhs=ident[:], start=True, stop=False),
                       reads=[rp, ident], writes=[pb_], inc=False)
                P.emit("pe", lambda e: e.matmul(pb_[:, 0:128], lhsT=rp[:, 1, e_, :], rhs=ident[:], start=False, stop=True),
                       reads=[rp, ident], writes=[pb_])
                if e_ % 2 == 0:
                    P.emit("act", lambda e: e.activation(out=bcs[:, e_, tt * 128:(tt + 1) * 128], in_=pb_[:, 0:128], func=AF.Copy),
                           reads=[pb_], writes=[bcs])
                else:
                    P.emit("dve", lambda e: e.tensor_copy(out=bcs[:, e_, tt * 128:(tt + 1) * 128], in_=pb_[:, 0:128]),
                           reads=[pb_], writes=[bcs])
            bc_one()

    def do_chunk(t0, tn, wi):
        P.dma("sp", xs[:, :, 0:tn], xT[:, t0:t0 + tn].rearrange("(kc p) t -> p kc t", p=128), writes=[xs])
        for h in range(2):
            P.dma("sp", atb[:, h * 8:(h + 1) * 8, 0:tn], at_d[h * 1024:(h + 1) * 1024, t0:t0 + tn].rearrange("(kc p) t -> p kc t", p=128),
                  writes=[atb], acc=(h > 0))
        P.dma("sp", ftv[:, :, 0:tn], ft_d[:, t0:t0 + tn].rearrange("(kc p) t -> p kc t", p=128), writes=[fg])
        for h in range(4):
            P.dma("sp", gtv[:, h * 8:(h + 1) * 8, 0:tn], gt_d[h * 1024:(h + 1) * 1024, t0:t0 + tn].rearrange("(kc p) t -> p kc t", p=128),
                  writes=[fg], acc=True)
        for pn in range(4):
            wpb, wpv = load_w(wp, 0, KC, pn * 512, 512)
            wfb, wfv = load_w(wf, 0, 8, pn * 512, 512)
            for jj in range(4):
                j = pn * 4 + jj

                def ya(j=j, jj=jj, wpb=wpb, wpv=wpv, wfb=wfb, wfv=wfv):
                    psA = psm.next()
                    mm_group(psA, tn, [(wpb, wpv[:, kc, jj * 128:(jj + 1) * 128], atb, atb[:, kc, 0:tn]) for kc in range(KC)])
                    psB = psm.next()
                    mm_group(psB, tn, [(wfb, wfv[:, kc, jj * 128:(jj + 1) * 128], fg, ftv[:, kc, 0:tn]) for kc in range(8)])
                    t1 = t1r.next()
                    t2 = t2r.next()
                    P.emit("dve", lambda e: e.tensor_tensor(out=t1[:, 0:tn], in0=psA[:, 0:tn], in1=gtv[:, j, 0:tn], op=ALU.mult),
                           reads=[psA, fg], writes=[t1])
                    P.emit("dve", lambda e: e.tensor_tensor(out=t2[:, 0:tn], in0=psB[:, 0:tn], in1=gtv[:, 16 + j, 0:tn], op=ALU.mult),
                           reads=[psB, fg], writes=[t2])
                    P.emit("dve", lambda e: e.tensor_tensor(out=yT[:, j, 0:tn], in0=t1[:, 0:tn], in1=t2[:, 0:tn], op=ALU.add),
                           reads=[t1, t2], writes=[yT])
                ya()
        for pn in range(4):
            wob, wov = load_w(wo, 0, KC, pn * 512, 512)
            for jj in range(4):
                j = pn * 4 + jj
                psZ = psm.next()
                mm_group(psZ, tn, [(wob, wov[:, kc, jj * 128:(jj + 1) * 128], yT, yT[:, kc, 0:tn]) for kc in range(KC)])
                resid(psZ, j, tn, 0, wi)
        rs = rstd_of_xs(tn)
        for kc in range(KC):
            def hk(kc=kc):
                t1 = t1r.next()
                P.emit("dve", lambda e: e.tensor_tensor(out=t1[:, 0:tn], in0=xs[:, kc, 0:tn], in1=rs[:, 1, 0:tn],
                                                                                   op=ALU.mult), reads=[xs, rs], writes=[t1])
                if not moe:
                    P.emit("act", lambda e: e.activation(out=atb[:, kc, 0:tn], in_=t1[:, 0:tn], func=AF.Identity,
                                                         bias=modb[:, wi, 1, kc:kc + 1], scale=a2b[:, wi, kc:kc + 1]),
                           reads=[t1, modb, a2b], writes=[atb])
                else:
                    t2 = t2r.next()
                    P.emit("act", lambda e: e.activation(out=t2[:, 0:tn], in_=t1[:, 0:tn], func=AF.Identity,
                                                         bias=modb[:, wi, 1, kc:kc + 1], scale=a2b[:, wi, kc:kc + 1]),
                           reads=[t1, modb, a2b], writes=[t2])
                    P.emit("dve", lambda e: e.tensor_copy(out=atb[:, kc, 0:tn], in_=t2[:, 0:tn]), reads=[t2], writes=[atb])
                    P.emit("dve", lambda e: e.tensor_tensor(out=yT[:, kc, 0:tn], in0=t2[:, 0:tn], in1=atb[:, kc, 0:tn], op=ALU.subtract),
                           reads=[t2, atb], writes=[yT])
            hk()
        if not moe:
            for pn in range(DFF // 512):
                wgb, wgv = load_w(wg, 0, KC, pn * 512, 512)
                wub, wuv = load_w(wu, 0, KC, pn * 512, 512)
                for jj in range(4):
                    ffn_gu(pn * 4 + jj, tn, wgb, wgv, wub, wuv, jj)
            for np_ in range(8):
                wab, wav = load_w(wd, 0, 22, np_ * 256, 256)
                wbb, wbv = load_w(wd, 22 * 128, 22, np_ * 256, 256)
                for jj in range(2):
                    j = np_ * 2 + jj
                    psD = psm.next()
                    parts = [(wab, wav[:, c, jj * 128:(jj + 1) * 128], fg, aTv[:, c, 0:tn]) for c in range(22)]
                    parts += [(wbb, wbv[:, c, jj * 128:(jj + 1) * 128], fg, aTv[:, 22 + c, 0:tn]) for c in range(22)]
                    mm_group(psD, tn, parts)
                    resid(psD, j, tn, 3, wi)
        else:
            if DBG.get("norouter"):
                P.emit("pool", lambda e: e.memset(bcs[:], 0.5), writes=[bcs])
            else:
                for tt in range(tn // 128):
                    router_tile(tt, DBG.get("rstage", 9))
            for e_ in range(DBG.get("nexp", NE)):
                for pn in range(6):
                    ncols = 512 if pn < 5 else 256
                    wgb, wgv = load_w(mg[e_], 0, KC, pn * 512, ncols)
                    wub, wuv = load_w(mu[e_], 0, KC, pn * 512, ncols)
                    for jj in range(ncols // 128):
                        ffn_gu(pn * 4 + jj, tn, wgb, wgv, wub, wuv, jj, bc_e=e_)
                for np_ in range(8):
                    wab, wav = load_w(md[e_], 0, 22, np_ * 256, 256)
                    for jj in range(2):
                        j = np_ * 2 + jj
                        psD = psm.next()
                        mm_group(psD, tn, [(wab, wav[:, c, jj * 128:(jj + 1) * 128], fg, aTv[:, c, 0:tn]) for c in range(22)])
                        resid(psD, j, tn, 3, wi)
        if last:
            rs2 = rstd_of_xs(tn)
            for kc in range(KC):
                P.emit("dve", lambda e, kc=kc: e.scalar_tensor_tensor(out=xs[:, kc, 0:tn], in0=xs[:, kc, 0:tn], scalar=fgb[:, kc:kc + 1],
                                                                      in1=rs2[:, 1, 0:tn], op0=ALU.mult, op1=ALU.mult),
                       reads=[xs, fgb, rs2], writes=[xs])
        P.dma("sp", xo[:, t0:t0 + tn].rearrange("(kc p) t -> p kc t", p=128), xs[:, :, 0:tn], reads=[xs], is_output=True)

    for (t0, tn) in CHUNKS[:DBG.get("nchunk", 9)]:
        if t0 >= TPC and not with_ctx:
            continue
        do_chunk(t0, tn, 1 if t0 >= TPC else 0)
    P.finish()
    return nc


def run_p4(nc, l, moe, with_ctx, inp, xT_cores, attnT_cores, FT_cores, gT_cores, mod):
    modv = np.stack([np.stack([mod[:, l, 32:48, wi], mod[:, l, 48:64, wi], mod[:, l, 64:80, wi], mod[:, l, 80:96, wi]], axis=1)
                     for wi in range(2)], axis=1)
    modv = np.ascontiguousarray(modv.astype(np.float32))
    maps = []
    for i in range(NCORES):
        m = {"xT": xT_cores[i], "attnT": attnT_cores[i], "FT": FT_cores[i], "gT": gT_cores[i],
             "wp": inp["w_attn_proj"][l], "wf": inp["w_four_proj"][l], "wo": inp["w_out"][l],
             "modv": modv, "gain2": fm(inp["norm_ffn_g"][l]), "fgain": fm(inp["final_norm_g"])}
        if moe:
            li = l // 2
            m["rw"] = np.ascontiguousarray(inp["router_w"][li].reshape(KC, 128, NE).transpose(1, 0, 2))
            m["mg"] = inp["moe_w_gate"][li]
            m["mu"] = inp["moe_w_up"][li]
            m["md"] = inp["moe_w_down"][li]
            m["ident"] = np.eye(128).astype(NPBF)
        else:
            li = l // 2
            m["wg"] = inp["ffn_w_gate"][li]
            m["wu"] = inp["ffn_w_up"][li]
            m["wd"] = inp["ffn_w_down"][li]
        maps.append(m)
    res = run(nc, maps)
    return [r["xo"] for r in res]


def kernel(**inp):
    inp = {k: np.asarray(v) for k, v in inp.items()}
    mod = run_p0(inp)
    x = inp["x"][0]
    ctx = inp["ctx"][0]
    xT_cores = [np.ascontiguousarray(np.concatenate([x[i * TPC:(i + 1) * TPC].T, ctx[i * CPC:(i + 1) * CPC].T], axis=1))
                for i in range(NCORES)]
    out = None
    for l in range(2):
        with_ctx = (l == 0)
        moe = (l % 2 == 1)
        last = (l == 1)
        r1 = run_p1(build_p1(), l, inp, xT_cores, mod)

        def gather(name, axis_tok):
            lat = np.concatenate([np.take(r[name], range(0, TPC), axis=axis_tok) for r in r1], axis=axis_tok)
            cx = np.concatenate([np.take(r[name], range(TPC, TT), axis=axis_tok) for r in r1], axis=axis_tok)
            return lat, cx
        q_lat, q_ctx = gather("qT", 1)
        k_lat, k_ctx = gather("kT", 1)
        v_lat, v_ctx = gather("v", 0)
        f_lat, f_ctx = gather("fT", 1)
        q_full = np.concatenate([q_lat, q_ctx], axis=1) if with_ctx else q_lat
        kT_full = np.concatenate([k_ctx, k_lat], axis=1)
        v_full = np.concatenate([v_ctx, v_lat], axis=0)
        oT = run_p2(build_p2(with_ctx), with_ctx, q_full, kT_full, v_full)
        fT_full = np.concatenate([f_lat, f_ctx], axis=1)
        FT = run_p3(build_p3a(), build_p3b(with_ctx), fT_full)

        def percore(a):
            res = []
            for i in range(NCORES):
                if with_ctx:
                    res.append(np.ascontiguousarray(np.concatenate(
                        [a[:, i * TPC:(i + 1) * TPC], a[:, SEQ + i * CPC:SEQ + (i + 1) * CPC]], axis=1)))
                else:
                    res.append(np.ascontiguousarray(a[:, i * TPC:(i + 1) * TPC]))
            return res
        attn_c = percore(oT)
        FT_c = percore(FT)
        ntok = TT if with_ctx else TPC
        g_c = [np.ascontiguousarray(r["gT"][:, :ntok]) for r in r1]
        x_c = [np.ascontiguousarray(xc[:, :ntok]) for xc in xT_cores]
        xo = run_p4(build_p4(moe, with_ctx, last), l, moe, with_ctx, inp, x_c, attn_c, FT_c, g_c, mod)
        if last:
            out = np.concatenate([xo[i][:, :TPC].T for i in range(NCORES)], axis=0)
        else:
            xT_cores = xo
    return np.ascontiguousarray(out.reshape(1, SEQ, D).astype(np.float32))
```

```python
import math
import numpy as np
import ml_dtypes
import concourse.bass as bass
import concourse.mybir as mybir
from concourse.bass_utils import run_bass_kernel_spmd

F32 = mybir.dt.float32
BF16 = mybir.dt.bfloat16
ALU = mybir.AluOpType
AF = mybir.ActivationFunctionType
AX = mybir.AxisListType
NPBF = ml_dtypes.bfloat16

NCORES = 8
D = 2048
KC = 16
SEQ = 16384
CTX = 256
TPC = SEQ // NCORES
CPC = CTX // NCORES
TT = TPC + CPC
NH, NKV, HD = 16, 4, 128
QW, KVW, FD = 2048, 512, 1024
INC = 8192
DFF = 5632
NE = 8
DFE = 2816
EPS = 1e-6
GRID_W = 64
ENGS = ("pe", "act", "dve", "pool", "sp")


class Buf:
    __slots__ = ("t", "last_w", "readers", "name", "ws")

    def __init__(self, t, name=""):
        self.t = t
        self.last_w = None
        self.ws = []
        self.readers = {}
        self.name = name

    def __getitem__(self, idx):
        return self.t[idx]


class Rot:
    def __init__(self, bufs):
        self.bufs = bufs
        self.i = 0

    def next(self):
        b = self.bufs[self.i % len(self.bufs)]
        self.i += 1
        return b


class Prog:
    def __init__(self, nc, same_engine_sync=True, n_dma_sems=8):
        self.nc = nc
        self.streams = {e: [] for e in ENGS}
        self.cnt = {e: 0 for e in ENGS}
        self.waited = {e: {} for e in ENGS}
        self.same_engine_sync = same_engine_sync
        self.sems = {}
        self.ctx = []
        for e in ENGS:
            self.sems[("eng", e)] = self._sem("s_" + e)
        self.dma_rot = {}
        self.dma_val = {}
        for q in ("sp", "act", "pool"):
            lst = []
            for i in range(n_dma_sems):
                k = ("dma", q + str(i))
                self.sems[k] = self._sem("d_%s%d" % (q, i))
                self.dma_val[k] = 0
                lst.append(k)
            self.dma_rot[q] = [lst, 0]
        self.out_tokens = []

    def _sem(self, name):
        cm = self.nc.semaphore(name)
        s = cm.__enter__()
        self.ctx.append(cm)
        return s

    def sbuf(self, name, shape, dtype):
        cm = self.nc.sbuf_tensor(name, shape, dtype)
        t = cm.__enter__()
        self.ctx.append(cm)
        return Buf(t, name)

    def psum(self, name, shape, dtype=F32):
        cm = self.nc.psum_tensor(name, shape, dtype)
        t = cm.__enter__()
        self.ctx.append(cm)
        return Buf(t, name)

    def rot(self, name, n, shape, dtype, psum=False):
        return Rot([(self.psum if psum else self.sbuf)("%s%d" % (name, i), shape, dtype) for i in range(n)])

    def _collect(self, e, reads, writes, acc=False):
        waits = {}

        def need(tok):
            if tok is None:
                return
            k, v = tok
            if k == ("eng", e):
                if e == "pe" or not self.same_engine_sync:
                    return
            if waits.get(k, 0) < v:
                waits[k] = v

        for b in reads:
            need(b.last_w)
            for t_ in b.ws:
                need(t_)
        for b in writes:
            need(b.last_w)
            for t_ in b.ws:
                if acc and t_[0][0] == "dma":
                    continue
                need(t_)
            for k, v in b.readers.items():
                if k == ("eng", e):
                    continue
                need((k, v))
        wl = []
        for k, v in waits.items():
            if self.waited[e].get(k, 0) >= v:
                continue
            self.waited[e][k] = v
            wl.append((k, v))
        return wl

    def _mark(self, tok, reads, writes, acc=False):
        k, v = tok
        for b in writes:
            if acc:
                b.ws.append(tok)
                continue
            b.last_w = tok
            b.ws = []
            b.readers = {}
        for b in reads:
            if b.readers.get(k, 0) < v:
                b.readers[k] = v

    def emit(self, e, fn, reads=(), writes=(), inc=True):
        wl = self._collect(e, reads, writes)
        if inc:
            self.cnt[e] += 1
            tok = (("eng", e), self.cnt[e])
        else:
            tok = (("eng", e), self.cnt[e] + 1)
        self.streams[e].append((wl, fn, ("eng", e) if inc else None, 1))
        self._mark(tok, reads, writes)
        return tok

    def dma(self, q, out, in_, reads=(), writes=(), is_output=False, acc=False):
        lst, i = self.dma_rot[q]
        k = lst[i % len(lst)]
        self.dma_rot[q][1] = i + 1
        wl = self._collect(q, reads, writes, acc)
        prev = self.dma_val[k]
        if prev > 0 and self.waited[q].get(k, 0) < prev:
            self.waited[q][k] = prev
            wl.append((k, prev))
        self.dma_val[k] = prev + 16
        tok = (k, prev + 16)
        self.streams[q].append((wl, lambda e: e.dma_start(out=out, in_=in_), k, 16))
        self._mark(tok, reads, writes, acc)
        if is_output:
            self.out_tokens.append(tok)
        return tok

    def finish(self):
        fin = {}
        for k, v in self.out_tokens:
            fin[k] = max(fin.get(k, 0), v)
        nc = self.nc
        sems = self.sems
        streams = self.streams
        emap = {"pe": "tensor", "act": "scalar", "dve": "vector", "pool": "gpsimd", "sp": "sync"}
        with nc.Block() as block:
            for e in ENGS:
                def body(eng, e=e):
                    for wl, fn, inc_k, inc_v in streams[e]:
                        for k, v in wl:
                            eng.wait_ge(sems[k], v)
                        ins = fn(eng)
                        if inc_k is not None:
                            ins.then_inc(sems[inc_k], inc_v)
                    if e == "sp":
                        for k, v in fin.items():
                            eng.wait_ge(sems[k], v)
                getattr(block, emap[e])(body)
        for cm in reversed(self.ctx):
            cm.__exit__(None, None, None)
        self.ctx = []


def new_nc():
    return bass.Bass("TRN2", target_bir_lowering=False)


def din(nc, name, shape, dt=F32):
    return nc.dram_tensor(name, list(shape), dt, kind="ExternalInput").ap()


def dout(nc, name, shape, dt=F32):
    return nc.dram_tensor(name, list(shape), dt, kind="ExternalOutput").ap()


def run(nc, in_maps):
    res = run_bass_kernel_spmd(nc, in_maps, core_ids=list(range(NCORES)))
    return res.results


def fm(v):
    v = np.asarray(v)
    return np.ascontiguousarray(v.reshape(-1, 128).T)


NMC = 12


def build_p0():
    nc = new_nc()
    cond = din(nc, "cond", [128, KC, 2])
    adaw = din(nc, "adaw", [2, D, NMC * 128])
    adab = din(nc, "adab", [128, 2, NMC])
    o = dout(nc, "mod", [128, 2, NMC, 2])
    P = Prog(nc)
    cs = P.sbuf("cs", [128, KC, 2], F32)
    cb = P.sbuf("cb", [128, KC, 2], BF16)
    bs = P.sbuf("bs", [128, 2, NMC], F32)
    ob = P.sbuf("ob", [128, 2, NMC, 2], F32)
    wb = [P.sbuf("w%d" % l, [128, KC, NMC * 128], BF16) for l in range(2)]
    ps = P.psum("ps", [128, 2, NMC, 2], F32)
    P.dma("sp", cs[:], cond, writes=[cs])
    P.dma("sp", bs[:], adab, writes=[bs])
    for l in range(2):
        for h in range(2):
            P.dma("pool", wb[l][:, h * 8:(h + 1) * 8, :],
                  adaw[l, h * 1024:(h + 1) * 1024, :].rearrange("(kc p) n -> p kc n", p=128), writes=[wb[l]], acc=(h > 0))
    P.emit("act", lambda e: e.activation(out=cb[:], in_=cs[:], func=AF.Silu), reads=[cs], writes=[cb])
    for l in range(2):
        for j in range(NMC):
            for kc in range(KC):
                P.emit("pe", lambda e, l=l, j=j, kc=kc: e.matmul(
                    ps[:, l, j, :], lhsT=wb[l][:, kc, j * 128:(j + 1) * 128], rhs=cb[:, kc, :],
                    start=(kc == 0), stop=(kc == KC - 1)), reads=[wb[l], cb], writes=[ps],
                    inc=(kc == KC - 1 and j == NMC - 1))
        P.emit("dve", lambda e, l=l: e.tensor_tensor(
            out=ob[:, l], in0=ps[:, l], in1=bs[:, l].unsqueeze(2).to_broadcast([128, NMC, 2]), op=ALU.add),
            reads=[ps, bs], writes=[ob])
    P.dma("sp", o, ob[:], reads=[ob], is_output=True)
    P.finish()
    return nc


def run_p0(inp):
    nc = build_p0()
    cond = np.stack([fm(inp["c"][0]), fm(inp["c_ctx"])], axis=-1).astype(np.float32)
    maps = []
    for i in range(NCORES):
        sl = slice(i * NMC * 128, (i + 1) * NMC * 128)
        adab = np.stack([fm(inp["ada_b"][l, sl]) for l in range(2)], axis=1)
        maps.append({"cond": cond, "adaw": np.ascontiguousarray(inp["ada_w"][:, :, sl]),
                     "adab": np.ascontiguousarray(adab)})
    res = run(nc, maps)
    full = np.concatenate([r["mod"] for r in res], axis=2)
    return full


CHUNKS = [(0, 512), (512, 512), (1024, 512), (1536, 512), (2048, CPC)]
SUBCH = [(i * 128, 128) for i in range(16)] + [(2048, CPC)]


def build_p1():
    nc = new_nc()
    xT = din(nc, "xT", [D, TT])
    w = din(nc, "w", [D, INC])
    modv = din(nc, "modv", [128, 2, 2, KC])
    gain = din(nc, "gain", [128, KC])
    bgate = din(nc, "bgate", [128, 32])
    qkg = din(nc, "qkg", [128, 2])
    cosd = din(nc, "cosT", [128, TT])
    sind = din(nc, "sinT", [128, TT])
    rmd = din(nc, "rm", [128, 128], BF16)
    qT = dout(nc, "qT", [QW, TT], BF16)
    kT = dout(nc, "kT", [KVW, TT], BF16)
    vo = dout(nc, "v", [TT, KVW], BF16)
    fT = dout(nc, "fT", [FD, TT], BF16)
    gT = dout(nc, "gT", [2 * D, TT], BF16)
    P = Prog(nc)
    hT = P.sbuf("hT", [128, KC, TT], BF16)
    cosb = P.sbuf("cosb", [128, TT], F32)
    sinb = P.sbuf("sinb", [128, TT], F32)
    modb = P.sbuf("modb", [128, 2, 2, KC], F32)
    gb = P.sbuf("gb", [128, KC], F32)
    ab = P.sbuf("ab", [128, 2, KC], F32)
    bgb = P.sbuf("bgb", [128, 32], F32)
    qkb = P.sbuf("qkb", [128, 2], F32)
    qks = P.sbuf("qks", [128, 2], F32)
    rmb = P.sbuf("rmb", [128, 128], BF16)
    ones = P.sbuf("ones", [128, 128], BF16)
    xs_rot = P.rot("xs", 2, [128, KC, 128], F32)
    sq_rot = P.rot("sqc", 2, [128, KC, 128], BF16)
    rs_rot = P.rot("rs", 3, [128, 3, 512], F32)
    wrot = P.rot("wp", 3, [128, KC, 512], BF16)
    psm = P.rot("psm", 3, [128, 512], F32, psum=True)
    pss = P.rot("pss", 2, [128, 512], F32, psum=True)
    psr = P.rot("psr", 2, [128, 512], F32, psum=True)
    sqh = P.rot("sqh", 2, [128, 512], BF16)
    qn_rot = P.rot("qn", 3, [128, 512], BF16)
    t1_rot = P.rot("t1", 2, [128, 512], F32)
    t2_rot = P.rot("t2", 2, [128, 512], F32)
    ob_rot = P.rot("ob", 4, [128, 512], BF16)

    for (dst, src) in ((cosb, cosd), (sinb, sind), (modb, modv), (gb, gain), (bgb, bgate), (qkb, qkg), (rmb, rmd)):
        P.dma("sp", dst[:], src, writes=[dst])
    P.emit("pool", lambda e: e.memset(ones[:], 1.0), writes=[ones])
    epsb = P.sbuf("epsb", [128, 1], F32)
    P.emit("pool", lambda e: e.memset(epsb[:], float(HD * EPS)), writes=[epsb])
    for r in rs_rot.bufs:
        P.emit("pool", lambda e, r=r: e.memset(r[:, 2, :], -0.5), writes=[r])
    for wi in range(2):
        P.emit("dve", lambda e, wi=wi: e.tensor_scalar(out=ab[:, wi, :], in0=modb[:, wi, 1, :], scalar1=1.0,
                                                       scalar2=float(math.sqrt(D)), op0=ALU.add, op1=ALU.mult),
               reads=[modb], writes=[ab])
        P.emit("dve", lambda e, wi=wi: e.tensor_tensor(out=ab[:, wi, :], in0=ab[:, wi, :], in1=gb[:], op=ALU.mult),
               reads=[ab, gb], writes=[ab])
    P.emit("dve", lambda e: e.tensor_scalar(out=qks[:], in0=qkb[:], scalar1=float(math.sqrt(HD)), scalar2=None,
                                            op0=ALU.mult), reads=[qkb], writes=[qks])

    class V:
        pass
    a_lat = ab.t[:, 0, :]
    a_ctx = ab.t[:, 1, :]
    b_lat = modb.t[:, 0, 0, :]
    b_ctx = modb.t[:, 1, 0, :]

    for (t0, tn) in SUBCH:
        isctx = t0 >= TPC
        a, b = (a_ctx, b_ctx) if isctx else (a_lat, b_lat)
        xs = xs_rot.next()
        P.dma("sp", xs[:, :, 0:tn], xT[:, t0:t0 + tn].rearrange("(kc p) t -> p kc t", p=128), writes=[xs])
        sq = sq_rot.next()
        P.emit("act", lambda e, sq=sq, xs=xs, tn=tn: e.activation(out=sq[:, :, 0:tn], in_=xs[:, :, 0:tn], func=AF.Square),
               reads=[xs], writes=[sq])
        ps_ = pss.next()
        for kc in range(KC):
            P.emit("pe", lambda e, ps_=ps_, sq=sq, kc=kc, tn=tn: e.matmul(
                ps_[:, 0:tn], lhsT=ones[:], rhs=sq[:, kc, 0:tn], start=(kc == 0), stop=(kc == KC - 1)),
                reads=[sq, ones], writes=[ps_], inc=(kc == KC - 1))
        rs = rs_rot.next()
        P.emit("dve", lambda e, rs=rs, ps_=ps_, tn=tn: e.tensor_scalar(
            out=rs[:, 0, 0:tn], in0=ps_[:, 0:tn], scalar1=float(D * EPS), scalar2=None, op0=ALU.add),
            reads=[ps_], writes=[rs])
        P.emit("pool", lambda e, rs=rs, tn=tn: e.tensor_tensor(
            out=rs[:, 1, 0:tn], in0=rs[:, 0, 0:tn], in1=rs[:, 2, 0:tn], op=ALU.pow), reads=[rs], writes=[rs])
        for kc in range(KC):
            eng = "dve" if kc % 2 == 0 else "pool"
            P.emit(eng, lambda e, kc=kc, rs=rs, xs=xs, tn=tn: e.tensor_tensor(
                out=xs[:, kc, 0:tn], in0=xs[:, kc, 0:tn], in1=rs[:, 1, 0:tn], op=ALU.mult), reads=[xs, rs], writes=[xs])
            P.emit("act", lambda e, xs=xs, kc=kc, a=a, b=b, t0=t0, tn=tn: e.activation(
                out=hT[:, kc, t0:t0 + tn], in_=xs[:, kc, 0:tn], func=AF.Identity,
                bias=b[:, kc:kc + 1], scale=a[:, kc:kc + 1]), reads=[xs, ab, modb], writes=[hT])

    pending = []

    def flush(n_keep):
        while len(pending) > n_keep:
            st = pending.pop(0)
            st()

    def head_epilogue(ps_, j, t0, tn, is_q):
        gcol = 0 if is_q else 1
        st = {}

        def stageA():
            sq = sqh.next()
            st["sq"] = sq
            P.emit("act", lambda e: e.activation(out=sq[:, 0:tn], in_=ps_[:, 0:tn], func=AF.Square),
                   reads=[ps_], writes=[sq])

        def stageB():
            sq = st["sq"]
            p2 = pss.next()
            P.emit("pe", lambda e: e.matmul(p2[:, 0:tn], lhsT=ones[:], rhs=sq[:, 0:tn], start=True, stop=True),
                   reads=[sq, ones], writes=[p2])
            rs = rs_rot.next()
            P.emit("act", lambda e: e.activation(out=rs[:, 0, 0:tn], in_=p2[:, 0:tn], func=AF.Sqrt, bias=epsb[:, 0:1]),
                   reads=[p2, epsb], writes=[rs])
            P.emit("dve", lambda e: e.reciprocal(out=rs[:, 1, 0:tn], in_=rs[:, 0, 0:tn]), reads=[rs], writes=[rs])
            qn = qn_rot.next()
            st["qn"] = qn
            P.emit("dve", lambda e: e.scalar_tensor_tensor(
                out=qn[:, 0:tn], in0=ps_[:, 0:tn], scalar=qks[:, gcol:gcol + 1], in1=rs[:, 1, 0:tn],
                op0=ALU.mult, op1=ALU.mult), reads=[ps_, qks, rs], writes=[qn])

        def stageC():
            qn = st["qn"]
            p3 = psr.next()
            P.emit("pe", lambda e: e.matmul(p3[:, 0:tn], lhsT=rmb[:], rhs=qn[:, 0:tn], start=True, stop=True),
                   reads=[qn, rmb], writes=[p3])
            t1 = t1_rot.next()
            P.emit("dve", lambda e: e.tensor_tensor(out=t1[:, 0:tn], in0=qn[:, 0:tn], in1=cosb[:, t0:t0 + tn],
                                                    op=ALU.mult), reads=[qn, cosb], writes=[t1])
            t2 = t2_rot.next()
            P.emit("dve", lambda e: e.tensor_tensor(out=t2[:, 0:tn], in0=p3[:, 0:tn], in1=sinb[:, t0:t0 + tn],
                                                    op=ALU.mult), reads=[p3, sinb], writes=[t2])
            ob = ob_rot.next()
            P.emit("dve", lambda e: e.tensor_tensor(out=ob[:, 0:tn], in0=t1[:, 0:tn], in1=t2[:, 0:tn], op=ALU.add),
                   reads=[t1, t2], writes=[ob])
            dst = qT[j * 128:(j + 1) * 128, t0:t0 + tn] if is_q else kT[(j - 16) * 128:(j - 15) * 128, t0:t0 + tn]
            P.dma("sp", dst, ob[:, 0:tn], reads=[ob], is_output=True)

        stageA()
        pending.append(stageB)
        pending.append(stageC)

    for pn in range(16):
        wbuf = wrot.next()
        for h in range(2):
            P.dma("pool", wbuf[:, h * 8:(h + 1) * 8, :],
                  w[h * 1024:(h + 1) * 1024, pn * 512:(pn + 1) * 512].rearrange("(kc p) n -> p kc n", p=128),
                  writes=[wbuf], acc=(h > 0))
        if pn == 5:
            tiles = [(i * 128, 128) for i in range(16)] + [(2048, CPC)]
            for (t0, tn) in tiles:
                ps_ = psm.next()
                for kc in range(KC):
                    P.emit("pe", lambda e, ps_=ps_, kc=kc, t0=t0, tn=tn, wbuf=wbuf: e.matmul(
                        ps_[0:tn, :], lhsT=hT[:, kc, t0:t0 + tn], rhs=wbuf[:, kc, :], start=(kc == 0), stop=(kc == KC - 1)),
                        reads=[hT, wbuf], writes=[ps_], inc=(kc == KC - 1))
                ob = ob_rot.next()
                P.emit("act", lambda e, ob=ob, ps_=ps_, tn=tn: e.activation(out=ob[0:tn, :], in_=ps_[0:tn, :], func=AF.Copy),
                       reads=[ps_], writes=[ob])
                P.dma("sp", vo[t0:t0 + tn, :], ob[0:tn, :], reads=[ob], is_output=True)
                flush(1)
            continue
        for jj in range(4):
            j = pn * 4 + jj
            for (t0, tn) in CHUNKS:
                ps_ = psm.next()
                for kc in range(KC):
                    P.emit("pe", lambda e, ps_=ps_, kc=kc, t0=t0, tn=tn, wbuf=wbuf, jj=jj: e.matmul(
                        ps_[:, 0:tn], lhsT=wbuf[:, kc, jj * 128:(jj + 1) * 128], rhs=hT[:, kc, t0:t0 + tn],
                        start=(kc == 0), stop=(kc == KC - 1)),
                        reads=[hT, wbuf], writes=[ps_], inc=(kc == KC - 1))
                flush(1)
                if j < 20:
                    head_epilogue(ps_, j, t0, tn, j < 16)
                elif j < 32:
                    ob = ob_rot.next()
                    P.emit("act", lambda e, ob=ob, ps_=ps_, tn=tn: e.activation(out=ob[:, 0:tn], in_=ps_[:, 0:tn], func=AF.Copy),
                           reads=[ps_], writes=[ob])
                    P.dma("sp", fT[(j - 24) * 128:(j - 23) * 128, t0:t0 + tn], ob[:, 0:tn], reads=[ob], is_output=True)
                else:
                    g = j - 32
                    ob = ob_rot.next()
                    P.emit("act", lambda e, ob=ob, ps_=ps_, tn=tn, g=g: e.activation(
                        out=ob[:, 0:tn], in_=ps_[:, 0:tn], func=AF.Sigmoid, bias=bgb[:, g:g + 1]),
                        reads=[ps_, bgb], writes=[ob])
                    P.dma("sp", gT[g * 128:(g + 1) * 128, t0:t0 + tn], ob[:, 0:tn], reads=[ob], is_output=True)
    flush(0)
    P.finish()
    return nc


def rope_tables():
    t = np.arange(SEQ)
    row = (t // GRID_W).astype(np.float32)
    col = (t % GRID_W).astype(np.float32)
    inv = (10000.0 ** (-np.arange(32, dtype=np.float32) / 32)).astype(np.float32)
    ar = row[:, None] * inv
    ac = col[:, None] * inv
    ang = np.concatenate([ar, ar, ac, ac], axis=-1)
    return np.cos(ang).T.astype(np.float32), np.sin(ang).T.astype(np.float32)


def rot_matrix():
    R = np.zeros((128, 128), np.float32)
    for base in (0, 64):
        for i in range(32):
            R[base + 32 + i, base + i] = -1.0
            R[base + i, base + 32 + i] = 1.0
    return R.astype(NPBF)


def run_p1(nc, l, inp, xT_cores, mod):
    cosT, sinT = rope_tables()
    rm = rot_matrix()
    maps = []
    modv = np.stack([np.stack([mod[:, l, 0:16, wi], mod[:, l, 16:32, wi]], axis=1) for wi in range(2)], axis=1)
    modv = np.ascontiguousarray(modv.astype(np.float32))
    for i in range(NCORES):
        cs = np.concatenate([cosT[:, i * TPC:(i + 1) * TPC], np.ones((128, CPC), np.float32)], axis=1)
        sn = np.concatenate([sinT[:, i * TPC:(i + 1) * TPC], np.zeros((128, CPC), np.float32)], axis=1)
        maps.append({
            "xT": xT_cores[i], "w": inp["w_in"][l], "modv": modv, "gain": fm(inp["norm_attn_g"][l]),
            "bgate": fm(inp["b_gate"][l]),
            "qkg": np.ascontiguousarray(np.stack([inp["q_norm_g"][l], inp["k_norm_g"][l]], axis=1)),
            "cosT": np.ascontiguousarray(cs), "sinT": np.ascontiguousarray(sn), "rm": rm})
    return run(nc, maps)


NKEY = CTX + SEQ
NKC = NKEY // 128
ATTN_SCALE = HD ** -0.5


def build_p2(with_ctx):
    nc = new_nc()
    nq = SEQ + (CTX if with_ctx else 0)
    qT = din(nc, "qT", [2, 128, nq], BF16)
    kT = din(nc, "kT", [128, NKEY], BF16)
    v = din(nc, "v", [NKEY, 128], BF16)
    oT = dout(nc, "oT", [2, 128, nq], BF16)
    P = Prog(nc)
    kb = P.sbuf("kb", [128, NKEY], BF16)
    vb = P.sbuf("vb", [128, NKC, 128], BF16)
    ones = P.sbuf("ones", [128, 128], BF16)
    qrot = P.rot("qc", 3, [128, 512], BF16)
    prot = P.rot("pb", 3, [128, 1024], BF16)
    rrot = P.rot("ri", 2, [128, 512], F32)
    orot = P.rot("ob", 2, [128, 512], BF16)
    psS = P.rot("psS", 2, [128, 1024], F32, psum=True)
    psO = P.rot("psO", 2, [128, 512], F32, psum=True)
    psL = P.rot("psL", 2, [128, 512], F32, psum=True)
    hlrot = P.rot("hl", 2, [128, 2, 512], BF16)

    class _AccRot:
        def __init__(self):
            self.items = []
            for i in range(2):
                b0 = P.sbuf("acc%d" % i, [128, 2, 512], F32)
                b1 = Buf(b0.t, "acc%db" % i)
                self.items.append((b0, b1))
            self.i = 0

        def next(self):
            it = self.items[self.i % 2]
            self.i += 1
            return it
    accrot = _AccRot()
    P.emit("pool", lambda e: e.memset(ones[:], 1.0), writes=[ones])
    for h in range(4):
        c0, c1 = h * 4160, (h + 1) * 4160
        P.dma("sp", kb[:, c0:c1], kT[:, c0:c1], writes=[kb], acc=(h > 0))
    for h in range(5):
        c0, c1 = h * 26, (h + 1) * 26
        P.dma("sp", vb[:, c0:c1, :], v[c0 * 128:c1 * 128, :].rearrange("(c p) d -> p c d", p=128), writes=[vb], acc=(h > 0))
    qchunks = [(i * 512, 512, NKC) for i in range(SEQ // 512)]
    if with_ctx:
        qchunks.append((SEQ, CTX, CTX // 128))
    def do_chunk(hh, t0, tn, nk):
        qc = qrot.next()
        P.dma("sp", qc[:, 0:tn], qT[hh, :, t0:t0 + tn], writes=[qc])
        po = psO.next()
        pl = psL.next()
        accb = accrot.next()
        acc = accb[0].t
        accs = accb
        sbufs = {}

        def S2(kp):
            ps = psS.next()
            sbufs[kp] = ps
            for h in range(2):
                kc = 2 * kp + h
                P.emit("pe", lambda e, h=h, kc=kc: e.matmul(ps[:, h * 512:h * 512 + tn], lhsT=kb[:, kc * 128:(kc + 1) * 128],
                                                            rhs=qc[:, 0:tn], start=True, stop=True),
                       reads=[kb, qc], writes=[ps], inc=(h == 1))

        def step2(kp):
            ps = sbufs.pop(kp)
            pb = prot.next()
            pv = pb.t[:, :].rearrange("p (h t) -> p h t", t=512)[:, :, 0:tn]
            sv = ps.t[:, :].rearrange("p (h t) -> p h t", t=512)[:, :, 0:tn]
            P.emit("act", lambda e: e.activation(out=pv, in_=sv, func=AF.Exp, scale=float(ATTN_SCALE)), reads=[ps], writes=[pb])
            for h in range(2):
                kc = 2 * kp + h

                def one(h=h, kc=kc):
                    P.emit("pe", lambda e: e.matmul(po[:, 0:tn], lhsT=vb[:, kc, :], rhs=pb[:, h * 512:h * 512 + tn],
                                                    start=(kc == 0), stop=(kc == nk - 1)),
                           reads=[vb, pb], writes=[po], inc=(kc == nk - 1))
                    eng = "dve"
                    if kc < 2:
                        P.emit(eng, lambda e: e.tensor_copy(out=acc[:, h, 0:tn], in_=pb[:, h * 512:h * 512 + tn]),
                               reads=[pb], writes=[accs[h]])
                    else:
                        P.emit(eng, lambda e: e.tensor_tensor(out=acc[:, h, 0:tn], in0=acc[:, h, 0:tn],
                                                              in1=pb[:, h * 512:h * 512 + tn], op=ALU.add),
                               reads=[pb, accs[h]], writes=[accs[h]])
                one()
        npairs = nk // 2
        S2(0)
        for kp in range(npairs):
            if kp + 1 < npairs:
                S2(kp + 1)
            step2(kp)
        P.emit("dve", lambda e: e.tensor_tensor(out=acc[:, 0, 0:tn], in0=acc[:, 0, 0:tn], in1=acc[:, 1, 0:tn], op=ALU.add),
               reads=[accs[0], accs[1]], writes=[accs[0]])
        hl = hlrot.next()
        P.emit("dve", lambda e: e.tensor_copy(out=hl[:, 0, 0:tn], in_=acc[:, 0, 0:tn]), reads=[accs[0]], writes=[hl])
        P.emit("dve", lambda e: e.tensor_tensor(out=hl[:, 1, 0:tn], in0=acc[:, 0, 0:tn], in1=hl[:, 0, 0:tn], op=ALU.subtract),
               reads=[accs[0], hl], writes=[hl])
        P.emit("pe", lambda e: e.matmul(pl[:, 0:tn], lhsT=ones[:], rhs=hl[:, 0, 0:tn], start=True, stop=False),
               reads=[ones, hl], writes=[pl], inc=False)
        P.emit("pe", lambda e: e.matmul(pl[:, 0:tn], lhsT=ones[:], rhs=hl[:, 1, 0:tn], start=False, stop=True),
               reads=[ones, hl], writes=[pl])
        ri = rrot.next()
        P.emit("dve", lambda e: e.reciprocal(out=ri[:, 0:tn], in_=pl[:, 0:tn]), reads=[pl], writes=[ri])
        ob = orot.next()
        P.emit("dve", lambda e: e.tensor_tensor(out=ob[:, 0:tn], in0=po[:, 0:tn], in1=ri[:, 0:tn],
                                                op=ALU.mult), reads=[po, ri], writes=[ob])
        P.dma("sp", oT[hh, :, t0:t0 + tn], ob[:, 0:tn], reads=[ob], is_output=True)

    for hh in range(2):
        for (t0, tn, nk) in qchunks:
            do_chunk(hh, t0, tn, nk)
    P.finish()
    return nc


def run_p2(nc, with_ctx, qT_full, kT_full, v_full):
    maps = []
    for i in range(NCORES):
        kv = i // 2
        maps.append({"qT": np.ascontiguousarray(qT_full[i * 256:(i + 1) * 256].reshape(2, 128, -1)),
                     "kT": np.ascontiguousarray(kT_full[kv * 128:(kv + 1) * 128]),
                     "v": np.ascontiguousarray(v_full[:, kv * 128:(kv + 1) * 128])})
    res = run(nc, maps)
    return np.concatenate([r["oT"].reshape(256, -1) for r in res], axis=0)


def build_p3a():
    nc = new_nc()
    fT = din(nc, "fT", [256, SEQ], BF16)
    ccs_d = din(nc, "ccs", [128, 2, 256], BF16)
    YT = dout(nc, "YT", [2, 128, SEQ], BF16)
    P = Prog(nc)
    fb = P.sbuf("fb", [128, 2, SEQ], BF16)
    ccs = P.sbuf("ccs_s", [128, 2, 256], BF16)
    yb = P.sbuf("yb", [128, 2, SEQ], BF16)
    psr = P.rot("ps", 4, [128, 512], F32, psum=True)
    P.dma("sp", ccs[:], ccs_d, writes=[ccs])
    for cc in range(2):
        for h in range(4):
            P.dma("sp", fb[:, cc, h * 4096:(h + 1) * 4096], fT[cc * 128:(cc + 1) * 128, h * 4096:(h + 1) * 4096],
                  writes=[fb], acc=(cc + h > 0))
    n = 0
    for tc in range(SEQ // 512):
        for ri in range(2):
            ps = psr.next()
            for cc in range(2):
                P.emit("pe", lambda e, ps=ps, tc=tc, ri=ri, cc=cc: e.matmul(
                    ps[:], lhsT=ccs[:, cc, ri * 128:(ri + 1) * 128], rhs=fb[:, cc, tc * 512:(tc + 1) * 512],
                    start=(cc == 0), stop=(cc == 1)), reads=[ccs, fb], writes=[ps], inc=(cc == 1))
            if n % 2 == 0:
                P.emit("act", lambda e, ps=ps, tc=tc, ri=ri: e.activation(out=yb[:, ri, tc * 512:(tc + 1) * 512], in_=ps[:], func=AF.Copy),
                       reads=[ps], writes=[yb])
            else:
                P.emit("dve", lambda e, ps=ps, tc=tc, ri=ri: e.tensor_copy(out=yb[:, ri, tc * 512:(tc + 1) * 512], in_=ps[:]),
                       reads=[ps], writes=[yb])
            n += 1
    for ri in range(2):
        for h in range(4):
            P.dma("sp", YT[ri, :, h * 4096:(h + 1) * 4096], yb[:, ri, h * 4096:(h + 1) * 4096], reads=[yb], is_output=True)
    P.finish()
    return nc


def build_p3b(with_ctx):
    nc = new_nc()
    Yd = din(nc, "Y", [2, 128, SEQ], BF16)
    fc_d = din(nc, "fc", [256, CTX], BF16)
    ccs_d = din(nc, "ccs", [128, 2, 256], BF16)
    w1_d = din(nc, "w1", [128, 2, 256], BF16)
    tw_d = din(nc, "tw", [128, 2, 512])
    w2_d = din(nc, "w2", [128, 2, 128], BF16)
    w256_d = din(nc, "w256", [128, 2, 2, 256], BF16)
    Fo = dout(nc, "Fo", [128, SEQ], BF16)
    Fc = dout(nc, "Fc", [128, CTX], BF16)
    P = Prog(nc)
    fb = P.sbuf("fb", [128, 2, SEQ], BF16)
    yr = P.sbuf("yr", [128, SEQ], BF16)
    yi = P.sbuf("yi", [128, SEQ], BF16)
    ccs = P.sbuf("ccs_s", [128, 2, 256], BF16)
    w1 = P.sbuf("w1_s", [128, 2, 256], BF16)
    tw = P.sbuf("tw_s", [128, 2, 512], F32)
    w2 = P.sbuf("w2_s", [128, 2, 128], BF16)
    w256 = P.sbuf("w256_s", [128, 2, 2, 256], BF16)
    fcb = P.sbuf("fcb", [128, 2, CTX], BF16)
    ycb = P.sbuf("ycb", [128, 2, 256], BF16)
    ocb = P.sbuf("ocb", [128, CTX], BF16)
    arot = P.rot("A", 2, [128, 512], F32)
    brot = P.rot("B", 2, [128, 512], F32)
    psr = P.rot("ps", 4, [128, 512], F32, psum=True)
    for (dst, src) in ((ccs, ccs_d), (w1, w1_d), (tw, tw_d), (w2, w2_d), (w256, w256_d)):
        P.dma("sp", dst[:], src, writes=[dst])
    for h in range(4):
        P.dma("sp", yr[:, h * 4096:(h + 1) * 4096], Yd[0, :, h * 4096:(h + 1) * 4096], writes=[yr], acc=(h > 0))
    for h in range(4):
        P.dma("sp", yi[:, h * 4096:(h + 1) * 4096], Yd[1, :, h * 4096:(h + 1) * 4096], writes=[yi], acc=(h > 0))
    yrv = yr.t[:, :].rearrange("p (j n) -> p j n", n=128)
    yiv = yi.t[:, :].rearrange("p (j n) -> p j n", n=128)
    for jp in range(64):
        ps = psr.next()
        psv = ps.t[:, :].rearrange("p (a c) -> p a c", c=256)
        for q in range(2):
            j = jp * 2 + q
            P.emit("pe", lambda e, psv=psv, q=q, j=j: e.matmul(psv[:, q, :], lhsT=yrv[:, j, :], rhs=w1[:, 0, :],
                                                               start=True, stop=False), reads=[yr, w1], writes=[ps], inc=False)
            P.emit("pe", lambda e, psv=psv, q=q, j=j: e.matmul(psv[:, q, :], lhsT=yiv[:, j, :], rhs=w1[:, 1, :],
                                                               start=False, stop=True), reads=[yi, w1], writes=[ps], inc=(q == 1))
        A = arot.next()
        B = brot.next()
        P.emit("dve", lambda e, A=A, ps=ps: e.tensor_tensor(out=A[:], in0=ps[:], in1=tw[:, 0, :], op=ALU.mult),
               reads=[ps, tw], writes=[A])
        P.emit("dve", lambda e, B=B, ps=ps: e.tensor_tensor(out=B[:], in0=ps[:], in1=tw[:, 1, :], op=ALU.mult),
               reads=[ps, tw], writes=[B])
        for q in range(2):
            o0 = jp * 256 + q * 128
            P.emit("pool", lambda e, A=A, B=B, q=q, o0=o0: e.tensor_tensor(
                out=fb[:, 0, o0:o0 + 128], in0=A[:, q * 256:q * 256 + 128], in1=B[:, q * 256 + 128:q * 256 + 256],
                op=ALU.subtract), reads=[A, B], writes=[fb])
            P.emit("pool", lambda e, A=A, B=B, q=q, o0=o0: e.tensor_tensor(
                out=fb[:, 1, o0:o0 + 128], in0=B[:, q * 256:q * 256 + 128], in1=A[:, q * 256 + 128:q * 256 + 256],
                op=ALU.add), reads=[A, B], writes=[fb])
    for cq in range(32):
        ps = psr.next()
        P.emit("pe", lambda e, ps=ps, cq=cq: e.matmul(ps[:], lhsT=w2[:, 0, :], rhs=fb[:, 0, cq * 512:(cq + 1) * 512],
                                                      start=True, stop=False), reads=[fb, w2], writes=[ps], inc=False)
        P.emit("pe", lambda e, ps=ps, cq=cq: e.matmul(ps[:], lhsT=w2[:, 1, :], rhs=fb[:, 1, cq * 512:(cq + 1) * 512],
                                                      start=False, stop=True), reads=[fb, w2], writes=[ps])
        if cq % 2 == 0:
            P.emit("act", lambda e, ps=ps, cq=cq: e.activation(out=yr[:, cq * 512:(cq + 1) * 512], in_=ps[:], func=AF.Copy,
                                                               scale=float(1.0 / 2048.0)), reads=[ps], writes=[yr])
        else:
            P.emit("dve", lambda e, ps=ps, cq=cq: e.tensor_scalar(out=yr[:, cq * 512:(cq + 1) * 512], in0=ps[:],
                                                                  scalar1=float(1.0 / 2048.0), scalar2=None, op0=ALU.mult),
                   reads=[ps], writes=[yr])
    for h in range(4):
        P.dma("sp", Fo[:, h * 4096:(h + 1) * 4096], yr[:, h * 4096:(h + 1) * 4096], reads=[yr], is_output=True)
    if with_ctx:
        for cc in range(2):
            P.dma("sp", fcb[:, cc, :], fc_d[cc * 128:(cc + 1) * 128, :], writes=[fcb], acc=(cc > 0))
        for tt in range(2):
            ps = psr.next()
            for cc in range(2):
                P.emit("pe", lambda e, ps=ps, tt=tt, cc=cc: e.matmul(
                    ps[:, 0:256], lhsT=fcb[:, cc, tt * 128:(tt + 1) * 128], rhs=ccs[:, cc, :], start=(cc == 0), stop=(cc == 1)),
                    reads=[fcb, ccs], writes=[ps], inc=(cc == 1))
            P.emit("act", lambda e, ps=ps, tt=tt: e.activation(out=ycb[:, tt, :], in_=ps[:, 0:256], func=AF.Copy),
                   reads=[ps], writes=[ycb])
        ps = psr.next()
        n = 0
        for tt in range(2):
            for ri in range(2):
                P.emit("pe", lambda e, ps=ps, tt=tt, ri=ri, n=n: e.matmul(
                    ps[:, 0:256], lhsT=ycb[:, tt, ri * 128:(ri + 1) * 128], rhs=w256[:, tt, ri, :], start=(n == 0), stop=(n == 3)),
                    reads=[ycb, w256], writes=[ps], inc=(n == 3))
                n += 1
        P.emit("act", lambda e, ps=ps: e.activation(out=ocb[:], in_=ps[:, 0:256], func=AF.Copy, scale=float(1.0 / 256.0)),
               reads=[ps], writes=[ocb])
    else:
        P.emit("pool", lambda e: e.memset(ocb[:], 0.0), writes=[ocb])
    P.dma("sp", Fc, ocb[:], reads=[ocb], is_output=True)
    P.finish()
    return nc


def p3_consts(half):
    p = np.arange(128)
    out = {}
    ccs = np.zeros((128, 2, 256), np.float64)
    j = 128 * half + np.arange(128)
    for cc in range(2):
        c = cc * 128 + p
        ang = 2 * np.pi * np.outer(c, j) / 256.0
        ccs[:, cc, 0:128] = np.cos(ang)
        ccs[:, cc, 128:256] = np.sin(ang)
    out["ccs"] = ccs.astype(NPBF)
    a128 = 2 * np.pi * np.outer(p, p) / 128.0
    C, S = np.cos(a128), np.sin(a128)
    w1 = np.zeros((128, 2, 256))
    w1[:, 0, 0:128] = C
    w1[:, 0, 128:256] = S
    w1[:, 1, 0:128] = -S
    w1[:, 1, 128:256] = C
    out["w1"] = w1.astype(NPBF)
    psi = 2 * np.pi * np.outer(p, p) / float(SEQ)
    tw = np.zeros((128, 2, 512))
    tw[:, 0, :] = np.tile(np.cos(psi), (1, 4))
    tw[:, 1, :] = np.tile(np.sin(psi), (1, 4))
    out["tw"] = tw.astype(np.float32)
    w2 = np.zeros((128, 2, 128))
    w2[:, 0] = C
    w2[:, 1] = -S
    out["w2"] = w2.astype(NPBF)
    w256 = np.zeros((128, 2, 2, 256))
    for tt in range(2):
        n = tt * 128 + p
        ang = 2 * np.pi * np.outer(n, np.arange(256)) / 256.0
        w256[:, tt, 0] = np.cos(ang)
        w256[:, tt, 1] = -np.sin(ang)
    out["w256"] = w256.astype(NPBF)
    out["ident"] = np.eye(128).astype(NPBF)
    return out


def run_p3(nca, ncb, fT_full):
    consts = [p3_consts(h) for h in range(2)]
    maps = []
    for i in range(NCORES):
        g, half = i // 2, i % 2
        maps.append({"fT": np.ascontiguousarray(fT_full[g * 256:(g + 1) * 256, :SEQ]), "ccs": consts[half]["ccs"]})
    ra = run(nca, maps)
    maps = []
    for i in range(NCORES):
        g, half = i // 2, i % 2
        c = consts[half]
        YT = ra[i]["YT"]
        Y = np.ascontiguousarray(YT.reshape(2, 128, 128, 128).transpose(0, 2, 1, 3)).reshape(2, 128, SEQ)
        maps.append({"Y": Y, "fc": np.ascontiguousarray(fT_full[g * 256:(g + 1) * 256, SEQ:]), "ccs": c["ccs"], "w1": c["w1"],
                     "tw": c["tw"], "w2": c["w2"], "w256": c["w256"]})
    rb = run(ncb, maps)
    outs = []
    for i in range(NCORES):
        Fo = rb[i]["Fo"].reshape(128, 128, 128)
        lat = np.ascontiguousarray(Fo.transpose(1, 0, 2)).reshape(128, SEQ)
        outs.append(np.concatenate([lat, rb[i]["Fc"]], axis=1))
    return np.concatenate(outs, axis=0)


DBG = {}


def build_p4(moe, with_ctx, last):
    nc = new_nc()
    ntok = TT if with_ctx else TPC
    xT = din(nc, "xT", [D, ntok])
    at_d = din(nc, "attnT", [QW, ntok], BF16)
    ft_d = din(nc, "FT", [FD, ntok], BF16)
    gt_d = din(nc, "gT", [2 * D, ntok], BF16)
    wp = din(nc, "wp", [QW, D])
    wf = din(nc, "wf", [FD, D])
    wo = din(nc, "wo", [D, D])
    if moe:
        rw_d = din(nc, "rw", [128, KC, 8])
        mg = din(nc, "mg", [NE, D, DFE])
        mu = din(nc, "mu", [NE, D, DFE])
        md = din(nc, "md", [NE, DFE, D])
        id_d = din(nc, "ident", [128, 128], BF16)
    else:
        wg = din(nc, "wg", [D, DFF])
        wu = din(nc, "wu", [D, DFF])
        wd = din(nc, "wd", [DFF, D])
    modv = din(nc, "modv", [128, 2, 4, KC])
    gain2 = din(nc, "gain2", [128, KC])
    fgain = din(nc, "fgain", [128, KC])
    xo = dout(nc, "xo", [D, ntok])
    P = Prog(nc)
    xs = P.sbuf("xs", [128, KC, 512], F32)
    atb = P.sbuf("atb", [128, KC, 512], BF16)
    fg = P.sbuf("fg", [128, (40 if moe else 44) * 512], BF16)
    yT = P.sbuf("yT", [128, KC, 512], BF16)
    ones = P.sbuf("ones", [128, 128], BF16)
    modb = P.sbuf("modb", [128, 2, 4, KC], F32)
    g2b = P.sbuf("g2b", [128, KC], F32)
    fgb = P.sbuf("fgb", [128, KC], F32)
    a2b = P.sbuf("a2b", [128, 2, KC], F32)
    wrot = P.rot("wb", 3, [128, 8192], BF16)
    t1r = P.rot("t1", 2, [128, 512], F32)
    t2r = P.rot("t2", 2, [128, 512], F32)
    sr = P.rot("sl", 2, [128, 512], F32)
    rsr = P.rot("rs", 2, [128, 3, 512], F32)
    psm = P.rot("psm", 6, [128, 512], F32, psum=True)
    pss = P.rot("pss", 2, [128, 512], F32, psum=True)
    ftv = fg.t[:, 0:8 * 512].rearrange("p (k t) -> p k t", t=512)
    gtv = fg.t[:, 8 * 512:40 * 512].rearrange("p (k t) -> p k t", t=512)
    aTv = fg.t[:, :].rearrange("p (k t) -> p k t", t=512)
    for (dst, src) in ((modb, modv), (g2b, gain2), (fgb, fgain)):
        P.dma("sp", dst[:], src, writes=[dst])
    P.emit("pool", lambda e: e.memset(ones[:], 1.0), writes=[ones])
    for r in rsr.bufs:
        P.emit("pool", lambda e, r=r: e.memset(r[:, 2, :], -0.5), writes=[r])
    for wi in range(2):
        P.emit("dve", lambda e, wi=wi: e.tensor_scalar(out=a2b[:, wi, :], in0=modb[:, wi, 2, :], scalar1=1.0,
                                                       scalar2=float(math.sqrt(D)), op0=ALU.add, op1=ALU.mult),
               reads=[modb], writes=[a2b])
        P.emit("dve", lambda e, wi=wi: e.tensor_tensor(out=a2b[:, wi, :], in0=a2b[:, wi, :], in1=g2b[:], op=ALU.mult),
               reads=[a2b, g2b], writes=[a2b])
    P.emit("dve", lambda e: e.tensor_scalar(out=fgb[:], in0=fgb[:], scalar1=float(math.sqrt(D)), scalar2=None, op0=ALU.mult),
           reads=[fgb], writes=[fgb])
    if moe:
        rwf = P.sbuf("rwf", [128, KC, 8], F32)
        rwb = P.sbuf("rwb", [128, KC, 16], BF16)
        ident = P.sbuf("ident_s", [128, 128], BF16)
        bcs = P.sbuf("bcs", [128, NE, 512], F32)
        l16 = P.sbuf("l16", [128, 16], F32)
        sm = P.sbuf("sm", [128, 8, 8], F32)
        sc1 = P.sbuf("sc1", [128, 8], F32)
        rep = P.rot("rep", 2, [128, 2, NE, 128], BF16)
        u2r = P.rot("u2", 2, [128, 512], F32)
        P.dma("sp", rwf[:], rw_d, writes=[rwf])
        P.dma("sp", ident[:], id_d, writes=[ident])
        P.emit("act", lambda e: e.activation(out=rwb[:, :, 0:8], in_=rwf[:], func=AF.Copy), reads=[rwf], writes=[rwb])
        P.emit("dve", lambda e: e.tensor_tensor(out=rwb[:, :, 8:16], in0=rwf[:], in1=rwb[:, :, 0:8], op=ALU.subtract),
               reads=[rwf, rwb], writes=[rwb])

    def load_w(src2d, k0, nkc, n0, ncols):
        wb = wrot.next()
        view = wb.t[:, 0:nkc * ncols].rearrange("p (k n) -> p k n", n=ncols)
        first = True
        for h0 in range(0, nkc, 8):
            h1 = min(nkc, h0 + 8)
            P.dma("pool", view[:, h0:h1, :],
                  src2d[k0 + h0 * 128:k0 + h1 * 128, n0:n0 + ncols].rearrange("(kc p) n -> p kc n", p=128),
                  writes=[wb], acc=(not first))
            first = False
        return wb, view

    def mm_group(ps, tn, parts, inc_last=True):
        n = len(parts)
        for i, (wbuf, lhsT, rbuf, rhs) in enumerate(parts):
            P.emit("pe", lambda e, lhsT=lhsT, rhs=rhs, i=i: e.matmul(ps[:, 0:tn], lhsT=lhsT, rhs=rhs, start=(i == 0), stop=(i == n - 1)),
                   reads=[wbuf, rbuf], writes=[ps], inc=(inc_last and i == n - 1))

    def resid(ps, j, tn, gcol, wi):
        P.emit("dve", lambda e: e.scalar_tensor_tensor(out=xs[:, j, 0:tn], in0=ps[:, 0:tn], scalar=modb[:, wi, gcol, j:j + 1],
                                                       in1=xs[:, j, 0:tn], op0=ALU.mult, op1=ALU.add),
               reads=[ps, modb, xs], writes=[xs])

    def rstd_of_xs(tn):
        P.emit("act", lambda e: e.activation(out=yT[:, :, 0:tn], in_=xs[:, :, 0:tn], func=AF.Square), reads=[xs], writes=[yT])
        ps_ = pss.next()
        mm_group(ps_, tn, [(ones, ones[:], yT, yT[:, kc, 0:tn]) for kc in range(KC)])
        rs = rsr.next()
        P.emit("dve", lambda e: e.tensor_scalar(out=rs[:, 0, 0:tn], in0=ps_[:, 0:tn], scalar1=float(D * EPS), scalar2=None,
                                                op0=ALU.add), reads=[ps_], writes=[rs])
        P.emit("pool", lambda e: e.tensor_tensor(out=rs[:, 1, 0:tn], in0=rs[:, 0, 0:tn], in1=rs[:, 2, 0:tn], op=ALU.pow),
               reads=[rs], writes=[rs])
        return rs

    def ffn_gu(c, tn, wgb, wgv, wub, wuv, jj, bc_e=None):
        psG = psm.next()
        mm_group(psG, tn, [(wgb, wgv[:, kc, jj * 128:(jj + 1) * 128], atb, atb[:, kc, 0:tn]) for kc in range(KC)])
        psU = psm.next()
        mm_group(psU, tn, [(wub, wuv[:, kc, jj * 128:(jj + 1) * 128], atb, atb[:, kc, 0:tn]) for kc in range(KC)])
        s_ = sr.next()
        P.emit("act", lambda e: e.activation(out=s_[:, 0:tn], in_=psG[:, 0:tn], func=AF.Silu), reads=[psG], writes=[s_])
        if bc_e is None:
            P.emit("dve", lambda e: e.tensor_tensor(out=aTv[:, c, 0:tn], in0=s_[:, 0:tn], in1=psU[:, 0:tn], op=ALU.mult),
                   reads=[s_, psU], writes=[fg])
        else:
            u2 = u2r.next()
            P.emit("dve", lambda e: e.tensor_tensor(out=u2[:, 0:tn], in0=psU[:, 0:tn], in1=bcs[:, bc_e, 0:tn], op=ALU.mult),
                   reads=[psU, bcs], writes=[u2])
            P.emit("dve", lambda e: e.tensor_tensor(out=aTv[:, c, 0:tn], in0=s_[:, 0:tn], in1=u2[:, 0:tn], op=ALU.mult),
                   reads=[s_, u2], writes=[fg])

    def router_tile(tt, rstage=9):
        psR = pss.next()
        parts = [(atb, atb[:, kc, tt * 128:(tt + 1) * 128], rwb, rwb[:, kc, 0:16]) for kc in range(KC)]
        n = len(parts)
        for i, (wbuf, lhsT, rbuf, rhs) in enumerate(parts):
            P.emit("pe", lambda e, lhsT=lhsT, rhs=rhs, i=i: e.matmul(psR[:, 0:16], lhsT=lhsT, rhs=rhs, start=(i == 0), stop=False),
                   reads=[wbuf, rbuf], writes=[psR], inc=False)
        for kc in range(KC):
            P.emit("pe", lambda e, kc=kc: e.matmul(psR[:, 0:8], lhsT=yT[:, kc, tt * 128:(tt + 1) * 128], rhs=rwb[:, kc, 0:8],
                                                   start=False, stop=(kc == KC - 1)),
                   reads=[yT, rwb], writes=[psR], inc=(kc == KC - 1))
        P.emit("dve", lambda e: e.tensor_copy(out=l16[:], in_=psR[:, 0:16]), reads=[psR], writes=[l16])
        if rstage < 1:
            if tt == 0:
                P.emit("pool", lambda e: e.memset(bcs[:], 0.5), writes=[bcs])
            return
        lg, m1, mk1, l2, m2, mk2, dd = (sm[:, 0, :], sm[:, 1, 0:1], sm[:, 2, :], sm[:, 3, :], sm[:, 1, 1:2], sm[:, 4, :], sm[:, 1, 2:3])
        ee, den, w1, w2 = sm[:, 1, 3:4], sm[:, 1, 4:5], sm[:, 1, 5:6], sm[:, 1, 6:7]
        comb = sm[:, 5, :]
        D_ = lambda fn: P.emit("dve", fn, reads=[sm, l16], writes=[sm])
        D_(lambda e: e.tensor_tensor(out=lg, in0=l16[:, 0:8], in1=l16[:, 8:16], op=ALU.add))
        D_(lambda e: e.reduce_max(out=m1, in_=lg, axis=AX.X))
        D_(lambda e: e.tensor_scalar(out=mk1, in0=lg, scalar1=m1, scalar2=None, op0=ALU.is_equal))
        D_(lambda e: e.scalar_tensor_tensor(out=l2, in0=mk1, scalar=-1e30, in1=lg, op0=ALU.mult, op1=ALU.add))
        D_(lambda e: e.reduce_max(out=m2, in_=l2, axis=AX.X))
        D_(lambda e: e.tensor_scalar(out=mk2, in0=l2, scalar1=m2, scalar2=None, op0=ALU.is_equal))
        D_(lambda e: e.tensor_tensor(out=dd, in0=m2, in1=m1, op=ALU.subtract))
        P.emit("act", lambda e: e.activation(out=ee, in_=dd, func=AF.Exp), reads=[sm], writes=[sm])
        D_(lambda e: e.tensor_scalar(out=den, in0=ee, scalar1=1.0, scalar2=None, op0=ALU.add))
        D_(lambda e: e.reciprocal(out=w1, in_=den))
        D_(lambda e: e.tensor_tensor(out=w2, in0=ee, in1=w1, op=ALU.mult))
        D_(lambda e: e.tensor_scalar(out=comb, in0=mk1, scalar1=w1, scalar2=None, op0=ALU.mult))
        D_(lambda e: e.scalar_tensor_tensor(out=comb, in0=mk2, scalar=w2, in1=comb, op0=ALU.mult, op1=ALU.add))
        if rstage < 2:
            if tt == 0:
                P.emit("pool", lambda e: e.memset(bcs[:], 0.5), writes=[bcs])
            return
        rp = rep.next()
        cb = comb.unsqueeze(2).to_broadcast([128, NE, 128])
        P.emit("dve", lambda e: e.tensor_copy(out=rp[:, 0], in_=cb), reads=[sm], writes=[rp])
        P.emit("dve", lambda e: e.tensor_tensor(out=rp[:, 1], in0=cb, in1=rp[:, 0], op=ALU.subtract), reads=[sm, rp], writes=[rp])
        if rstage < 3:
            if tt == 0:
                P.emit("pool", lambda e: e.memset(bcs[:], 0.5), writes=[bcs])
            return
        for e_ in range(NE):
            def bc_one(e_=e_):
                pb_ = psm.next()
                P.emit("pe", lambda e: e.matmul(pb_[:, 0:128], lhsT=rp[:, 0, e_, :], rhs=ident[:], start=True, stop=False),
                       reads=[rp, ident], writes=[pb_], inc=False)
                P.emit("pe", lambda e: e.matmul(pb_[:, 0:128], lhsT=rp[:, 1, e_, :], rhs=ident[:], start=False, stop=True),
                       reads=[rp, ident], writes=[pb_])
                if e_ % 2 == 0:
                    P.emit("act", lambda e: e.activation(out=bcs[:, e_, tt * 128:(tt + 1) * 128], in_=pb_[:, 0:128], func=AF.Copy),
                           reads=[pb_], writes=[bcs])
                else:
                    P.emit("dve", lambda e: e.tensor_copy(out=bcs[:, e_, tt * 128:(tt + 1) * 128], in_=pb_[:, 0:128]),
                           reads=[pb_], writes=[bcs])
            bc_one()

    def do_chunk(t0, tn, wi):
        P.dma("sp", xs[:, :, 0:tn], xT[:, t0:t0 + tn].rearrange("(kc p) t -> p kc t", p=128), writes=[xs])
        for h in range(2):
            P.dma("sp", atb[:, h * 8:(h + 1) * 8, 0:tn], at_d[h * 1024:(h + 1) * 1024, t0:t0 + tn].rearrange("(kc p) t -> p kc t", p=128),
                  writes=[atb], acc=(h > 0))
        P.dma("sp", ftv[:, :, 0:tn], ft_d[:, t0:t0 + tn].rearrange("(kc p) t -> p kc t", p=128), writes=[fg])
        for h in range(4):
            P.dma("sp", gtv[:, h * 8:(h + 1) * 8, 0:tn], gt_d[h * 1024:(h + 1) * 1024, t0:t0 + tn].rearrange("(kc p) t -> p kc t", p=128),
                  writes=[fg], acc=True)
        for pn in range(4):
            wpb, wpv = load_w(wp, 0, KC, pn * 512, 512)
            wfb, wfv = load_w(wf, 0, 8, pn * 512, 512)
            for jj in range(4):
                j = pn * 4 + jj

                def ya(j=j, jj=jj, wpb=wpb, wpv=wpv, wfb=wfb, wfv=wfv):
                    psA = psm.next()
                    mm_group(psA, tn, [(wpb, wpv[:, kc, jj * 128:(jj + 1) * 128], atb, atb[:, kc, 0:tn]) for kc in range(KC)])
                    psB = psm.next()
                    mm_group(psB, tn, [(wfb, wfv[:, kc, jj * 128:(jj + 1) * 128], fg, ftv[:, kc, 0:tn]) for kc in range(8)])
                    t1 = t1r.next()
                    t2 = t2r.next()
                    P.emit("dve", lambda e: e.tensor_tensor(out=t1[:, 0:tn], in0=psA[:, 0:tn], in1=gtv[:, j, 0:tn], op=ALU.mult),
                           reads=[psA, fg], writes=[t1])
                    P.emit("dve", lambda e: e.tensor_tensor(out=t2[:, 0:tn], in0=psB[:, 0:tn], in1=gtv[:, 16 + j, 0:tn], op=ALU.mult),
                           reads=[psB, fg], writes=[t2])
                    P.emit("dve", lambda e: e.tensor_tensor(out=yT[:, j, 0:tn], in0=t1[:, 0:tn], in1=t2[:, 0:tn], op=ALU.add),
                           reads=[t1, t2], writes=[yT])
                ya()
        for pn in range(4):
            wob, wov = load_w(wo, 0, KC, pn * 512, 512)
            for jj in range(4):
                j = pn * 4 + jj
                psZ = psm.next()
                mm_group(psZ, tn, [(wob, wov[:, kc, jj * 128:(jj + 1) * 128], yT, yT[:, kc, 0:tn]) for kc in range(KC)])
                resid(psZ, j, tn, 0, wi)
        rs = rstd_of_xs(tn)
        for kc in range(KC):
            def hk(kc=kc):
                t1 = t1r.next()
                P.emit("dve", lambda e: e.tensor_tensor(out=t1[:, 0:tn], in0=xs[:, kc, 0:tn], in1=rs[:, 1, 0:tn],
                                                                                   op=ALU.mult), reads=[xs, rs], writes=[t1])
                if not moe:
                    P.emit("act", lambda e: e.activation(out=atb[:, kc, 0:tn], in_=t1[:, 0:tn], func=AF.Identity,
                                                         bias=modb[:, wi, 1, kc:kc + 1], scale=a2b[:, wi, kc:kc + 1]),
                           reads=[t1, modb, a2b], writes=[atb])
                else:
                    t2 = t2r.next()
                    P.emit("act", lambda e: e.activation(out=t2[:, 0:tn], in_=t1[:, 0:tn], func=AF.Identity,
                                                         bias=modb[:, wi, 1, kc:kc + 1], scale=a2b[:, wi, kc:kc + 1]),
                           reads=[t1, modb, a2b], writes=[t2])
                    P.emit("dve", lambda e: e.tensor_copy(out=atb[:, kc, 0:tn], in_=t2[:, 0:tn]), reads=[t2], writes=[atb])
                    P.emit("dve", lambda e: e.tensor_tensor(out=yT[:, kc, 0:tn], in0=t2[:, 0:tn], in1=atb[:, kc, 0:tn], op=ALU.subtract),
                           reads=[t2, atb], writes=[yT])
            hk()
        if not moe:
            for pn in range(DFF // 512):
                wgb, wgv = load_w(wg, 0, KC, pn * 512, 512)
                wub, wuv = load_w(wu, 0, KC, pn * 512, 512)
                for jj in range(4):
                    ffn_gu(pn * 4 + jj, tn, wgb, wgv, wub, wuv, jj)
            for np_ in range(8):
                wab, wav = load_w(wd, 0, 22, np_ * 256, 256)
                wbb, wbv = load_w(wd, 22 * 128, 22, np_ * 256, 256)
                for jj in range(2):
                    j = np_ * 2 + jj
                    psD = psm.next()
                    parts = [(wab, wav[:, c, jj * 128:(jj + 1) * 128], fg, aTv[:, c, 0:tn]) for c in range(22)]
                    parts += [(wbb, wbv[:, c, jj * 128:(jj + 1) * 128], fg, aTv[:, 22 + c, 0:tn]) for c in range(22)]
                    mm_group(psD, tn, parts)
                    resid(psD, j, tn, 3, wi)
        else:
            if DBG.get("norouter"):
                P.emit("pool", lambda e: e.memset(bcs[:], 0.5), writes=[bcs])
            else:
                for tt in range(tn // 128):
                    router_tile(tt, DBG.get("rstage", 9))
            for e_ in range(DBG.get("nexp", NE)):
                for pn in range(6):
                    ncols = 512 if pn < 5 else 256
                    wgb, wgv = load_w(mg[e_], 0, KC, pn * 512, ncols)
                    wub, wuv = load_w(mu[e_], 0, KC, pn * 512, ncols)
                    for jj in range(ncols // 128):
                        ffn_gu(pn * 4 + jj, tn, wgb, wgv, wub, wuv, jj, bc_e=e_)
                for np_ in range(8):
                    wab, wav = load_w(md[e_], 0, 22, np_ * 256, 256)
                    for jj in range(2):
                        j = np_ * 2 + jj
                        psD = psm.next()
                        mm_group(psD, tn, [(wab, wav[:, c, jj * 128:(jj + 1) * 128], fg, aTv[:, c, 0:tn]) for c in range(22)])
                        resid(psD, j, tn, 3, wi)
        if last:
            rs2 = rstd_of_xs(tn)
            for kc in range(KC):
                P.emit("dve", lambda e, kc=kc: e.scalar_tensor_tensor(out=xs[:, kc, 0:tn], in0=xs[:, kc, 0:tn], scalar=fgb[:, kc:kc + 1],
                                                                      in1=rs2[:, 1, 0:tn], op0=ALU.mult, op1=ALU.mult),
                       reads=[xs, fgb, rs2], writes=[xs])
        P.dma("sp", xo[:, t0:t0 + tn].rearrange("(kc p) t -> p kc t", p=128), xs[:, :, 0:tn], reads=[xs], is_output=True)

    for (t0, tn) in CHUNKS[:DBG.get("nchunk", 9)]:
        if t0 >= TPC and not with_ctx:
            continue
        do_chunk(t0, tn, 1 if t0 >= TPC else 0)
    P.finish()
    return nc


def run_p4(nc, l, moe, with_ctx, inp, xT_cores, attnT_cores, FT_cores, gT_cores, mod):
    modv = np.stack([np.stack([mod[:, l, 32:48, wi], mod[:, l, 48:64, wi], mod[:, l, 64:80, wi], mod[:, l, 80:96, wi]], axis=1)
                     for wi in range(2)], axis=1)
    modv = np.ascontiguousarray(modv.astype(np.float32))
    maps = []
    for i in range(NCORES):
        m = {"xT": xT_cores[i], "attnT": attnT_cores[i], "FT": FT_cores[i], "gT": gT_cores[i],
             "wp": inp["w_attn_proj"][l], "wf": inp["w_four_proj"][l], "wo": inp["w_out"][l],
             "modv": modv, "gain2": fm(inp["norm_ffn_g"][l]), "fgain": fm(inp["final_norm_g"])}
        if moe:
            li = l // 2
            m["rw"] = np.ascontiguousarray(inp["router_w"][li].reshape(KC, 128, NE).transpose(1, 0, 2))
            m["mg"] = inp["moe_w_gate"][li]
            m["mu"] = inp["moe_w_up"][li]
            m["md"] = inp["moe_w_down"][li]
            m["ident"] = np.eye(128).astype(NPBF)
        else:
            li = l // 2
            m["wg"] = inp["ffn_w_gate"][li]
            m["wu"] = inp["ffn_w_up"][li]
            m["wd"] = inp["ffn_w_down"][li]
        maps.append(m)
    res = run(nc, maps)
    return [r["xo"] for r in res]


def kernel(**inp):
    inp = {k: np.asarray(v) for k, v in inp.items()}
    mod = run_p0(inp)
    x = inp["x"][0]
    ctx = inp["ctx"][0]
    xT_cores = [np.ascontiguousarray(np.concatenate([x[i * TPC:(i + 1) * TPC].T, ctx[i * CPC:(i + 1) * CPC].T], axis=1))
                for i in range(NCORES)]
    out = None
    for l in range(2):
        with_ctx = (l == 0)
        moe = (l % 2 == 1)
        last = (l == 1)
        r1 = run_p1(build_p1(), l, inp, xT_cores, mod)

        def gather(name, axis_tok):
            lat = np.concatenate([np.take(r[name], range(0, TPC), axis=axis_tok) for r in r1], axis=axis_tok)
            cx = np.concatenate([np.take(r[name], range(TPC, TT), axis=axis_tok) for r in r1], axis=axis_tok)
            return lat, cx
        q_lat, q_ctx = gather("qT", 1)
        k_lat, k_ctx = gather("kT", 1)
        v_lat, v_ctx = gather("v", 0)
        f_lat, f_ctx = gather("fT", 1)
        q_full = np.concatenate([q_lat, q_ctx], axis=1) if with_ctx else q_lat
        kT_full = np.concatenate([k_ctx, k_lat], axis=1)
        v_full = np.concatenate([v_ctx, v_lat], axis=0)
        oT = run_p2(build_p2(with_ctx), with_ctx, q_full, kT_full, v_full)
        fT_full = np.concatenate([f_lat, f_ctx], axis=1)
        FT = run_p3(build_p3a(), build_p3b(with_ctx), fT_full)

        def percore(a):
            res = []
            for i in range(NCORES):
                if with_ctx:
                    res.append(np.ascontiguousarray(np.concatenate(
                        [a[:, i * TPC:(i + 1) * TPC], a[:, SEQ + i * CPC:SEQ + (i + 1) * CPC]], axis=1)))
                else:
                    res.append(np.ascontiguousarray(a[:, i * TPC:(i + 1) * TPC]))
            return res
        attn_c = percore(oT)
        FT_c = percore(FT)
        ntok = TT if with_ctx else TPC
        g_c = [np.ascontiguousarray(r["gT"][:, :ntok]) for r in r1]
        x_c = [np.ascontiguousarray(xc[:, :ntok]) for xc in xT_cores]
        xo = run_p4(build_p4(moe, with_ctx, last), l, moe, with_ctx, inp, x_c, attn_c, FT_c, g_c, mod)
        if last:
            out = np.concatenate([xo[i][:, :TPC].T for i in range(NCORES)], axis=0)
        else:
            xT_cores = xo
    return np.ascontiguousarray(out.reshape(1, SEQ, D).astype(np.float32))
```
